# Optimizing a Trainium2 kernel written in Bass

```python
import jax, jax.numpy as jnp
from jax import lax
import numpy as np

D_MODEL = 1024
BATCH = 2
SEQ = 8192
DEPTH = 1

D_POOL = D_MODEL // 2
POOL_WINDOWS = (2, 4, 8, 16)
N_POOL_GROUPS = len(POOL_WINDOWS)
POOL_GROUP_DIM = D_POOL // N_POOL_GROUPS
V_HEAD_DIM = 64
N_HEADS = (D_MODEL // 2) // V_HEAD_DIM
D_ATTN = N_HEADS * V_HEAD_DIM
D_MIX = D_POOL + D_ATTN
QK_NOPE_DIM = 64
QK_ROPE_DIM = 32
QK_DIM = QK_NOPE_DIM + QK_ROPE_DIM
Q_LORA_RANK = D_MODEL // 4
KV_LORA_RANK = D_MODEL // 8
ROPE_THETA = 10000.0
Q_BLOCK = 128
D_IN_PROJ = D_POOL + Q_LORA_RANK + KV_LORA_RANK + QK_ROPE_DIM
N_EXPERTS = 32
TOP_K = 4
D_EXPERT = D_MODEL
SWIGLU_ALPHA = 1.702
SWIGLU_LIMIT = 7.0
MOE_BLOCK = 128
RMS_EPS = 1e-6

kernel_name = 'hymba_pool_mla_moe_block'


def rmsnorm(x, g):
    xf = x.astype(jnp.float32)
    y = xf * lax.rsqrt(jnp.mean(xf * xf, axis=-1, keepdims=True) + RMS_EPS)
    return (y * g.astype(jnp.float32)).astype(x.dtype)


def rope_tables(positions):
    half = QK_ROPE_DIM // 2
    inv = ROPE_THETA ** (-jnp.arange(half, dtype=jnp.float32) / half)
    ang = positions.astype(jnp.float32)[..., None] * inv
    return jnp.cos(ang), jnp.sin(ang)


def apply_rope(x, cos, sin):
    half = QK_ROPE_DIM // 2
    xf = x.astype(jnp.float32)
    x1, x2 = xf[..., :half], xf[..., half:]
    out = jnp.concatenate([x1 * cos - x2 * sin, x2 * cos + x1 * sin], axis=-1)
    return out.astype(x.dtype)


def pool_mixer(u, w_pool, b_pool, pool_scale):
    B, S, _ = u.shape
    uf = u.reshape(B, S, N_POOL_GROUPS, POOL_GROUP_DIM).astype(jnp.float32)
    csum = jnp.pad(jnp.cumsum(uf, axis=1), ((0, 0), (1, 0), (0, 0), (0, 0)))
    t = jnp.arange(S)
    pooled = []
    for g, w in enumerate(POOL_WINDOWS):
        c = csum[:, :, g]
        lo = jnp.pad(c[:, :S + 1 - w], ((0, 0), (w - 1, 0), (0, 0)))
        cnt = jnp.minimum(t + 1, w).astype(jnp.float32)[None, :, None]
        pooled.append((c[:, 1:] - lo) / cnt)
    pooled = jnp.stack(pooled, axis=2)
    diff = (pooled - uf).astype(u.dtype)
    mixed = jnp.einsum('bsgc,gcd->bsgd', diff, w_pool) + b_pool
    return mixed.reshape(B, S, D_POOL) * pool_scale


def mla_attention(q_lat, kv_lat, k_rope_raw, cos, sin, g_q_a, w_q_b, g_kv_a, w_kv_b):
    B, S, _ = q_lat.shape
    q = (rmsnorm(q_lat, g_q_a) @ w_q_b).reshape(B, S, N_HEADS, QK_DIM)
    q_nope = q[..., :QK_NOPE_DIM]
    q_rope = apply_rope(q[..., QK_NOPE_DIM:], cos[:, :, None, :], sin[:, :, None, :])
    kv = (rmsnorm(kv_lat, g_kv_a) @ w_kv_b).reshape(B, S, N_HEADS, QK_NOPE_DIM + V_HEAD_DIM)
    k_nope, v = kv[..., :QK_NOPE_DIM], kv[..., QK_NOPE_DIM:]
    k_rope = apply_rope(k_rope_raw, cos, sin)
    scale = QK_DIM ** -0.5
    nb = S // Q_BLOCK
    qn_b = q_nope.reshape(B, nb, Q_BLOCK, N_HEADS, QK_NOPE_DIM).transpose(1, 0, 2, 3, 4)
    qr_b = q_rope.reshape(B, nb, Q_BLOCK, N_HEADS, QK_ROPE_DIM).transpose(1, 0, 2, 3, 4)
    k_idx = jnp.arange(S)

    def attend(args):
        qn, qr, blk = args
        s = (jnp.einsum('bqhd,bkhd->bhqk', qn, k_nope, preferred_element_type=jnp.float32)
             + jnp.einsum('bqhr,bkr->bhqk', qr, k_rope, preferred_element_type=jnp.float32)) * scale
        q_idx = blk * Q_BLOCK + jnp.arange(Q_BLOCK)
        s = jnp.where(k_idx[None, :] <= q_idx[:, None], s, -jnp.inf)
        p = jax.nn.softmax(s, axis=-1).astype(v.dtype)
        return jnp.einsum('bhqk,bkhd->bqhd', p, v)

    out = lax.map(attend, (qn_b, qr_b, jnp.arange(nb)))
    return out.transpose(1, 0, 2, 3, 4).reshape(B, S, D_ATTN)


def moe(h, w_router, b_router, w_gate_up, b_gate_up, w_down, b_down):
    B, S, D = h.shape
    T = B * S
    ht = h.reshape(T, D)
    logits = (ht @ w_router + b_router).astype(jnp.float32)
    top_vals, top_idx = lax.top_k(logits, TOP_K)
    gates = jax.nn.softmax(top_vals, axis=-1)
    n_assign = T * TOP_K
    e_flat = top_idx.reshape(-1).astype(jnp.int32)
    tok_flat = jnp.repeat(jnp.arange(T, dtype=jnp.int32), TOP_K)
    g_flat = gates.reshape(-1)
    order = jnp.argsort(e_flat)
    e_sorted = e_flat[order]
    counts = jnp.zeros((N_EXPERTS,), jnp.int32).at[e_flat].add(1)
    padded = ((counts + MOE_BLOCK - 1) // MOE_BLOCK) * MOE_BLOCK
    start = jnp.cumsum(counts) - counts
    pad_end = jnp.cumsum(padded)
    pad_start = pad_end - padded
    dest = pad_start[e_sorted] + (jnp.arange(n_assign, dtype=jnp.int32) - start[e_sorted])
    n_blocks = n_assign // MOE_BLOCK + N_EXPERTS
    n_rows = n_blocks * MOE_BLOCK
    row_tok = jnp.full((n_rows,), T, jnp.int32).at[dest].set(tok_flat[order])
    row_gate = jnp.zeros((n_rows,), jnp.float32).at[dest].set(g_flat[order])
    block_expert = jnp.minimum(
        jnp.searchsorted(pad_end, jnp.arange(n_blocks, dtype=jnp.int32) * MOE_BLOCK, side='right'),
        N_EXPERTS - 1)
    h_pad = jnp.concatenate([ht, jnp.zeros((1, D), ht.dtype)], axis=0)
    xb = h_pad[row_tok].reshape(n_blocks, MOE_BLOCK, D)

    def expert_block(args):
        xblk, e = args
        gu = xblk @ w_gate_up[e] + b_gate_up[e]
        gate = jnp.minimum(gu[:, :D_EXPERT], SWIGLU_LIMIT)
        up = jnp.clip(gu[:, D_EXPERT:], -SWIGLU_LIMIT, SWIGLU_LIMIT)
        act = gate * jax.nn.sigmoid(SWIGLU_ALPHA * gate) * (up + 1)
        return act @ w_down[e] + b_down[e]

    yb = lax.map(expert_block, (xb, block_expert)).reshape(n_rows, D)
    y = jnp.zeros((T + 1, D), h.dtype).at[row_tok].add(row_gate[:, None].astype(h.dtype) * yb)
    return y[:T].reshape(B, S, D)


def setup_inputs(seed: int = 0) -> dict:
    key = jax.random.key(seed)
    ks = jax.random.split(key, 24)
    L, D, E, F = DEPTH, D_MODEL, N_EXPERTS, D_EXPERT
    nrm = lambda k, shape, fan_in: jax.random.normal(k, shape, jnp.float32) * (fan_in ** -0.5)
    gain = lambda k, shape: 1.0 + 0.05 * jax.random.normal(k, shape, jnp.float32)
    small = lambda k, shape: 0.01 * jax.random.normal(k, shape, jnp.float32)
    x = jax.random.normal(ks[0], (BATCH, SEQ, D), jnp.float32)
    positions = jnp.broadcast_to(jnp.arange(SEQ, dtype=jnp.int32), (BATCH, SEQ))
    return {
        'x': x,
        'positions': positions,
        'g_attn_norm': gain(ks[1], (L, D)),
        'w_in': nrm(ks[2], (L, D, D_IN_PROJ), D),
        'w_pool': nrm(ks[3], (L, N_POOL_GROUPS, POOL_GROUP_DIM, POOL_GROUP_DIM), POOL_GROUP_DIM),
        'b_pool': small(ks[4], (L, N_POOL_GROUPS, POOL_GROUP_DIM)),
        'pool_scale': gain(ks[5], (L, D_POOL)),
        'g_q_a': gain(ks[6], (L, Q_LORA_RANK)),
        'w_q_b': nrm(ks[7], (L, Q_LORA_RANK, N_HEADS * QK_DIM), Q_LORA_RANK),
        'g_kv_a': gain(ks[8], (L, KV_LORA_RANK)),
        'w_kv_b': nrm(ks[9], (L, KV_LORA_RANK, N_HEADS * (QK_NOPE_DIM + V_HEAD_DIM)), KV_LORA_RANK),
        'g_out_pool': gain(ks[10], (L, D_POOL)),
        'g_out_attn': gain(ks[11], (L, D_ATTN)),
        'w_out': nrm(ks[12], (L, D_MIX, D), D_MIX),
        'g_ffn_norm': gain(ks[13], (L, D)),
        'w_router': nrm(ks[14], (L, D, E), D),
        'b_router': small(ks[15], (L, E)),
        'w_gate_up': nrm(ks[16], (L, E, D, 2 * F), D),
        'b_gate_up': small(ks[17], (L, E, 2 * F)),
        'w_down': nrm(ks[18], (L, E, F, D), F),
        'b_down': small(ks[19], (L, E, D)),
        'g_final': gain(ks[20], (D,)),
    }


def reference(x, positions, g_attn_norm, w_in, w_pool, b_pool, pool_scale, g_q_a, w_q_b,
              g_kv_a, w_kv_b, g_out_pool, g_out_attn, w_out, g_ffn_norm, w_router, b_router,
              w_gate_up, b_gate_up, w_down, b_down, g_final):
    cos, sin = rope_tables(positions)
    o1 = D_POOL
    o2 = o1 + Q_LORA_RANK
    o3 = o2 + KV_LORA_RANK
    for l in range(DEPTH):
        h = rmsnorm(x, g_attn_norm[l])
        proj = h @ w_in[l]
        u, q_lat, kv_lat, k_r = proj[..., :o1], proj[..., o1:o2], proj[..., o2:o3], proj[..., o3:]
        y_pool = pool_mixer(u, w_pool[l], b_pool[l], pool_scale[l])
        y_attn = mla_attention(q_lat, kv_lat, k_r, cos, sin, g_q_a[l], w_q_b[l], g_kv_a[l], w_kv_b[l])
        mix = jnp.concatenate([rmsnorm(y_pool, g_out_pool[l]), rmsnorm(y_attn, g_out_attn[l])], axis=-1)
        x = x + mix @ w_out[l]
        h = rmsnorm(x, g_ffn_norm[l])
        x = x + moe(h, w_router[l], b_router[l], w_gate_up[l], b_gate_up[l], w_down[l], b_down[l])
    return rmsnorm(x, g_final)
```

```python
import numpy as np
from contextlib import ExitStack
import ml_dtypes
import concourse.bass as bass
import concourse.mybir as mybir
from concourse.bass_utils import run_bass_kernel_spmd

F32 = mybir.dt.float32
BF16 = mybir.dt.bfloat16
I32 = mybir.dt.int32
AF = mybir.ActivationFunctionType
ALU = mybir.AluOpType

ENGS = ('pe', 'act', 'dve', 'pool', 'sp')
EPS = 1e-6
PI = float(np.pi)
TWO_PI = float(2 * np.pi)


class Op:
    __slots__ = ('eng', 'fn', 'waits', 'flag', 'count', 'dkey', 'dcount', 'done')

    def __init__(self, eng, fn):
        self.eng = eng
        self.fn = fn
        self.waits = []
        self.flag = False
        self.count = None
        self.dkey = None
        self.dcount = None
        self.done = False


class Sched:
    def __init__(self, nc, stack):
        self.nc = nc
        self.stack = stack
        self.sem = {e: stack.enter_context(nc.semaphore('s_' + e)) for e in ENGS}
        self.dsem = {}
        self.dcnt = {}
        self.ops = {e: [] for e in ENGS}
        self.base = {e: 0 for e in ENGS}
        self.lastw = {}
        self.readers = {}
        self.waited = {e: {} for e in ENGS}
        self.no_barrier_keys = set()
        self.flush_id = 0
        self._regs = {}
        self.enabled = True

    def _dsem(self, key):
        if key not in self.dsem:
            self.dsem[key] = self.stack.enter_context(self.nc.semaphore('d_' + str(key)))
            self.dcnt[key] = 0
        return self.dsem[key]

    def op(self, eng, fn, r=(), w=(), dma=None):
        if not self.enabled:
            return None
        o = Op(eng, fn)
        deps = []
        for t in r:
            d = self.lastw.get(t)
            if d is not None:
                deps.append(d)
        for t in w:
            d = self.lastw.get(t)
            if d is not None:
                deps.append(d)
            rd = self.readers.get(t)
            if rd:
                deps.extend(rd.values())
        seen = set()
        for d in deps:
            if d is o or id(d) in seen or d.done:
                continue
            seen.add(id(d))
            if d.dkey is None and d.eng == 'pe' and eng == 'pe':
                continue
            if d.dkey is None:
                d.flag = True
                o.waits.append(d)
            else:
                o.waits.append(('d', d.dkey, self.dcnt[d.dkey]))
        if dma is not None:
            self._dsem(dma)
            self.dcnt[dma] += 16
            o.dkey = dma
            o.dcount = self.dcnt[dma]
        for t in w:
            self.lastw[t] = o
            self.readers[t] = {}
        for t in r:
            rk = ('dma', o.dkey) if o.dkey is not None else o.eng
            self.readers.setdefault(t, {})[rk] = o
        self.ops[eng].append(o)
        return o

    def reg(self, eng, val):
        key = (self.flush_id, val)
        if key not in self._regs:
            self._regs[key] = eng.to_reg(val)
        return self._regs[key]

    def pe(self, fn, r=(), w=()):
        return self.op('pe', fn, r, w)

    def act(self, fn, r=(), w=()):
        return self.op('act', fn, r, w)

    def dve(self, fn, r=(), w=()):
        return self.op('dve', fn, r, w)

    def pool(self, fn, r=(), w=()):
        return self.op('pool', fn, r, w)

    def dma(self, fn, key, r=(), w=(), q='sp'):
        return self.op(q, fn, r, w, dma=key)

    def flush(self, final_dma_keys=()):
        nc = self.nc
        if not any(self.ops[e] for e in ENGS):
            return
        lasts = []
        for e in ENGS:
            for o in reversed(self.ops[e]):
                if o.dkey is None and o.fn is not None:
                    o.flag = True
                    lasts.append(o)
                    break
        for e in ENGS:
            b = Op(e, None)
            b.waits = [o for o in lasts if o.eng != e]
            for k in self.dcnt:
                if k not in self.no_barrier_keys:
                    b.waits.append(('d', k, self.dcnt[k]))
            self.ops[e].append(b)
        for e in ENGS:
            c = self.base[e]
            for o in self.ops[e]:
                if o.dkey is None and o.flag:
                    c += 1
                    o.count = c
            self.base[e] = c
        sem, dsem, waited, ops = self.sem, self.dsem, self.waited, self.ops

        def replay(ename):
            def body(eng):
                wd = waited[ename]
                for o in ops[ename]:
                    for d in o.waits:
                        if isinstance(d, tuple):
                            k, s, v = ('d', d[1]), dsem[d[1]], d[2]
                        else:
                            k, s, v = d.eng, sem[d.eng], d.count
                        if wd.get(k, 0) < v:
                            eng.wait_ge(s, v)
                            wd[k] = v
                    if o.fn is None:
                        continue
                    inst = o.fn(eng)
                    if o.dkey is not None:
                        inst.then_inc(dsem[o.dkey], 16)
                    elif o.flag:
                        inst.then_inc(sem[ename], 1)
            return body

        self.flush_id += 1
        with nc.Block() as blk:
            blk.tensor(replay('pe'))
            blk.scalar(replay('act'))
            blk.vector(replay('dve'))
            blk.gpsimd(replay('pool'))
            blk.sync(replay('sp'))
        for e in ENGS:
            for o in self.ops[e]:
                if o.dkey is None:
                    o.done = True
            self.ops[e] = []
        for t in list(self.lastw.keys()):
            if self.lastw[t].done:
                del self.lastw[t]
        for t in list(self.readers.keys()):
            rd = self.readers[t]
            for k in [k for k, o in rd.items() if o.done]:
                del rd[k]
            if not rd:
                del self.readers[t]


def build(stage=99, debug=False, upto='Z'):
    nc = bass.Bass("TRN2", target_bir_lowering=False)
    D = lambda name, shape, dt, kind="ExternalInput": nc.dram_tensor(name, shape, dt, kind=kind).ap()
    x_all = D("x_all", [8192, 1024], F32)
    x_own = D("x_own", [2304, 1024], F32)
    pos_all = D("pos_all", [1, 8192], I32)
    pos_own = D("pos_own", [1, 2048], I32)
    masks_d = D("masks", [128, 8192], BF16)
    invcnt_d = D("invcnt", [128, 64], F32)
    invf_d = D("invf", [128, 1], F32)
    g_attn_d = D("g_attn", [128, 8], F32)
    w_in = D("w_in", [1024, 928], F32)
    w_pool_d = D("w_pool", [128, 512], F32)
    b_pool_d = D("b_pool", [128, 4], F32)
    pool_scale_d = D("pool_scale", [128, 4], F32)
    g_q_d = D("g_q", [128, 2], F32)
    w_q_b = D("w_q_b", [256, 768], F32)
    g_kv_d = D("g_kv", [128, 1], F32)
    w_kv_b = D("w_kv_b", [128, 1024], F32)
    g_op_d = D("g_op", [128, 4], F32)
    g_oa_d = D("g_oa", [64, 8], F32)
    w_out = D("w_out", [1024, 1024], F32)
    g_ffn_d = D("g_ffn", [1, 1024], F32)
    g_fin_d = D("g_fin", [1, 1024], F32)
    w_r = D("w_r", [1024, 32], F32)
    b_r = D("b_r", [1, 32], F32)
    w_gu_p = [D("w_gu%d" % k, [8, 1024, 2048], F32) for k in range(4)] if stage >= 2 else None
    b_gu_d = D("b_gu", [128, 512], F32)
    w_d_p = [D("w_d%d" % k, [8, 1024, 1024], F32) for k in range(4)] if stage >= 2 else None
    b_d_d = D("b_d", [32, 1024], F32)
    y = D("y", [2048, 1024], F32, kind="ExternalOutput")
    if debug:
        dbg_x1 = D("dbg_x1", [2048, 1024], F32, kind="ExternalOutput")
        dbg_yp = D("dbg_yp", [128, 8192], BF16, kind="ExternalOutput")
        dbg_ya = D("dbg_ya", [64, 16384], BF16, kind="ExternalOutput")
        dbg_cn = D("dbg_cn", [128, 8192], BF16, kind="ExternalOutput")
        dbg_kt = D("dbg_kt", [96, 8192], BF16, kind="ExternalOutput")
        dbg_gates = D("dbg_gates", [128, 512], F32, kind="ExternalOutput")
        dbg_rs = D("dbg_rs", [128, 32], F32, kind="ExternalOutput")
    final_keys = []
    CAP = 384
    NROWS = 32 * CAP
    BIGV = 1.0e6
    xs = nc.dram_tensor("xs_scratch", [NROWS, 1024], BF16, kind="Internal").ap()
    ys = nc.dram_tensor("ys_scratch", [NROWS, 1024], F32, kind="Internal").ap()
    wbf_gu = nc.dram_tensor("wbf_gu", [32, 1024, 2048], BF16, kind="Internal").ap() if stage >= 2 else None
    wbf_d = nc.dram_tensor("wbf_d", [32, 1024, 1024], BF16, kind="Internal").ap() if stage >= 2 else None

    with ExitStack() as top:
        S = Sched(nc, top)
        uid = [0]

        def T(st, name, shape, dt):
            uid[0] += 1
            return st.enter_context(nc.sbuf_tensor("%s_%d" % (name, uid[0]), shape, dt))

        def PS(st, name, shape, dt):
            uid[0] += 1
            return st.enter_context(nc.psum_tensor("%s_%d" % (name, uid[0]), shape, dt))
        BIG0 = T(top, "BIG0", [128, 16384], F32)
        BIG1 = T(top, "BIG1", [128, 16384], BF16)
        BIG2 = T(top, "BIG2", [128, 24576], BF16)
        B0 = BIG0[:]
        B0b = BIG0[:].bitcast(BF16)
        B0i = BIG0[:].bitcast(I32)
        B1 = BIG1[:]
        B2 = BIG2[:]
        ident_f = T(top, "ident_f", [128, 128], F32)
        ident_b = T(top, "ident_b", [128, 128], BF16)
        ones_b = T(top, "ones_b", [128, 128], BF16)
        sel_f = T(top, "sel_f", [128, 64], F32)
        epsc = T(top, "epsc", [128, 1], F32)
        invf = T(top, "invf_t", [128, 1], F32)
        rstdp = T(top, "rstdp", [128, 16], F32)
        rstda = T(top, "rstda", [128, 16], F32)
        ssa = T(top, "ssa", [128, 16], F32)
        gates = T(top, "gates", [128, 512], F32)
        g_oa = T(top, "g_oa_t", [64, 8], F32)
        junk = T(top, "junk", [128, 1024], BF16)
        Lst = T(top, "Lst", [128, 128], BF16)
        tri_f = T(top, "tri_f", [128, 128], F32)
        CE = T(top, "CE", [128, 32], F32)
        CEi = T(top, "CEi", [128, 32], I32)
        carry = T(top, "carry", [128, 32], F32)
        desti = T(top, "desti", [128, 64], I32)
        gk = T(top, "gk", [128, 64], F32)

        ypoolT = B2[:, 0:8192].rearrange("p (g t) -> p g t", g=4)
        yattnT = B2[:, 8192:24576].rearrange("p (h t) -> p h t", h=8)

        S.pool(lambda e: e.memset(ident_f[:], 0.0), w=['ident_f'])
        S.pool(lambda e: e.affine_select(out=ident_f[:], in_=ident_f[:], pattern=[[-1, 128]],
                                         compare_op=ALU.not_equal, fill=1.0, base=0, channel_multiplier=1),
               r=['ident_f'], w=['ident_f'])
        S.dve(lambda e: e.tensor_copy(out=ident_b[:], in_=ident_f[:]), r=['ident_f'], w=['ident_b'])
        S.pool(lambda e: e.memset(ones_b[:], 1.0), w=['ones_b'])
        S.pool(lambda e: e.memset(epsc[:], EPS), w=['epsc'])
        S.pool(lambda e: e.memset(sel_f[:], 0.0), w=['sel_f'])
        S.pool(lambda e: e.memset(sel_f[64:65, :], 1.0), r=['sel_f'], w=['sel_f'])
        S.dma(lambda e: e.dma_start(out=invf[:], in_=invf_d), 'c0', w=['invf'])
        S.pool(lambda e: e.memset(tri_f[:], 1.0), w=['tri_f'])
        S.pool(lambda e: e.affine_select(out=tri_f[:], in_=tri_f[:], pattern=[[1, 128]], compare_op=ALU.is_gt, fill=0.0, base=0, channel_multiplier=-1),
               r=['tri_f'], w=['tri_f'])
        S.dve(lambda e: e.tensor_copy(out=Lst[:], in_=tri_f[:]), r=['tri_f'], w=['Lst'])
        S.pool(lambda e: e.memset(gk[:], 0.0), w=['gk0'])
        S.pool(lambda e: e.memset(carry[:], 0.0), w=['carry'])
        S.pool(lambda e: e.iota(out=CEi[:], pattern=[[CAP, 32]], base=0, channel_multiplier=0), w=['CEi'])
        S.dve(lambda e: e.tensor_copy(out=CE[:], in_=CEi[:]), r=['CEi'], w=['CE'])
        S.pool(lambda e: e.memset(B0b[:, 8192:16384], 0.0), w=['zsrc'])
        for zz in range(NROWS // 1024):
            S.dma(lambda e, zz=zz: e.dma_start(out=xs[zz * 1024:(zz + 1) * 1024, :].rearrange("(n p) d -> p n d", p=128),
                                               in_=B0b[:, 8192:16384].rearrange("p (n d) -> p n d", n=8)), 'zf', r=['zsrc'])
        S.dma(lambda e: e.dma_start(out=g_oa[:], in_=g_oa_d), 'c1', w=['g_oa'])

        def rms_tile(src_ap, slot_tag, ss_ap, ss_tag, xb_ap, xb_tag, ncols=1024, inv_n=1.0 / 1024):
            S.act(lambda e: e.activation(out=junk[:, 0:ncols], in_=src_ap, func=AF.Square, accum_out=ss_ap),
                  r=[slot_tag], w=['junk', ss_tag])
            S.act(lambda e: e.activation(out=ss_ap, in_=ss_ap, func=AF.Ln, bias=epsc[:, 0:1], scale=inv_n),
                  r=[ss_tag, 'epsc'], w=[ss_tag])
            S.act(lambda e: e.activation(out=ss_ap, in_=ss_ap, func=AF.Exp, scale=-0.5), r=[ss_tag], w=[ss_tag])
            if xb_ap is not None:
                S.dve(lambda e: e.tensor_scalar(out=xb_ap, in0=src_ap, scalar1=ss_ap, scalar2=None, op0=ALU.mult),
                      r=[slot_tag, ss_tag], w=[xb_tag])

        def rope_tables(st, pos_src_ap, n, posi, posf, ang, kf, sn, cs, tagp, otag=None):
            otag = otag or tagp
            sl = slice(64, 96)
            S.dma(lambda e: e.dma_start(out=posi[sl, 0:n], in_=pos_src_ap.partition_broadcast(32)), 'pos', w=[tagp + 'posi'])
            S.dve(lambda e: e.tensor_copy(out=posf[sl, 0:n], in_=posi[sl, 0:n]), r=[tagp + 'posi'], w=[tagp + 'posf'])
            S.dve(lambda e: e.tensor_scalar(out=ang[sl, 0:n], in0=posf[sl, 0:n], scalar1=invf[sl, 0:1], scalar2=None, op0=ALU.mult),
                  r=[tagp + 'posf', 'invf'], w=[tagp + 'ang'])
            S.dve(lambda e: e.tensor_scalar(out=posi[sl, 0:n], in0=ang[sl, 0:n], scalar1=1.0 / TWO_PI, scalar2=None, op0=ALU.mult),
                  r=[tagp + 'ang', tagp + 'posf'], w=[tagp + 'posi'])
            S.dve(lambda e: e.tensor_copy(out=kf[sl, 0:n], in_=posi[sl, 0:n]), r=[tagp + 'posi'], w=[tagp + 'kf'])
            C1 = 6.28125
            C2 = float(TWO_PI - C1)
            S.dve(lambda e: e.scalar_tensor_tensor(out=ang[sl, 0:n], in0=kf[sl, 0:n], scalar=-C1, in1=ang[sl, 0:n], op0=ALU.mult, op1=ALU.add),
                  r=[tagp + 'kf', tagp + 'ang'], w=[tagp + 'ang'])
            S.dve(lambda e: e.scalar_tensor_tensor(out=ang[sl, 0:n], in0=kf[sl, 0:n], scalar=-C2, in1=ang[sl, 0:n], op0=ALU.mult, op1=ALU.add),
                  r=[tagp + 'kf', tagp + 'ang'], w=[tagp + 'ang'])
            S.dve(lambda e: e.tensor_scalar(out=kf[sl, 0:n], in0=ang[sl, 0:n], scalar1=PI, scalar2=-TWO_PI, op0=ALU.is_gt, op1=ALU.mult),
                  r=[tagp + 'ang'], w=[tagp + 'kf'])
            S.dve(lambda e: e.tensor_tensor(out=ang[sl, 0:n], in0=ang[sl, 0:n], in1=kf[sl, 0:n], op=ALU.add),
                  r=[tagp + 'ang', tagp + 'kf'], w=[tagp + 'ang'])
            S.dve(lambda e: e.tensor_scalar(out=kf[sl, 0:n], in0=ang[sl, 0:n], scalar1=-PI, scalar2=TWO_PI, op0=ALU.is_lt, op1=ALU.mult),
                  r=[tagp + 'ang'], w=[tagp + 'kf'])
            S.dve(lambda e: e.tensor_tensor(out=ang[sl, 0:n], in0=ang[sl, 0:n], in1=kf[sl, 0:n], op=ALU.add),
                  r=[tagp + 'ang', tagp + 'kf'], w=[tagp + 'ang'])
            S.act(lambda e: e.activation(out=sn[sl, 0:n], in_=ang[sl, 0:n], func=AF.Sin), r=[tagp + 'ang'], w=[otag + 'sn'])
            S.dve(lambda e: e.tensor_scalar(out=kf[sl, 0:n], in0=ang[sl, 0:n], scalar1=PI / 2, scalar2=-TWO_PI, op0=ALU.is_gt, op1=ALU.mult),
                  r=[tagp + 'ang'], w=[tagp + 'kf'])
            S.dve(lambda e: e.scalar_tensor_tensor(out=ang[sl, 0:n], in0=ang[sl, 0:n], scalar=PI / 2, in1=kf[sl, 0:n], op0=ALU.add, op1=ALU.add),
                  r=[tagp + 'ang', tagp + 'kf'], w=[tagp + 'ang'])
            S.act(lambda e: e.activation(out=cs[sl, 0:n], in_=ang[sl, 0:n], func=AF.Sin), r=[tagp + 'ang'], w=[otag + 'cs'])

        with ExitStack() as p2:
            Wkvlat = T(p2, "Wkvlat", [128, 8, 128], BF16)
            Wkr = T(p2, "Wkr", [128, 8, 96], BF16)
            Wkrot = T(p2, "Wkrot", [128, 8, 96], BF16)
            Wq = T(p2, "Wq", [128, 2, 768], BF16)
            Wqrot = T(p2, "Wqrot", [128, 2, 768], BF16)
            Wkv = T(p2, "Wkv", [128, 1024], BF16)
            wpool = T(p2, "wpool", [128, 512], BF16)
            qnT = T(p2, "qnT", [128, 2, 2048], BF16)
            g_attn = T(p2, "g_attn_t", [128, 8], F32)
            ng_attn = T(p2, "ng_attn_t", [128, 8], F32)
            g_q = T(p2, "g_q_t", [128, 2], F32)
            ng_q = T(p2, "ng_q_t", [128, 2], F32)
            g_kv = T(p2, "g_kv_t", [128, 1], F32)
            g_op = T(p2, "g_op_t", [128, 4], F32)
            bpool = T(p2, "bpool_t", [128, 4], F32)
            pscale = T(p2, "pscale_t", [128, 4], F32)
            bsc = T(p2, "bsc", [128, 4], F32)
            scg = T(p2, "scg", [128, 4], F32)
            bsg = T(p2, "bsg", [128, 4], F32)
            invcnt = T(p2, "invcnt_t", [128, 64], F32)

            Win_uq = B2[:, 8192:8192 + 6144].rearrange("p (c n) -> p c n", c=8)
            for i, (dst, src) in enumerate([(g_attn, g_attn_d), (g_q, g_q_d), (g_kv, g_kv_d), (g_op, g_op_d),
                                            (bpool, b_pool_d), (pscale, pool_scale_d), (invcnt, invcnt_d)]):
                S.dma(lambda e, dst=dst, src=src: e.dma_start(out=dst[:], in_=src), 'c%d' % (2 + i), w=[('cst', i)])
            S.dve(lambda e: e.tensor_scalar(out=ng_attn[:], in0=g_attn[:], scalar1=-1.0, scalar2=None, op0=ALU.mult), r=[('cst', 0)], w=['ng_attn'])
            S.dve(lambda e: e.tensor_scalar(out=ng_q[:], in0=g_q[:], scalar1=-1.0, scalar2=None, op0=ALU.mult), r=[('cst', 1)], w=['ng_q'])
            S.dve(lambda e: e.tensor_tensor(out=bsc[:], in0=bpool[:], in1=pscale[:], op=ALU.mult), r=[('cst', 4), ('cst', 5)], w=['bsc'])
            S.dve(lambda e: e.tensor_tensor(out=scg[:], in0=pscale[:], in1=g_op[:], op=ALU.mult), r=[('cst', 3), ('cst', 5)], w=['scg'])
            S.dve(lambda e: e.tensor_tensor(out=bsg[:], in0=bsc[:], in1=g_op[:], op=ALU.mult), r=[('cst', 3), 'bsc'], w=['bsg'])
            S.pool(lambda e: e.memset(Wkr[:], 0.0), w=['Wkr'])
            S.pool(lambda e: e.memset(Wkrot[:], 0.0), w=['Wkrot'])
            S.pool(lambda e: e.memset(Wqrot[:], 0.0), w=['Wqrot'])
            S.dma(lambda e: e.dma_start(out=wpool[:], in_=w_pool_d), 'wpool', w=['wpool'], q='pool')
            stg = B0[:, 0:4096]
            for half in range(2):
                stv = stg[:, 0:3712].rearrange("p (c n) -> p c n", c=4)
                S.dma(lambda e, half=half, stv=stv: e.dma_start(
                    out=stv, in_=w_in[half * 512:(half + 1) * 512, :].rearrange("(c p) n -> p c n", p=128)), 'stg', w=['stg'])
                for c in range(4):
                    cc = half * 4 + c
                    gs = g_attn[:, cc:cc + 1]
                    ngs = ng_attn[:, cc:cc + 1]
                    S.dve(lambda e, c=c, cc=cc, gs=gs, stv=stv: e.tensor_scalar(out=Win_uq[:, cc, :], in0=stv[:, c, 0:768], scalar1=gs, scalar2=None, op0=ALU.mult),
                          r=['stg', ('cst', 0)], w=['Win_uq'])
                    S.dve(lambda e, c=c, cc=cc, gs=gs, stv=stv: e.tensor_scalar(out=Wkvlat[:, cc, :], in0=stv[:, c, 768:896], scalar1=gs, scalar2=None, op0=ALU.mult),
                          r=['stg', ('cst', 0)], w=['Wkvlat'])
                    S.dve(lambda e, c=c, cc=cc, gs=gs, stv=stv: e.tensor_scalar(out=Wkr[:, cc, 64:96], in0=stv[:, c, 896:928], scalar1=gs, scalar2=None, op0=ALU.mult),
                          r=['stg', ('cst', 0), 'Wkr'], w=['Wkr'])
                    S.dve(lambda e, c=c, cc=cc, ngs=ngs, stv=stv: e.tensor_scalar(out=Wkrot[:, cc, 64:80], in0=stv[:, c, 912:928], scalar1=ngs, scalar2=None, op0=ALU.mult),
                          r=['stg', 'ng_attn', 'Wkrot'], w=['Wkrot'])
                    S.dve(lambda e, c=c, cc=cc, gs=gs, stv=stv: e.tensor_scalar(out=Wkrot[:, cc, 80:96], in0=stv[:, c, 896:912], scalar1=gs, scalar2=None, op0=ALU.mult),
                          r=['stg', ('cst', 0), 'Wkrot'], w=['Wkrot'])
            stq = stg[:, 0:1536].rearrange("p (c n) -> p c n", c=2)
            S.dma(lambda e: e.dma_start(out=stq, in_=w_q_b.rearrange("(c p) n -> p c n", p=128)), 'stg', w=['stg'])
            for c in range(2):
                S.dve(lambda e, c=c: e.tensor_scalar(out=Wq[:, c, :], in0=stq[:, c, :], scalar1=g_q[:, c:c + 1], scalar2=None, op0=ALU.mult),
                      r=['stg', ('cst', 1)], w=['Wq'])
                sq4 = stq[:, c, :].rearrange("p (h d) -> p h d", h=8)
                wr4 = Wqrot[:, c, :].rearrange("p (h d) -> p h d", h=8)
                S.dve(lambda e, c=c, sq4=sq4, wr4=wr4: e.tensor_scalar(out=wr4[:, :, 64:80], in0=sq4[:, :, 80:96], scalar1=ng_q[:, c:c + 1], scalar2=None, op0=ALU.mult),
                      r=['stg', 'ng_q', 'Wqrot'], w=['Wqrot'])
                S.dve(lambda e, c=c, sq4=sq4, wr4=wr4: e.tensor_scalar(out=wr4[:, :, 80:96], in0=sq4[:, :, 64:80], scalar1=g_q[:, c:c + 1], scalar2=None, op0=ALU.mult),
                      r=['stg', ('cst', 1), 'Wqrot'], w=['Wqrot'])
            S.dma(lambda e: e.dma_start(out=stg[:, 0:1024], in_=w_kv_b), 'stg', w=['stg'])
            S.dve(lambda e: e.tensor_scalar(out=Wkv[:], in0=stg[:, 0:1024], scalar1=g_kv[:, 0:1], scalar2=None, op0=ALU.mult),
                  r=['stg', ('cst', 2)], w=['Wkv'])
            S.pool(lambda e: e.memset(B0[:, 9216:13824], 0.0), w=['S1', 'S2'])
            if stage >= 2:
                S.no_barrier_keys.add('cv')
                for e_ in range(32):
                    S.dma(lambda e, e_=e_: e.dma_start(out=wbf_gu[e_], in_=w_gu_p[e_ // 8][e_ % 8]), 'cv', w=[('wbf_gu', e_)], q='pool')
                    S.dma(lambda e, e_=e_: e.dma_start(out=wbf_d[e_], in_=w_d_p[e_ // 8][e_ % 8]), 'cv', w=[('wbf_d', e_)], q='pool')
            S.flush()
            if upto == 'W':
                S.enabled = False

            with ExitStack() as pb:
                uT = B0[:, 0:9216].rearrange("p (g t) -> p g t", g=4)
                S1 = B0[:, 9216:11520]
                S2 = B0[:, 11520:13824]
                xa = [B0[:, 13824:14848], B0[:, 14848:15872]]
                diffT = B1[:, 0:9216].rearrange("p (g t) -> p g t", g=4)
                hT = [B1[:, 9216:12288].rearrange("p (c t) -> p c t", c=8), B1[:, 12288:15360].rearrange("p (c t) -> p c t", c=8)]
                xb = B1[:, 15360:16384]
                qn_ext = B2[:, 14336:14336 + 4608].rearrange("p (c t) -> p c t", c=2)
                ss_b = T(pb, "ss_b", [128, 18], F32)
                sqq = T(pb, "sqq", [128, 2, 384], BF16)
                rq = T(pb, "rq", [128, 384], F32)
                ysq = T(pb, "ysq", [128, 4, 512], BF16)
                tmp16 = T(pb, "tmp16", [128, 16], F32)
                pT = [PS(pb, "pT0", [128, 8, 128], BF16), PS(pb, "pT1", [128, 8, 128], BF16)]
                pU = [PS(pb, "pU0", [128, 512], F32), PS(pb, "pU1", [128, 512], F32)]
                pQ = [PS(pb, "pQ0", [128, 512], F32), PS(pb, "pQ1", [128, 512], F32)]
                pSS = PS(pb, "pSS", [128, 512], F32)
                xbs = [xb, T(pb, "xb2", [128, 1024], BF16)[:]]

                xa = xa + [T(pb, "xa2", [128, 1024], F32)[:], T(pb, "xa3", [128, 1024], F32)[:]]

                def stats_b(tl):
                    s4 = tl % 4
                    S.dma(lambda e, tl=tl, s4=s4: e.dma_start(out=xa[s4], in_=x_own[tl * 128:(tl + 1) * 128, :]), 'xa%d' % s4, w=[('xa', s4)])
                    rms_tile(xa[s4], ('xa', s4), ss_b[:, tl:tl + 1], ('ssb', tl), None, None)

                def norm_T_b(tl):
                    sl_ = tl % 2
                    s4 = tl % 4
                    xbc = xbs[sl_]
                    S.dve(lambda e, tl=tl, s4=s4, xbc=xbc: e.tensor_scalar(out=xbc, in0=xa[s4], scalar1=ss_b[:, tl:tl + 1], scalar2=None, op0=ALU.mult),
                          r=[('xa', s4), ('ssb', tl)], w=[('xb', sl_)])
                    for c in range(8):
                        S.pe(lambda e, c=c, sl_=sl_, xbc=xbc: e.transpose(out=pT[sl_][:, c, :], in_=xbc[:, c * 128:(c + 1) * 128], identity=ident_b[:]),
                             r=[('xb', sl_), 'ident_b'], w=[('pT', sl_)])

                def super_b(st):
                    hr = [('hT', st % 2, k) for k in range(3)]
                    for g in range(4):
                        for c in range(8):
                            S.pe(lambda e, g=g, c=c, st=st: e.matmul(pU[g % 2][:, 0:384], lhsT=Win_uq[:, c, g * 128:(g + 1) * 128], rhs=hT[st % 2][:, c, :],
                                                                      start=(c == 0), stop=(c == 7)),
                                 r=hr + ['Win_uq'], w=[('pU', g % 2)])
                        if g % 2 == 0:
                            S.act(lambda e, g=g, st=st: e.copy(out=uT[:, g, st * 384:(st + 1) * 384], in_=pU[g % 2][:, 0:384]), r=[('pU', g % 2)], w=[('uT', g)])
                        else:
                            S.dve(lambda e, g=g, st=st: e.tensor_copy(out=uT[:, g, st * 384:(st + 1) * 384], in_=pU[g % 2][:, 0:384]), r=[('pU', g % 2)], w=[('uT', g)])
                    for q in range(2):
                        for c in range(8):
                            S.pe(lambda e, q=q, c=c, st=st: e.matmul(pQ[q][:, 0:384], lhsT=Win_uq[:, c, 512 + q * 128:512 + (q + 1) * 128], rhs=hT[st % 2][:, c, :],
                                                                      start=(c == 0), stop=(c == 7)),
                                 r=hr + ['Win_uq'], w=[('pQ', q)])
                        S.act(lambda e, q=q: e.activation(out=sqq[:, q, :], in_=pQ[q][:, 0:384], func=AF.Square), r=[('pQ', q)], w=[('sqq', q)])
                    for q in range(2):
                        S.pe(lambda e, q=q: e.matmul(pSS[:, 0:384], lhsT=ones_b[:], rhs=sqq[:, q, :], start=(q == 0), stop=(q == 1)),
                             r=[('sqq', q), 'ones_b'], w=['pSS'])
                    S.act(lambda e: e.activation(out=rq[:], in_=pSS[:, 0:384], func=AF.Ln, bias=epsc[:, 0:1], scale=1.0 / 256),
                          r=['pSS', 'epsc'], w=['rq'])
                    S.act(lambda e: e.activation(out=rq[:], in_=rq[:], func=AF.Exp, scale=-0.5), r=['rq'], w=['rq'])
                    for q in range(2):
                        S.dve(lambda e, q=q, st=st: e.tensor_tensor(out=qn_ext[:, q, st * 384:(st + 1) * 384], in0=pQ[q][:, 0:384], in1=rq[:], op=ALU.mult),
                              r=[('pQ', q), 'rq'], w=['qn_ext'])

                def copy_b(tl):
                    sl_ = tl % 2
                    st = tl // 3
                    k3 = tl % 3
                    S.dve(lambda e: e.tensor_copy(out=hT[st % 2][:, :, k3 * 128:(k3 + 1) * 128], in_=pT[sl_][:]),
                          r=[('pT', sl_)], w=[('hT', st % 2, k3)])
                    if k3 == 2:
                        super_b(st)

                stats_b(0)
                stats_b(1)
                for tl in range(18):
                    if tl + 2 < 18:
                        stats_b(tl + 2)
                    norm_T_b(tl)
                    if tl >= 1:
                        copy_b(tl - 1)
                copy_b(17)

                for q in range(2):
                    S.dve(lambda e, q=q: e.tensor_copy(out=qnT[:, q, :].rearrange("p (i s) -> p i s", i=16),
                                                       in_=qn_ext[:, q, :].rearrange("p (i s) -> p i s", i=16)[:, :, 16:144]),
                          r=['qn_ext'], w=['qnT'])
                N = 2304

                def shift_add(dst, src, sh, rtag, wtag):
                    S.dve(lambda e: e.tensor_tensor(out=dst[:, sh:N], in0=src[:, sh:N], in1=src[:, 0:N - sh], op=ALU.add), r=[rtag], w=[wtag])

                for g, wdw in enumerate((2, 4, 8, 16)):
                    ug = uT[:, g, :]
                    shift_add(S1, ug, 1, ('uT', g), 'S1')
                    fin, ftag = S1, 'S1'
                    if wdw >= 4:
                        shift_add(S2, S1, 2, 'S1', 'S2')
                        fin, ftag = S2, 'S2'
                    if wdw >= 8:
                        shift_add(S1, S2, 4, 'S2', 'S1')
                        fin, ftag = S1, 'S1'
                    if wdw >= 16:
                        shift_add(S2, S1, 8, 'S1', 'S2')
                        fin, ftag = S2, 'S2'
                    S.dve(lambda e, g=g, fin=fin, wdw=wdw, ug=ug: e.scalar_tensor_tensor(out=diffT[:, g, 16:N], in0=fin[:, 16:N], scalar=1.0 / wdw, in1=ug[:, 16:N],
                                                                                     op0=ALU.mult, op1=ALU.subtract),
                          r=[ftag, ('uT', g)], w=[('diffT', g)])
                    S.dve(lambda e, g=g, fin=fin: e.tensor_tensor(out=tmp16[:], in0=fin[:, 16:32], in1=invcnt[:, g * 16:(g + 1) * 16], op=ALU.mult),
                          r=[ftag, ('cst', 6)], w=['tmp16'])
                    S.dve(lambda e, g=g, ug=ug: e.tensor_tensor(out=diffT[:, g, 16:32], in0=tmp16[:], in1=ug[:, 16:32], op=ALU.subtract),
                          r=['tmp16', ('uT', g), ('diffT', g)], w=[('diffT', g)])
                wpv = wpool[:].rearrange("p (g d) -> p g d", g=4)
                for qg in range(4):
                    for g in range(4):
                        rhs = diffT[:, g, :].rearrange("p (i s) -> p i s", i=16)[:, 4 * qg:4 * qg + 4, 16:144]
                        S.pe(lambda e, g=g, rhs=rhs: e.matmul(pU[g % 2][:, 0:512].rearrange("p (a b) -> p a b", a=4), lhsT=wpv[:, g, :], rhs=rhs, start=True, stop=True),
                             r=[('diffT', g), 'wpool'], w=[('pU', g % 2)])
                        S.act(lambda e, g=g, qg=qg: e.activation(out=ypoolT[:, g, qg * 512:(qg + 1) * 512], in_=pU[g % 2][:, 0:512], func=AF.Identity,
                                                                bias=bsg[:, g:g + 1], scale=scg[:, g:g + 1]),
                              r=[('pU', g % 2), 'bsg', 'scg'], w=[('ypoolT', qg)])
                        S.act(lambda e, g=g: e.activation(out=ysq[:, g, :], in_=pU[g % 2][:, 0:512], func=AF.Square,
                                                          bias=bsc[:, g:g + 1], scale=pscale[:, g:g + 1]),
                              r=[('pU', g % 2), 'bsc', ('cst', 5)], w=[('ysq', g)])
                    for t4 in range(4):
                        i = qg * 4 + t4
                        for g in range(4):
                            S.pe(lambda e, g=g, t4=t4, i=i: e.matmul(pSS[:, i:i + 1], lhsT=ysq[:, g, t4 * 128:(t4 + 1) * 128], rhs=ones_b[:, 0:1],
                                                                     start=(g == 0), stop=(g == 3)),
                                 r=[('ysq', g), 'ones_b'], w=['pSS'])
                S.act(lambda e: e.activation(out=rstdp[:], in_=pSS[:, 0:16], func=AF.Ln, bias=epsc[:, 0:1], scale=1.0 / 512),
                      r=['pSS', 'epsc'], w=['rstdp'])
                S.act(lambda e: e.activation(out=rstdp[:], in_=rstdp[:], func=AF.Exp, scale=-0.5), r=['rstdp'], w=['rstdp'])
                if debug:
                    S.dma(lambda e: e.dma_start(out=dbg_yp, in_=B2[:, 0:8192]), 'dbg0', r=[('ypoolT', q) for q in range(4)])
                    final_keys.append('dbg0')
                S.flush()
                if upto == 'B':
                    S.enabled = False

            cnT = B1[:, 0:8192]
            KT = B1[:, 8192:16384]
            with ExitStack() as pa:
                xa = [B0[:, 0:1024], B0[:, 1024:2048]]
                posf, ang, kf, sn, cs, t1 = [B0[:, 2048 + k * 512:2048 + (k + 1) * 512] for k in range(6)]
                posi = B0i[:, 5120:5632]
                rkv = B0[:, 5632:6144]
                t2 = B0[:, 6144:6656]
                hT = [B0b[:, 14336:18432].rearrange("p (c t) -> p c t", c=8), B0b[:, 18432:22528].rearrange("p (c t) -> p c t", c=8)]
                xb = B0b[:, 22528:23552]
                sq = B0b[:, 23552:24064]
                ss_a = T(pa, "ss_a", [128, 64], F32)
                pT = [PS(pa, "pT0", [128, 8, 128], BF16), PS(pa, "pT1", [128, 8, 128], BF16)]
                pKV = PS(pa, "pKV", [128, 512], F32)
                pKR = PS(pa, "pKR", [128, 512], F32)
                pKO = PS(pa, "pKO", [128, 512], F32)
                pSS = PS(pa, "pSS", [128, 512], F32)
                sl = slice(64, 96)
                xbs = [xb, B0b[:, 24064:25088]]

                xa = xa + [B0[:, 12544:13568], B0[:, 13568:14592]]

                def stats_a(tl):
                    s4 = tl % 4
                    S.dma(lambda e, tl=tl, s4=s4: e.dma_start(out=xa[s4], in_=x_all[tl * 128:(tl + 1) * 128, :]), 'xa%d' % s4, w=[('xa', s4)])
                    rms_tile(xa[s4], ('xa', s4), ss_a[:, tl:tl + 1], ('ssa_', tl), None, None)

                def norm_T_a(tl):
                    sl_ = tl % 2
                    s4 = tl % 4
                    xbc = xbs[sl_]
                    S.dve(lambda e, tl=tl, s4=s4, xbc=xbc: e.tensor_scalar(out=xbc, in0=xa[s4], scalar1=ss_a[:, tl:tl + 1], scalar2=None, op0=ALU.mult),
                          r=[('xa', s4), ('ssa_', tl)], w=[('xb', sl_)])
                    for c in range(8):
                        S.pe(lambda e, c=c, sl_=sl_, xbc=xbc: e.transpose(out=pT[sl_][:, c, :], in_=xbc[:, c * 128:(c + 1) * 128], identity=ident_b[:]),
                             r=[('xb', sl_), 'ident_b'], w=[('pT', sl_)])

                def super_a(st):
                    hr = [('hT', st % 2, k) for k in range(4)]
                    cols = slice(st * 512, (st + 1) * 512)
                    for c in range(8):
                        S.pe(lambda e, c=c, st=st: e.matmul(pKV[:], lhsT=Wkvlat[:, c, :], rhs=hT[st % 2][:, c, :], start=(c == 0), stop=(c == 7)),
                             r=hr + ['Wkvlat'], w=['pKV'])
                    for c in range(8):
                        S.pe(lambda e, c=c, st=st: e.matmul(pKR[0:96, :], lhsT=Wkr[:, c, :], rhs=hT[st % 2][:, c, :], start=(c == 0), stop=(c == 7)),
                             r=hr + ['Wkr'], w=['pKR'])
                    for c in range(8):
                        S.pe(lambda e, c=c, st=st: e.matmul(pKO[0:96, :], lhsT=Wkrot[:, c, :], rhs=hT[st % 2][:, c, :], start=(c == 0), stop=(c == 7)),
                             r=hr + ['Wkrot'], w=['pKO'])
                    S.act(lambda e: e.activation(out=sq, in_=pKV[:], func=AF.Square), r=['pKV'], w=['sq'])
                    S.pe(lambda e: e.matmul(pSS[:], lhsT=ones_b[:], rhs=sq, start=True, stop=True), r=['sq', 'ones_b'], w=['pSS'])
                    S.act(lambda e: e.activation(out=rkv, in_=pSS[:], func=AF.Ln, bias=epsc[:, 0:1], scale=1.0 / 128),
                          r=['pSS', 'epsc'], w=['rkv'])
                    S.act(lambda e: e.activation(out=rkv, in_=rkv, func=AF.Exp, scale=-0.5), r=['rkv'], w=['rkv'])
                    S.dve(lambda e, cols=cols: e.tensor_tensor(out=cnT[:, cols], in0=pKV[:], in1=rkv, op=ALU.mult), r=['pKV', 'rkv'], w=[('cnT', st)])
                    rope_tables(st, pos_all[0:1, cols], 512, posi, posf, ang, kf, sn, cs, 'A')
                    S.dve(lambda e: e.tensor_tensor(out=t1[sl, :], in0=pKR[sl, :], in1=cs[sl, :], op=ALU.mult), r=['pKR', 'Acs'], w=['t1'])
                    S.dve(lambda e: e.tensor_tensor(out=t2[sl, :], in0=pKO[sl, :], in1=sn[sl, :], op=ALU.mult), r=['pKO', 'Asn'], w=['t2'])
                    S.dve(lambda e, cols=cols: e.tensor_tensor(out=KT[sl, cols], in0=t1[sl, :], in1=t2[sl, :], op=ALU.add), r=['t1', 't2'], w=[('KTr', st)])

                def copy_a(tl):
                    sl_ = tl % 2
                    st = tl // 4
                    k4 = tl % 4
                    S.dve(lambda e: e.tensor_copy(out=hT[st % 2][:, :, k4 * 128:(k4 + 1) * 128], in_=pT[sl_][:]),
                          r=[('pT', sl_)], w=[('hT', st % 2, k4)])
                    if k4 == 3:
                        super_a(st)

                stats_a(0)
                stats_a(1)
                for tl in range(64):
                    if tl + 2 < 64:
                        stats_a(tl + 2)
                    norm_T_a(tl)
                    if tl >= 1:
                        copy_a(tl - 1)
                copy_a(63)

                if debug:
                    S.dma(lambda e: e.dma_start(out=dbg_cn, in_=cnT), 'dbg1', r=[('cnT', s) for s in range(16)])
                    final_keys.append('dbg1')
                S.flush()
                if upto == 'A':
                    S.enabled = False

            with ExitStack() as pc:
                masks = B0b[:, 0:8192].rearrange("p (m q) -> p m q", m=16)
                V = B0b[:, 8192:8192 + 4160].rearrange("p (n d) -> p n d", n=64)
                QT = B0b[:, 12352:14400]
                PT = [B0b[:, 14400 + k * 512:14400 + (k + 1) * 512] for k in range(4)]
                cosq = B0[:, 8224:10272]
                sinq = B0[:, 10272:12320]
                Osb = B0[:, 12320:12832]
                rec = B0[:, 12832:13344]
                posf, ang, kf = [B0[:, 13344 + k * 512:13344 + (k + 1) * 512] for k in range(3)]
                posi = B0i[:, 14880:15392]
                ytmp = B0[:, 15392:15904]
                ysqa = T(pc, "ysqa", [64, 512], BF16)
                PT = PT + [T(pc, "PT4", [128, 512], BF16)[:], T(pc, "PT5", [128, 512], BF16)[:]]
                dh = T(pc, "dh", [128, 512], BF16)
                dl = T(pc, "dl", [128, 512], BF16)
                ps = [PS(pc, "ps%d" % k, [128, 512], F32) for k in range(8)]
                S.dma(lambda e: e.dma_start(out=B0b[:, 0:8192], in_=masks_d), 'masks', w=['masks'])
                S.pool(lambda e: e.memset(V[:, :, 64:65], 1.0), w=['Vones'])
                for ch in range(4):
                    cc = slice(ch * 512, (ch + 1) * 512)
                    rope_tables(ch, pos_own[0:1, cc], 512, posi, posf, ang, kf, sinq[:, cc], cosq[:, cc], 'C', 'C%d' % ch)
                tabr = ['C%dsn' % ch for ch in range(4)] + ['C%dcs' % ch for ch in range(4)]
                scale = float(96 ** -0.5)
                sl = slice(64, 96)
                t1, t2 = posf, ang
                cnt = 0
                for h in range(8):
                    for st in range(16):
                        cols = slice(st * 512, (st + 1) * 512)
                        S.pe(lambda e, h=h, st=st, cols=cols: e.matmul(ps[st % 2][0:64, :], lhsT=Wkv[:, h * 128:h * 128 + 64], rhs=cnT[:, cols], start=True, stop=True),
                             r=['Wkv', ('cnT', st)], w=[('ps', st % 2)])
                        if st % 2 == 0:
                            S.act(lambda e, st=st, cols=cols: e.copy(out=KT[0:64, cols], in_=ps[st % 2][0:64, :]), r=[('ps', st % 2)], w=[('KTn', st)])
                        else:
                            S.dve(lambda e, st=st, cols=cols: e.tensor_copy(out=KT[0:64, cols], in_=ps[st % 2][0:64, :]), r=[('ps', st % 2)], w=[('KTn', st)])
                    for n8 in range(8):
                        for k in range(8):
                            n = n8 * 8 + k
                            S.pe(lambda e, h=h, n=n, k=k, n8=n8: e.matmul(ps[n8 % 2][:, k * 64:(k + 1) * 64], lhsT=cnT[:, n * 128:(n + 1) * 128],
                                                                           rhs=Wkv[:, h * 128 + 64:h * 128 + 128], start=True, stop=True),
                                 r=['Wkv', ('cnT', n // 4)], w=[('ps', n8 % 2)])
                        if n8 % 2 == 0:
                            S.dve(lambda e, n8=n8: e.tensor_copy(out=V[:, n8 * 8:(n8 + 1) * 8, 0:64], in_=ps[n8 % 2][:].rearrange("p (a b) -> p a b", a=8)),
                                  r=[('ps', n8 % 2)], w=[('V', n8)])
                        else:
                            S.act(lambda e, n8=n8: e.copy(out=V[:, n8 * 8:(n8 + 1) * 8, 0:64], in_=ps[n8 % 2][:].rearrange("p (a b) -> p a b", a=8)),
                                  r=[('ps', n8 % 2)], w=[('V', n8)])
                    for qg in range(4):
                        cols = slice(qg * 512, (qg + 1) * 512)
                        for c in range(2):
                            S.pe(lambda e, h=h, c=c, cols=cols: e.matmul(ps[0][0:96, :], lhsT=Wq[:, c, h * 96:(h + 1) * 96], rhs=qnT[:, c, cols], start=(c == 0), stop=(c == 1)),
                                 r=['Wq', 'qnT'], w=[('ps', 0)])
                        for c in range(2):
                            S.pe(lambda e, h=h, c=c, cols=cols: e.matmul(ps[1][0:96, :], lhsT=Wqrot[:, c, h * 96:(h + 1) * 96], rhs=qnT[:, c, cols], start=(c == 0), stop=(c == 1)),
                                 r=['Wqrot', 'qnT'], w=[('ps', 1)])
                        S.act(lambda e, cols=cols: e.copy(out=QT[0:64, cols], in_=ps[0][0:64, :]), r=[('ps', 0)], w=[('QT', qg)])
                        S.dve(lambda e, cols=cols: e.tensor_tensor(out=t1[sl, :], in0=ps[0][sl, :], in1=cosq[sl, cols], op=ALU.mult), r=[('ps', 0)] + tabr, w=['t1'])
                        S.dve(lambda e, cols=cols: e.tensor_tensor(out=t2[sl, :], in0=ps[1][sl, :], in1=sinq[sl, cols], op=ALU.mult), r=[('ps', 1)] + tabr, w=['t2'])
                        S.dve(lambda e, cols=cols: e.tensor_tensor(out=QT[sl, cols], in0=t1[sl, :], in1=t2[sl, :], op=ALU.add), r=['t1', 't2', ('QT', qg)], w=[('QT', qg)])
                    for qg in range(4):
                        qc0 = qg * 512
                        qcols = slice(qg * 512, (qg + 1) * 512)
                        nkb = 16 * qg + 16
                        base = cnt
                        order = [16 * qg] + [16 * qg + m for m in range(4, 16)] + list(range(16 * qg)) + [16 * qg + 1, 16 * qg + 2, 16 * qg + 3]
                        LA = 4

                        def c0_of(kb, qg=qg):
                            m = kb - 16 * qg
                            return 0 if m < 0 else (m // 4) * 128

                        def qk(ui, base=base, qc0=qc0, qg=qg, order=order):
                            kb = order[ui]
                            c0 = c0_of(kb)
                            b_ = (base + ui) % 5
                            S.pe(lambda e, kb=kb, b_=b_, c0=c0: e.matmul(ps[b_][:, c0:512], lhsT=KT[0:96, kb * 128:(kb + 1) * 128], rhs=QT[0:96, qc0 + c0:qc0 + 512],
                                                                          start=True, stop=True),
                                 r=[('KTn', kb // 4), ('KTr', kb // 4), ('QT', qg)], w=[('ps', b_)])

                        def pv(ui, base=base, qg=qg, nkb=nkb, order=order):
                            kb = order[ui]
                            c0 = c0_of(kb)
                            b_ = (base + ui) % 5
                            pt = (base + ui) % 6
                            S.act(lambda e, b_=b_, pt=pt, c0=c0: e.activation(out=PT[pt][:, c0:512], in_=ps[b_][:, c0:512], func=AF.Exp, scale=scale), r=[('ps', b_)], w=[('PT', pt)])
                            if kb >= 16 * qg:
                                m = kb - 16 * qg
                                S.dve(lambda e, pt=pt, m=m, c0=c0: e.tensor_tensor(out=PT[pt][:, c0:512], in0=PT[pt][:, c0:512], in1=masks[:, m, c0:512], op=ALU.mult),
                                      r=[('PT', pt), 'masks'], w=[('PT', pt)])
                            S.pe(lambda e, kb=kb, pt=pt, c0=c0, ui=ui: e.matmul(ps[5][0:65, c0:512], lhsT=V[:, kb, 0:65], rhs=PT[pt][:, c0:512], start=(ui == 0), stop=(ui == nkb - 1)),
                                 r=[('V', kb // 8), 'Vones', ('PT', pt)], w=[('ps', 5)])

                        for ui in range(min(LA, nkb)):
                            qk(ui)
                        for ui in range(nkb):
                            pv(ui)
                            if ui + LA < nkb:
                                qk(ui + LA)
                        cnt += nkb
                        S.act(lambda e: e.copy(out=Osb[0:65, :], in_=ps[5][0:65, :]), r=[('ps', 5)], w=['Osb'])
                        S.dve(lambda e: e.tensor_copy(out=dh[64:65, :], in_=Osb[64:65, :]), r=['Osb'], w=['dh'])
                        S.dve(lambda e: e.tensor_tensor(out=dl[64:65, :], in0=Osb[64:65, :], in1=dh[64:65, :], op=ALU.subtract), r=['Osb', 'dh'], w=['dl'])
                        S.pe(lambda e: e.matmul(ps[6][0:64, :], lhsT=ones_b[64:65, 0:64], rhs=dh[64:65, :], start=True, stop=False), r=['dh', 'ones_b'], w=[('ps', 6)])
                        S.pe(lambda e: e.matmul(ps[6][0:64, :], lhsT=ones_b[64:65, 0:64], rhs=dl[64:65, :], start=False, stop=True), r=['dl', 'ones_b'], w=[('ps', 6)])
                        S.dve(lambda e: e.reciprocal(out=rec[0:64, :], in_=ps[6][0:64, :]), r=[('ps', 6)], w=['rec'])
                        S.dve(lambda e: e.tensor_tensor(out=ytmp[0:64, :], in0=Osb[0:64, :], in1=rec[0:64, :], op=ALU.mult), r=['Osb', 'rec'], w=['ytmp'])
                        S.dve(lambda e, h=h, qcols=qcols: e.tensor_scalar(out=yattnT[0:64, h, qcols], in0=ytmp[0:64, :], scalar1=g_oa[:, h:h + 1], scalar2=None, op0=ALU.mult),
                              r=['ytmp', 'g_oa'], w=[('yattnT', h, qg)])
                        S.act(lambda e: e.activation(out=ysqa[:], in_=ytmp[0:64, :], func=AF.Square), r=['ytmp'], w=['ysqa'])
                        for t4 in range(4):
                            S.pe(lambda e, t4=t4: e.matmul(ps[7][:, t4:t4 + 1], lhsT=ysqa[:, t4 * 128:(t4 + 1) * 128], rhs=ones_b[0:64, 0:1], start=True, stop=True),
                                 r=['ysqa', 'ones_b'], w=[('ps', 7)])
                        if h == 0:
                            S.dve(lambda e, qg=qg: e.tensor_copy(out=ssa[:, qg * 4:(qg + 1) * 4], in_=ps[7][:, 0:4]), r=[('ps', 7)], w=[('ssa', qg)])
                        else:
                            S.dve(lambda e, qg=qg: e.tensor_tensor(out=ssa[:, qg * 4:(qg + 1) * 4], in0=ssa[:, qg * 4:(qg + 1) * 4], in1=ps[7][:, 0:4], op=ALU.add),
                                  r=[('ps', 7), ('ssa', qg)], w=[('ssa', qg)])
                S.act(lambda e: e.activation(out=rstda[:], in_=ssa[:], func=AF.Ln, bias=epsc[:, 0:1], scale=1.0 / 512),
                      r=[('ssa', q) for q in range(4)] + ['epsc'], w=['rstda'])
                S.act(lambda e: e.activation(out=rstda[:], in_=rstda[:], func=AF.Exp, scale=-0.5), r=['rstda'], w=['rstda'])
                if debug:
                    S.dma(lambda e: e.dma_start(out=dbg_ya, in_=B2[0:64, 8192:24576]), 'dbg2', r=[('yattnT', h, q) for h in range(8) for q in range(4)])
                    S.dma(lambda e: e.dma_start(out=dbg_kt, in_=KT[0:96, :]), 'dbg3', r=[('KTn', s) for s in range(16)] + [('KTr', s) for s in range(16)])
                    S.dma(lambda e: e.dma_start(out=dbg_rs[:, 0:16], in_=rstdp[:]), 'dbg4', r=['rstdp'])
                    S.dma(lambda e: e.dma_start(out=dbg_rs[:, 16:32], in_=rstda[:]), 'dbg4', r=['rstda'])
                    final_keys.extend(['dbg2', 'dbg3', 'dbg4'])
                S.flush()
                if upto == 'C':
                    S.enabled = False

        xn = B0.rearrange("p (i d) -> p i d", i=16)
        h2T = B1.rearrange("p (c t) -> p c t", c=8)

        with ExitStack() as pd:
            Wop = T(pd, "Wop", [128, 4, 1024], BF16)
            Woa = T(pd, "Woa", [64, 8, 1024], BF16)
            xo = [T(pd, "xo0", [128, 1024], F32), T(pd, "xo1", [128, 1024], F32)]
            pP = [PS(pd, "pP0", [128, 512], F32), PS(pd, "pP1", [128, 512], F32)]
            pA = [PS(pd, "pA0", [128, 512], F32), PS(pd, "pA1", [128, 512], F32)]
            S.dma(lambda e: e.dma_start(out=Wop[:], in_=w_out[0:512, :].rearrange("(g p) n -> p g n", p=128)), 'wop', w=['Wop'], q='pool')
            S.dma(lambda e: e.dma_start(out=Woa[:], in_=w_out[512:1024, :].rearrange("(h p) n -> p h n", p=64)), 'woa', w=['Woa'], q='pool')
            for i in range(16):
                tcols = slice(i * 128, (i + 1) * 128)
                S.dma(lambda e, i=i: e.dma_start(out=xo[i % 2][:], in_=x_own[144 * i + 16:144 * i + 144, :]), 'xo%d' % (i % 2), w=[('xo', i % 2)])
                for half in range(2):
                    hc = slice(half * 512, (half + 1) * 512)
                    for g in range(4):
                        S.pe(lambda e, g=g, half=half, tcols=tcols, hc=hc: e.matmul(pP[half][:], lhsT=ypoolT[:, g, tcols], rhs=Wop[:, g, hc], start=(g == 0), stop=(g == 3)),
                             r=['Wop'], w=[('pP', half)])
                    for h in range(8):
                        S.pe(lambda e, h=h, half=half, tcols=tcols, hc=hc: e.matmul(pA[half][:], lhsT=yattnT[0:64, h, tcols], rhs=Woa[:, h, hc], start=(h == 0), stop=(h == 7)),
                             r=['Woa'], w=[('pA', half)])
                    S.dve(lambda e, i=i, half=half, hc=hc: e.scalar_tensor_tensor(out=xn[:, i, hc], in0=pP[half][:], scalar=rstdp[:, i:i + 1], in1=xo[i % 2][:, hc],
                                                                                 op0=ALU.mult, op1=ALU.add),
                          r=[('pP', half), ('xo', i % 2)], w=[('xn', i, half)])
                    S.dve(lambda e, i=i, half=half, hc=hc: e.scalar_tensor_tensor(out=xn[:, i, hc], in0=pA[half][:], scalar=rstda[:, i:i + 1], in1=xn[:, i, hc],
                                                                                 op0=ALU.mult, op1=ALU.add),
                          r=[('pA', half), ('xn', i, half)], w=[('xn', i, half)])
                if debug:
                    S.dma(lambda e, i=i: e.dma_start(out=dbg_x1[i * 128:(i + 1) * 128, :], in_=xn[:, i, :]), 'dbg5', r=[('xn', i, 0), ('xn', i, 1)])
            if debug:
                final_keys.append('dbg5')
            S.flush()
            if upto == 'D':
                S.enabled = False

        Wgu = [B1.rearrange("p (c n) -> p c n", c=8), B2[:, 0:16384].rearrange("p (c n) -> p c n", c=8)]
        Wd = B2[:, 16384:24576].rearrange("p (c n) -> p c n", c=8)

        def load_gu(e_):
            S.dma(lambda e, e_=e_: e.dma_start(out=Wgu[e_ % 2], in_=wbf_gu[e_].rearrange("(c p) n -> p c n", p=128)),
                  'wgu%d' % (e_ % 2), r=[('wbf_gu', e_)], w=[('Wgu', e_ % 2)])

        def load_d(e_):
            S.dma(lambda e, e_=e_: e.dma_start(out=Wd, in_=wbf_d[e_].rearrange("(c p) n -> p c n", p=128)), 'wd', r=[('wbf_d', e_)], w=['Wd'])

        S.no_barrier_keys.update(['wgu0', 'wgu1', 'wd'])

        with ExitStack() as pe_:
            gffn = T(pe_, "gffn", [128, 1024], F32)
            wrf = T(pe_, "wrf", [128, 8, 32], F32)
            brb = T(pe_, "brb", [128, 32], F32)
            h2tok = T(pe_, "h2tok", [128, 1024], F32)
            hib = T(pe_, "hib", [128, 1024], BF16)
            lob = T(pe_, "lob", [128, 1024], BF16)
            loT = T(pe_, "loT", [128, 8, 128], BF16)
            whi = T(pe_, "whi", [128, 8, 32], BF16)
            wlo = T(pe_, "wlo", [128, 8, 32], BF16)
            ss2 = T(pe_, "ss2", [128, 16], F32)
            lg = T(pe_, "lg", [128, 32], F32)
            top8 = T(pe_, "top8", [128, 8], F32)
            msk = T(pe_, "msk", [128, 32], F32)
            ex = T(pe_, "ex", [128, 32], F32)
            nm = T(pe_, "nm", [128, 1], F32)
            den = T(pe_, "den", [128, 1], F32)
            ptr = [PS(pe_, "ptr0", [128, 8, 128], BF16), PS(pe_, "ptr1", [128, 8, 128], BF16)]
            plg = PS(pe_, "plg", [128, 512], F32)
            plb = [PS(pe_, "plb0", [128, 512], F32), PS(pe_, "plb1", [128, 512], F32)]
            bdb = T(pe_, "bdb", [32, 1024], BF16)
            gb = T(pe_, "gb", [128, 32], BF16)
            gT = T(pe_, "gT", [32, 128], BF16)
            hiT = T(pe_, "hiT", [128, 8, 128], BF16)
            hibs = [hib, T(pe_, "hib1", [128, 1024], BF16)]
            mb = T(pe_, "mb", [128, 32], BF16)
            dtab = T(pe_, "dtab", [128, 32], F32)
            ov = T(pe_, "ov", [128, 32], F32)
            oh = T(pe_, "oh", [128, 32], F32)
            tmp32 = T(pe_, "tmp32", [128, 32], F32)
            destf = T(pe_, "destf", [128, 4], F32)
            okf = T(pe_, "okf", [128, 4], F32)
            ppx = PS(pe_, "ppx", [128, 512], F32)
            S.dma(lambda e: e.dma_start(out=bdb[:], in_=b_d_d), 'e3', w=['bdb'], q='pool')
            if stage >= 2:
                load_gu(0)
                load_gu(1)
                load_d(0)
            S.dma(lambda e: e.dma_start(out=gffn[:], in_=g_ffn_d.partition_broadcast(128)), 'e0', w=['gffn'])
            S.dma(lambda e: e.dma_start(out=wrf[:], in_=w_r.rearrange("(c p) n -> p c n", p=128)), 'e1', w=['wrf'])
            S.dma(lambda e: e.dma_start(out=brb[:], in_=b_r.partition_broadcast(128)), 'e2', w=['brb'])
            S.dve(lambda e: e.tensor_copy(out=whi[:], in_=wrf[:]), r=['wrf'], w=['whi'])
            S.dve(lambda e: e.tensor_tensor(out=wlo[:], in0=wrf[:], in1=whi[:], op=ALU.subtract), r=['wrf', 'whi'], w=['wlo'])
            for i in range(16):
                S.act(lambda e, i=i: e.activation(out=junk[:], in_=xn[:, i, :], func=AF.Square, accum_out=ss2[:, i:i + 1]),
                      r=[('xn', i, 0), ('xn', i, 1)], w=['junk', ('ss2q', i)])
            S.act(lambda e: e.activation(out=ss2[:], in_=ss2[:], func=AF.Ln, bias=epsc[:, 0:1], scale=1.0 / 1024),
                  r=[('ss2q', i) for i in range(16)] + ['epsc'], w=['ss2all'])
            S.act(lambda e: e.activation(out=ss2[:], in_=ss2[:], func=AF.Exp, scale=-0.5), r=['ss2all'], w=['ss2all'])
            for i in range(16):
                xt = [('xn', i, 0), ('xn', i, 1)]
                tc_ = slice(i * 128, (i + 1) * 128)
                S.dve(lambda e, i=i: e.scalar_tensor_tensor(out=h2tok[:], in0=xn[:, i, :], scalar=ss2[:, i:i + 1], in1=gffn[:], op0=ALU.mult, op1=ALU.mult),
                      r=xt + ['ss2all', 'gffn'], w=['h2tok'])
                hib = hibs[i % 2]
                hbt = ('hib', i % 2)
                S.dve(lambda e, hib=hib: e.tensor_copy(out=hib[:], in_=h2tok[:]), r=['h2tok'], w=[hbt])
                S.dve(lambda e, hib=hib: e.tensor_tensor(out=lob[:], in0=h2tok[:], in1=hib[:], op=ALU.subtract), r=['h2tok', hbt], w=['lob'])
                for c in range(8):
                    S.pe(lambda e, c=c, hib=hib: e.transpose(out=ptr[0][:, c, :], in_=hib[:, c * 128:(c + 1) * 128], identity=ident_b[:]), r=[hbt, 'ident_b'], w=[('ptr', 0)])
                for c in range(8):
                    S.pe(lambda e, c=c: e.transpose(out=ptr[1][:, c, :], in_=lob[:, c * 128:(c + 1) * 128], identity=ident_b[:]), r=['lob', 'ident_b'], w=[('ptr', 1)])
                S.act(lambda e: e.copy(out=hiT[:], in_=ptr[0][:]), r=[('ptr', 0)], w=['hiT'])
                S.dve(lambda e: e.tensor_copy(out=loT[:], in_=ptr[1][:]), r=[('ptr', 1)], w=['loT'])
                k = 0
                for c in range(8):
                    for (lt, ltag, rt, rtag) in ((hiT[:, c, :], 'hiT', whi, 'whi'), (hiT[:, c, :], 'hiT', wlo, 'wlo'), (loT[:, c, :], 'loT', whi, 'whi')):
                        S.pe(lambda e, lt=lt, rt=rt, c=c, k=k: e.matmul(plg[:, 0:32], lhsT=lt, rhs=rt[:, c, :], start=(k == 0), stop=(k == 23)),
                             r=[ltag, rtag], w=['plg'])
                        k += 1
                S.dve(lambda e: e.tensor_tensor(out=lg[:], in0=plg[:, 0:32], in1=brb[:], op=ALU.add), r=['plg', 'brb'], w=['lg'])
                S.dve(lambda e: e.max(out=top8[:], in_=lg[:]), r=['lg'], w=['top8'])
                S.dve(lambda e: e.tensor_scalar(out=msk[:], in0=lg[:], scalar1=top8[:, 3:4], scalar2=None, op0=ALU.is_ge), r=['lg', 'top8'], w=['msk'])
                S.dve(lambda e: e.tensor_scalar(out=nm[:], in0=top8[:, 0:1], scalar1=-1.0, scalar2=None, op0=ALU.mult), r=['top8'], w=['nm'])
                S.act(lambda e: e.activation(out=ex[:], in_=lg[:], func=AF.Exp, bias=nm[:, 0:1], scale=1.0), r=['lg', 'nm'], w=['ex'])
                S.dve(lambda e: e.tensor_tensor(out=ex[:], in0=ex[:], in1=msk[:], op=ALU.mult), r=['ex', 'msk'], w=['ex'])
                S.dve(lambda e: e.reduce_sum(out=den[:], in_=ex[:], axis=mybir.AxisListType.X), r=['ex'], w=['den'])
                S.dve(lambda e: e.reciprocal(out=den[:], in_=den[:]), r=['den'], w=['den'])
                S.dve(lambda e, i=i: e.tensor_scalar(out=gates[:, i * 32:(i + 1) * 32], in0=ex[:], scalar1=den[:, 0:1], scalar2=None, op0=ALU.mult),
                      r=['ex', 'den'], w=[('gates', i)])
                if stage >= 2:
                    S.dve(lambda e: e.tensor_copy(out=mb[:], in_=msk[:]), r=['msk'], w=['mb'])
                    S.pe(lambda e: e.matmul(ppx[:, 0:32], lhsT=Lst[:], rhs=mb[:], start=True, stop=True), r=['Lst', 'mb'], w=['ppx'])
                    S.pe(lambda e: e.matmul(ppx[:, 32:64], lhsT=ones_b[:], rhs=mb[:], start=True, stop=True), r=['ones_b', 'mb'], w=['ppx'])
                    S.dve(lambda e: e.tensor_tensor(out=dtab[:], in0=ppx[:, 0:32], in1=carry[:], op=ALU.add), r=['ppx', 'carry'], w=['dtab'])
                    S.dve(lambda e: e.tensor_tensor(out=carry[:], in0=carry[:], in1=ppx[:, 32:64], op=ALU.add), r=['ppx', 'carry'], w=['carry'])
                    S.dve(lambda e: e.tensor_scalar(out=ov[:], in0=dtab[:], scalar1=float(CAP), scalar2=BIGV, op0=ALU.is_ge, op1=ALU.mult), r=['dtab'], w=['ov'])
                    S.dve(lambda e: e.tensor_tensor(out=dtab[:], in0=dtab[:], in1=CE[:], op=ALU.add), r=['dtab', 'CE'], w=['dtab'])
                    S.dve(lambda e: e.tensor_tensor(out=dtab[:], in0=dtab[:], in1=ov[:], op=ALU.add), r=['dtab', 'ov'], w=['dtab'])
                    for k in range(4):
                        S.dve(lambda e, k=k: e.tensor_scalar(out=oh[:], in0=lg[:], scalar1=top8[:, k:k + 1], scalar2=None, op0=ALU.is_equal), r=['lg', 'top8'], w=['oh'])
                        S.dve(lambda e, k=k: e.scalar_tensor_tensor(out=tmp32[:], in0=oh[:], scalar=1.0, in1=dtab[:], op0=ALU.mult, op1=ALU.mult, accum_out=destf[:, k:k + 1]),
                              r=['oh', 'dtab'], w=['tmp32', 'destf'])
                        S.dve(lambda e, k=k, i=i: e.scalar_tensor_tensor(out=tmp32[:], in0=oh[:], scalar=1.0, in1=gates[:, i * 32:(i + 1) * 32], op0=ALU.mult, op1=ALU.mult,
                                                                       accum_out=gk[:, 4 * i + k:4 * i + k + 1]),
                              r=['oh', ('gates', i), 'gk0'], w=['tmp32', ('gk', i)])
                    S.dve(lambda e, i=i: e.tensor_copy(out=desti[:, 4 * i:4 * i + 4], in_=destf[:]), r=['destf'], w=[('desti', i)])
                    S.dve(lambda e: e.tensor_scalar(out=okf[:], in0=destf[:], scalar1=float(NROWS), scalar2=None, op0=ALU.is_lt), r=['destf'], w=['okf'])
                    S.dve(lambda e, i=i: e.tensor_tensor(out=gk[:, 4 * i:4 * i + 4], in0=gk[:, 4 * i:4 * i + 4], in1=okf[:], op=ALU.mult), r=['okf', ('gk', i)], w=[('gk', i)])
                    for k in range(4):
                        S.dma(lambda e, k=k, i=i, hib=hib: e.indirect_dma_start(
                            out=xs[:, :], out_offset=bass.IndirectOffsetOnAxis(ap=desti[:, 4 * i + k:4 * i + k + 1], axis=0),
                            in_=hib[:, :], in_offset=None, bounds_check=S.reg(e, NROWS - 1), oob_is_err=False),
                            'sc%d' % (i % 2), r=[hbt, ('desti', i)], q='pool')
                S.dve(lambda e, i=i: e.tensor_copy(out=gb[:], in_=gates[:, i * 32:(i + 1) * 32]), r=[('gates', i)], w=['gb'])
                S.pe(lambda e: e.transpose(out=ptr[1][0:32, 0, :], in_=gb[:], identity=ident_b[:]), r=['gb', 'ident_b'], w=[('ptr', 1)])
                S.act(lambda e: e.copy(out=gT[:], in_=ptr[1][0:32, 0, :]), r=[('ptr', 1)], w=['gT'])
                for half in range(2):
                    hc = slice(half * 512, (half + 1) * 512)
                    S.pe(lambda e, half=half, hc=hc: e.matmul(plb[half][:], lhsT=gT[:], rhs=bdb[:, hc], start=True, stop=True), r=['gT', 'bdb'], w=[('plb', half)])
                    S.dve(lambda e, i=i, half=half, hc=hc: e.tensor_tensor(out=xn[:, i, hc], in0=xn[:, i, hc], in1=plb[half][:], op=ALU.add),
                          r=[('plb', half), ('xn', i, half)], w=[('xn', i, half)])
            if debug:
                S.dma(lambda e: e.dma_start(out=dbg_gates, in_=gates[:]), 'dbg6', r=[('gates', i) for i in range(16)])
                final_keys.append('dbg6')
            S.flush()
            if upto == 'E':
                S.enabled = False

        if stage >= 2:
            with ExitStack() as pf:
                KC = CAP // 128
                bgu = T(pf, "bgu", [128, 512], F32)
                xg = [T(pf, "xg0", [128, KC, 1024], BF16), T(pf, "xg1", [128, KC, 1024], BF16)]
                selT = [T(pf, "selT0", [128, 8, CAP], BF16), T(pf, "selT1", [128, 8, CAP], BF16)]
                actT = T(pf, "actT", [128, 8, CAP], BF16)
                yo = T(pf, "yo", [128, KC, 1024], F32)
                g1 = T(pf, "g1", [128, CAP], F32)
                sg = T(pf, "sg", [128, CAP], F32)
                ur = T(pf, "ur", [128, CAP], F32)
                tt = T(pf, "tt", [128, CAP], F32)
                pTx = [PS(pf, "pTx0", [128, 8, 128], BF16), PS(pf, "pTx1", [128, 8, 128], BF16)]
                pG = [PS(pf, "pG0", [128, 512], F32), PS(pf, "pG1", [128, 512], F32)]
                pUp = [PS(pf, "pUp0", [128, 512], F32), PS(pf, "pUp1", [128, 512], F32)]
                pY = [PS(pf, "pY0", [128, 512], F32), PS(pf, "pY1", [128, 512], F32)]
                S.dma(lambda e: e.dma_start(out=bgu[:], in_=b_gu_d), 'f0', w=['bgu'])

                def load_x(e_):
                    S.dma(lambda e, e_=e_: e.dma_start(out=xg[e_ % 2][:], in_=xs[e_ * CAP:(e_ + 1) * CAP, :].rearrange("(k p) d -> p k d", p=128)),
                          'xg%d' % (e_ % 2), w=[('xg', e_ % 2)])

                load_x(0)
                blk = 0
                for ex_ in range(32):
                    sl_ = ex_ % 2
                    if ex_ + 1 < 32:
                        load_x(ex_ + 1)
                    for k in range(KC):
                        for c in range(8):
                            S.pe(lambda e, k=k, c=c, sl_=sl_: e.transpose(out=pTx[k % 2][:, c, :], in_=xg[sl_][:, k, c * 128:(c + 1) * 128], identity=ident_b[:]),
                                 r=[('xg', sl_), 'ident_b'], w=[('pTx', k % 2)])
                        if k % 2 == 0:
                            S.act(lambda e, k=k, sl_=sl_: e.copy(out=selT[sl_][:, :, k * 128:(k + 1) * 128], in_=pTx[k % 2][:]), r=[('pTx', k % 2)], w=[('selT', sl_, k)])
                        else:
                            S.dve(lambda e, k=k, sl_=sl_: e.tensor_copy(out=selT[sl_][:, :, k * 128:(k + 1) * 128], in_=pTx[k % 2][:]), r=[('pTx', k % 2)], w=[('selT', sl_, k)])
                    sr = [('selT', sl_, k) for k in range(KC)]
                    W = Wgu[sl_]
                    for fc in range(8):
                        pb_ = blk % 2
                        blk += 1
                        for c in range(8):
                            S.pe(lambda e, fc=fc, c=c, pb_=pb_, W=W, sl_=sl_: e.matmul(pG[pb_][:, 0:CAP], lhsT=W[:, c, fc * 128:(fc + 1) * 128], rhs=selT[sl_][:, c, :],
                                                                                 start=(c == 0), stop=(c == 7)),
                                 r=sr + [('Wgu', sl_)], w=[('pG', pb_)])
                        for c in range(8):
                            S.pe(lambda e, fc=fc, c=c, pb_=pb_, W=W, sl_=sl_: e.matmul(pUp[pb_][:, 0:CAP], lhsT=W[:, c, 1024 + fc * 128:1024 + (fc + 1) * 128], rhs=selT[sl_][:, c, :],
                                                                                 start=(c == 0), stop=(c == 7)),
                                 r=sr + [('Wgu', sl_)], w=[('pUp', pb_)])
                        bg = bgu[:, ex_ * 16 + fc:ex_ * 16 + fc + 1]
                        bu = bgu[:, ex_ * 16 + 8 + fc:ex_ * 16 + 8 + fc + 1]
                        S.dve(lambda e, pb_=pb_, bg=bg: e.tensor_scalar(out=g1[:], in0=pG[pb_][:, 0:CAP], scalar1=bg, scalar2=7.0, op0=ALU.add, op1=ALU.min),
                              r=[('pG', pb_), 'bgu'], w=['g1'])
                        S.act(lambda e: e.activation(out=sg[:], in_=g1[:], func=AF.Sigmoid, scale=1.702), r=['g1'], w=['sg'])
                        S.act(lambda e, pb_=pb_, bu=bu: e.activation(out=ur[:], in_=pUp[pb_][:, 0:CAP], func=AF.Identity, bias=bu, scale=1.0),
                              r=[('pUp', pb_), 'bgu'], w=['ur'])
                        S.dve(lambda e: e.tensor_scalar(out=ur[:], in0=ur[:], scalar1=7.0, scalar2=-7.0, op0=ALU.min, op1=ALU.max), r=['ur'], w=['ur'])
                        S.dve(lambda e: e.tensor_tensor(out=tt[:], in0=g1[:], in1=sg[:], op=ALU.mult), r=['g1', 'sg'], w=['tt'])
                        S.dve(lambda e, fc=fc: e.scalar_tensor_tensor(out=actT[:, fc, :], in0=ur[:], scalar=1.0, in1=tt[:], op0=ALU.add, op1=ALU.mult),
                              r=['ur', 'tt'], w=[('actT', fc)])
                    if ex_ + 2 < 32:
                        load_gu(ex_ + 2)
                    for k in range(KC):
                        for half in range(2):
                            hc = slice(half * 512, (half + 1) * 512)
                            for fc in range(8):
                                S.pe(lambda e, fc=fc, k=k, half=half, hc=hc: e.matmul(pY[half][:], lhsT=actT[:, fc, k * 128:(k + 1) * 128], rhs=Wd[:, fc, hc],
                                                                                 start=(fc == 0), stop=(fc == 7)),
                                     r=[('actT', fc), 'Wd'], w=[('pY', half)])
                            if half == 0:
                                S.act(lambda e, k=k, half=half, hc=hc: e.copy(out=yo[:, k, hc], in_=pY[half][:]), r=[('pY', half)], w=[('yo', k, half)])
                            else:
                                S.dve(lambda e, k=k, half=half, hc=hc: e.tensor_copy(out=yo[:, k, hc], in_=pY[half][:]), r=[('pY', half)], w=[('yo', k, half)])
                    if ex_ + 1 < 32:
                        load_d(ex_ + 1)
                    S.dma(lambda e, ex_=ex_: e.dma_start(out=ys[ex_ * CAP:(ex_ + 1) * CAP, :].rearrange("(k p) d -> p k d", p=128), in_=yo[:]),
                          'ys', r=[('yo', k, h_) for k in range(KC) for h_ in range(2)])
                S.flush()
                if upto == 'F':
                    S.enabled = False
            with ExitStack() as pf2:
                NG = 8
                gbf = BIG2[:].bitcast(F32)
                gbuf = [gbf[:, k * 1024:(k + 1) * 1024] for k in range(NG)]
                S.pool(lambda e: e.memset(gbf[:, 0:NG * 1024], 0.0), w=[('gbuf', k) for k in range(NG)])
                for i in range(16):
                    for k in range(4):
                        j = 4 * i + k
                        gs = j % NG
                        S.dma(lambda e, j=j, gs=gs: e.indirect_dma_start(
                            out=gbuf[gs], out_offset=None, in_=ys[:, :],
                            in_offset=bass.IndirectOffsetOnAxis(ap=desti[:, j:j + 1], axis=0), bounds_check=S.reg(e, NROWS - 1), oob_is_err=False),
                            'ga%d' % gs, w=[('gbuf', gs)], q='pool')
                        S.dve(lambda e, i=i, j=j, gs=gs: e.scalar_tensor_tensor(out=xn[:, i, :], in0=gbuf[gs], scalar=gk[:, j:j + 1], in1=xn[:, i, :], op0=ALU.mult, op1=ALU.add),
                              r=[('gbuf', gs), ('xn', i, 0), ('xn', i, 1)], w=[('xn', i, 0), ('xn', i, 1)])
                S.flush()

        S.enabled = True
        with ExitStack() as pg:
            gfin = T(pg, "gfin", [128, 1024], F32)
            ot = [T(pg, "ot0", [128, 1024], F32), T(pg, "ot1", [128, 1024], F32)]
            ss3 = T(pg, "ss3", [128, 16], F32)
            S.dma(lambda e: e.dma_start(out=gfin[:], in_=g_fin_d.partition_broadcast(128)), 'g0', w=['gfin'])
            for i in range(16):
                S.act(lambda e, i=i: e.activation(out=junk[:], in_=xn[:, i, :], func=AF.Square, accum_out=ss3[:, i:i + 1]),
                      r=[('xn', i, 0), ('xn', i, 1)], w=['junk', ('ss3q', i)])
            S.act(lambda e: e.activation(out=ss3[:], in_=ss3[:], func=AF.Ln, bias=epsc[:, 0:1], scale=1.0 / 1024),
                  r=[('ss3q', i) for i in range(16)] + ['epsc'], w=['ss3all'])
            S.act(lambda e: e.activation(out=ss3[:], in_=ss3[:], func=AF.Exp, scale=-0.5), r=['ss3all'], w=['ss3all'])
            for i in range(16):
                xt = [('xn', i, 0), ('xn', i, 1)]
                S.dve(lambda e, i=i: e.scalar_tensor_tensor(out=ot[i % 2][:], in0=xn[:, i, :], scalar=ss3[:, i:i + 1], in1=gfin[:], op0=ALU.mult, op1=ALU.mult),
                      r=xt + ['ss3all', 'gfin'], w=[('ot', i % 2)])
                S.dma(lambda e, i=i: e.dma_start(out=y[i * 128:(i + 1) * 128, :], in_=ot[i % 2][:]), 'yo%d' % (i % 2), r=[('ot', i % 2)])
            final_keys.extend(['yo0', 'yo1'])
            S.flush(final_dma_keys=final_keys)
    return nc


def make_in_maps(stage, x, positions, g_attn_norm, w_in, w_pool, b_pool, pool_scale, g_q_a, w_q_b,
                 g_kv_a, w_kv_b, g_out_pool, g_out_attn, w_out, g_ffn_norm, w_router, b_router,
                 w_gate_up, b_gate_up, w_down, b_down, g_final):
    f32 = lambda a: np.ascontiguousarray(np.asarray(a, dtype=np.float32))
    x = f32(x)
    positions = np.asarray(positions, dtype=np.int32)
    inv = (10000.0 ** (-np.arange(16, dtype=np.float32) / 16)).astype(np.float32)
    invf = np.zeros((128, 1), np.float32)
    invf[64:80, 0] = inv
    invf[80:96, 0] = inv
    shared = dict(
        invf=invf,
        g_attn=f32(np.asarray(g_attn_norm)[0].reshape(8, 128).T),
        w_in=f32(np.asarray(w_in)[0]),
        w_pool=f32(np.asarray(w_pool)[0].transpose(1, 0, 2).reshape(128, 512)),
        b_pool=f32(np.asarray(b_pool)[0].T),
        pool_scale=f32(np.asarray(pool_scale)[0].reshape(4, 128).T),
        g_q=f32(np.asarray(g_q_a)[0].reshape(2, 128).T),
        w_q_b=f32(np.asarray(w_q_b)[0]),
        g_kv=f32(np.asarray(g_kv_a)[0].reshape(128, 1)),
        w_kv_b=f32(np.asarray(w_kv_b)[0]),
        g_op=f32(np.asarray(g_out_pool)[0].reshape(4, 128).T),
        g_oa=f32(np.asarray(g_out_attn)[0].reshape(8, 64).T),
        w_out=f32(np.asarray(w_out)[0]),
        g_ffn=f32(np.asarray(g_ffn_norm)[0].reshape(1, 1024)),
        g_fin=f32(np.asarray(g_final).reshape(1, 1024)),
        w_r=f32(np.asarray(w_router)[0]),
        b_r=f32(np.asarray(b_router)[0].reshape(1, 32)),
        b_gu=f32(np.asarray(b_gate_up)[0].reshape(32, 16, 128).transpose(2, 0, 1).reshape(128, 512)),
        b_d=f32(np.asarray(b_down)[0].reshape(32, 1024)),
    )
    wgu = f32(np.asarray(w_gate_up)[0])
    wdn = f32(np.asarray(w_down)[0])
    for k in range(4 if stage >= 2 else 0):
        shared["w_gu%d" % k] = wgu[8 * k:8 * k + 8]
        shared["w_d%d" % k] = wdn[8 * k:8 * k + 8]
    in_maps = []
    kk = np.arange(128)[:, None]
    qq = np.arange(128)[None, :]
    tri = (kk <= qq)
    for c in range(8):
        b, j = c // 4, c % 4
        xb = x[b]
        xo = np.zeros((16, 144, 1024), np.float32)
        po = np.zeros((16, 128), np.int32)
        for i in range(16):
            n = 4 * i + j
            lo = 128 * n - 16
            if lo < 0:
                xo[i, 16:] = xb[0:128]
            else:
                xo[i] = xb[lo:lo + 144]
            po[i] = positions[b, 128 * n:128 * n + 128]
        mk = np.zeros((128, 16, 4, 128), np.float32)
        for m in range(16):
            for r in range(4):
                if m < 4 * r + j:
                    mk[:, m, r, :] = 1.0
                elif m == 4 * r + j:
                    mk[:, m, r, :] = tri
        ic = np.zeros((128, 4, 16), np.float32)
        for gi, wdw in enumerate((2, 4, 8, 16)):
            if j == 0:
                ic[:, gi, :] = 1.0 / np.minimum(np.arange(16) + 1, wdw).astype(np.float32)
            else:
                ic[:, gi, :] = 1.0 / wdw
        m = dict(shared)
        m.update(
            x_all=np.ascontiguousarray(xb),
            x_own=xo.reshape(2304, 1024),
            pos_all=np.ascontiguousarray(positions[b].reshape(1, 8192)),
            pos_own=po.reshape(1, 2048),
            masks=mk.reshape(128, 8192).astype(ml_dtypes.bfloat16),
            invcnt=ic.reshape(128, 64),
        )
        in_maps.append(m)
    return in_maps


def assemble(results, key="y"):
    out = np.zeros((2, 8192, 1024), np.float32)
    for c in range(8):
        b, j = c // 4, c % 4
        yc = np.asarray(results[c][key]).reshape(16, 128, 1024)
        for i in range(16):
            n = 4 * i + j
            out[b, 128 * n:128 * n + 128] = yc[i]
    return out


def kernel(**inputs):
    nc = build()
    in_maps = make_in_maps(99, **inputs)
    res = run_bass_kernel_spmd(nc, in_maps, core_ids=list(range(8)))
    return assemble(res.results)
```

```python
import numpy as np
from contextlib import ExitStack
import ml_dtypes
import concourse.bass as bass
import concourse.mybir as mybir
from concourse.bass_utils import run_bass_kernel_spmd

F32 = mybir.dt.float32
BF16 = mybir.dt.bfloat16
I32 = mybir.dt.int32
AF = mybir.ActivationFunctionType
ALU = mybir.AluOpType

ENGS = ('pe', 'act', 'dve', 'pool', 'sp')
EPS = 1e-6
PI = float(np.pi)
TWO_PI = float(2 * np.pi)


class Op:
    __slots__ = ('eng', 'fn', 'waits', 'flag', 'count', 'dkey', 'dcount', 'done')

    def __init__(self, eng, fn):
        self.eng = eng
        self.fn = fn
        self.waits = []
        self.flag = False
        self.count = None
        self.dkey = None
        self.dcount = None
        self.done = False


class Sched:
    def __init__(self, nc, stack):
        self.nc = nc
        self.stack = stack
        self.sem = {e: stack.enter_context(nc.semaphore('s_' + e)) for e in ENGS}
        self.dsem = {}
        self.dcnt = {}
        self.ops = {e: [] for e in ENGS}
        self.base = {e: 0 for e in ENGS}
        self.lastw = {}
        self.readers = {}
        self.waited = {e: {} for e in ENGS}
        self.no_barrier_keys = set()
        self.flush_id = 0
        self._regs = {}
        self.enabled = True

    def _dsem(self, key):
        if key not in self.dsem:
            self.dsem[key] = self.stack.enter_context(self.nc.semaphore('d_' + str(key)))
            self.dcnt[key] = 0
        return self.dsem[key]

    def op(self, eng, fn, r=(), w=(), dma=None):
        if not self.enabled:
            return None
        o = Op(eng, fn)
        deps = []
        for t in r:
            d = self.lastw.get(t)
            if d is not None:
                deps.append(d)
        for t in w:
            d = self.lastw.get(t)
            if d is not None:
                deps.append(d)
            rd = self.readers.get(t)
            if rd:
                deps.extend(rd.values())
        seen = set()
        for d in deps:
            if d is o or id(d) in seen or d.done:
                continue
            seen.add(id(d))
            if d.dkey is None and d.eng == 'pe' and eng == 'pe':
                continue
            if d.dkey is None:
                d.flag = True
                o.waits.append(d)
            else:
                o.waits.append(('d', d.dkey, self.dcnt[d.dkey]))
        if dma is not None:
            self._dsem(dma)
            self.dcnt[dma] += 16
            o.dkey = dma
            o.dcount = self.dcnt[dma]
        for t in w:
            self.lastw[t] = o
            self.readers[t] = {}
        for t in r:
            rk = ('dma', o.dkey) if o.dkey is not None else o.eng
            self.readers.setdefault(t, {})[rk] = o
        self.ops[eng].append(o)
        return o

    def reg(self, eng, val):
        key = (self.flush_id, val)
        if key not in self._regs:
            self._regs[key] = eng.to_reg(val)
        return self._regs[key]

    def pe(self, fn, r=(), w=()):
        return self.op('pe', fn, r, w)

    def act(self, fn, r=(), w=()):
        return self.op('act', fn, r, w)

    def dve(self, fn, r=(), w=()):
        return self.op('dve', fn, r, w)

    def pool(self, fn, r=(), w=()):
        return self.op('pool', fn, r, w)

    def dma(self, fn, key, r=(), w=(), q='sp'):
        return self.op(q, fn, r, w, dma=key)

    def flush(self, final_dma_keys=()):
        nc = self.nc
        if not any(self.ops[e] for e in ENGS):
            return
        lasts = []
        for e in ENGS:
            for o in reversed(self.ops[e]):
                if o.dkey is None and o.fn is not None:
                    o.flag = True
                    lasts.append(o)
                    break
        for e in ENGS:
            b = Op(e, None)
            b.waits = [o for o in lasts if o.eng != e]
            for k in self.dcnt:
                if k not in self.no_barrier_keys:
                    b.waits.append(('d', k, self.dcnt[k]))
            self.ops[e].append(b)
        for e in ENGS:
            c = self.base[e]
            for o in self.ops[e]:
                if o.dkey is None and o.flag:
                    c += 1
                    o.count = c
            self.base[e] = c
        sem, dsem, waited, ops = self.sem, self.dsem, self.waited, self.ops

        def replay(ename):
            def body(eng):
                wd = waited[ename]
                for o in ops[ename]:
                    for d in o.waits:
                        if isinstance(d, tuple):
                            k, s, v = ('d', d[1]), dsem[d[1]], d[2]
                        else:
                            k, s, v = d.eng, sem[d.eng], d.count
                        if wd.get(k, 0) < v:
                            eng.wait_ge(s, v)
                            wd[k] = v
                    if o.fn is None:
                        continue
                    inst = o.fn(eng)
                    if o.dkey is not None:
                        inst.then_inc(dsem[o.dkey], 16)
                    elif o.flag:
                        inst.then_inc(sem[ename], 1)
            return body

        self.flush_id += 1
        with nc.Block() as blk:
            blk.tensor(replay('pe'))
            blk.scalar(replay('act'))
            blk.vector(replay('dve'))
            blk.gpsimd(replay('pool'))
            blk.sync(replay('sp'))
        for e in ENGS:
            for o in self.ops[e]:
                if o.dkey is None:
                    o.done = True
            self.ops[e] = []
        for t in list(self.lastw.keys()):
            if self.lastw[t].done:
                del self.lastw[t]
        for t in list(self.readers.keys()):
            rd = self.readers[t]
            for k in [k for k, o in rd.items() if o.done]:
                del rd[k]
            if not rd:
                del self.readers[t]


def build(stage=99, debug=False, upto='Z'):
    nc = bass.Bass("TRN2", target_bir_lowering=False)
    D = lambda name, shape, dt, kind="ExternalInput": nc.dram_tensor(name, shape, dt, kind=kind).ap()
    x_all = D("x_all", [8192, 1024], F32)
    x_own = D("x_own", [2304, 1024], F32)
    pos_all = D("pos_all", [1, 8192], I32)
    pos_own = D("pos_own", [1, 2048], I32)
    masks_d = D("masks", [128, 8192], BF16)
    invcnt_d = D("invcnt", [128, 64], F32)
    invf_d = D("invf", [128, 1], F32)
    g_attn_d = D("g_attn", [128, 8], F32)
    w_in = D("w_in", [1024, 928], F32)
    w_pool_d = D("w_pool", [128, 512], F32)
    b_pool_d = D("b_pool", [128, 4], F32)
    pool_scale_d = D("pool_scale", [128, 4], F32)
    g_q_d = D("g_q", [128, 2], F32)
    w_q_b = D("w_q_b", [256, 768], F32)
    g_kv_d = D("g_kv", [128, 1], F32)
    w_kv_b = D("w_kv_b", [128, 1024], F32)
    g_op_d = D("g_op", [128, 4], F32)
    g_oa_d = D("g_oa", [64, 8], F32)
    w_out = D("w_out", [1024, 1024], F32)
    g_ffn_d = D("g_ffn", [1, 1024], F32)
    g_fin_d = D("g_fin", [1, 1024], F32)
    w_r = D("w_r", [1024, 32], F32)
    b_r = D("b_r", [1, 32], F32)
    w_gu_p = [D("w_gu%d" % k, [8, 1024, 2048], F32) for k in range(4)] if stage >= 2 else None
    b_gu_d = D("b_gu", [128, 512], F32)
    w_d_p = [D("w_d%d" % k, [8, 1024, 1024], F32) for k in range(4)] if stage >= 2 else None
    b_d_d = D("b_d", [32, 1024], F32)
    y = D("y", [2048, 1024], F32, kind="ExternalOutput")
    if debug:
        dbg_x1 = D("dbg_x1", [2048, 1024], F32, kind="ExternalOutput")
        dbg_yp = D("dbg_yp", [128, 8192], BF16, kind="ExternalOutput")
        dbg_ya = D("dbg_ya", [64, 16384], BF16, kind="ExternalOutput")
        dbg_cn = D("dbg_cn", [128, 8192], BF16, kind="ExternalOutput")
        dbg_kt = D("dbg_kt", [96, 8192], BF16, kind="ExternalOutput")
        dbg_gates = D("dbg_gates", [128, 512], F32, kind="ExternalOutput")
        dbg_rs = D("dbg_rs", [128, 32], F32, kind="ExternalOutput")
    final_keys = []
    CAP = 384
    NROWS = 32 * CAP
    BIGV = 1.0e6
    xs = nc.dram_tensor("xs_scratch", [NROWS, 1024], BF16, kind="Internal").ap()
    ys = nc.dram_tensor("ys_scratch", [NROWS, 1024], F32, kind="Internal").ap()

    with ExitStack() as top:
        S = Sched(nc, top)
        uid = [0]

        def T(st, name, shape, dt):
            uid[0] += 1
            return st.enter_context(nc.sbuf_tensor("%s_%d" % (name, uid[0]), shape, dt))

        def PS(st, name, shape, dt):
            uid[0] += 1
            return st.enter_context(nc.psum_tensor("%s_%d" % (name, uid[0]), shape, dt))
        BIG0 = T(top, "BIG0", [128, 16384], F32)
        BIG1 = T(top, "BIG1", [128, 16384], BF16)
        BIG2 = T(top, "BIG2", [128, 24576], BF16)
        B0 = BIG0[:]
        B0b = BIG0[:].bitcast(BF16)
        B0i = BIG0[:].bitcast(I32)
        B1 = BIG1[:]
        B2 = BIG2[:]
        ident_f = T(top, "ident_f", [128, 128], F32)
        ident_b = T(top, "ident_b", [128, 128], BF16)
        ones_b = T(top, "ones_b", [128, 128], BF16)
        sel_f = T(top, "sel_f", [128, 64], F32)
        epsc = T(top, "epsc", [128, 1], F32)
        invf = T(top, "invf_t", [128, 1], F32)
        rstdp = T(top, "rstdp", [128, 16], F32)
        rstda = T(top, "rstda", [128, 16], F32)
        ssa = T(top, "ssa", [128, 16], F32)
        gates = T(top, "gates", [128, 512], F32)
        g_oa = T(top, "g_oa_t", [64, 8], F32)
        junk = T(top, "junk", [128, 1024], BF16)
        Lst = T(top, "Lst", [128, 128], BF16)
        tri_f = T(top, "tri_f", [128, 128], F32)
        CE = T(top, "CE", [128, 32], F32)
        CEi = T(top, "CEi", [128, 32], I32)
        carry = T(top, "carry", [128, 32], F32)
        desti = T(top, "desti", [128, 64], I32)
        gk = T(top, "gk", [128, 64], F32)

        ypoolT = B2[:, 0:8192].rearrange("p (g t) -> p g t", g=4)
        yattnT = B2[:, 8192:24576].rearrange("p (h t) -> p h t", h=8)

        S.pool(lambda e: e.memset(ident_f[:], 0.0), w=['ident_f'])
        S.pool(lambda e: e.affine_select(out=ident_f[:], in_=ident_f[:], pattern=[[-1, 128]],
                                         compare_op=ALU.not_equal, fill=1.0, base=0, channel_multiplier=1),
               r=['ident_f'], w=['ident_f'])
        S.dve(lambda e: e.tensor_copy(out=ident_b[:], in_=ident_f[:]), r=['ident_f'], w=['ident_b'])
        S.pool(lambda e: e.memset(ones_b[:], 1.0), w=['ones_b'])
        S.pool(lambda e: e.memset(epsc[:], EPS), w=['epsc'])
        S.pool(lambda e: e.memset(sel_f[:], 0.0), w=['sel_f'])
        S.pool(lambda e: e.memset(sel_f[64:65, :], 1.0), r=['sel_f'], w=['sel_f'])
        S.dma(lambda e: e.dma_start(out=invf[:], in_=invf_d), 'c0', w=['invf'])
        S.pool(lambda e: e.memset(tri_f[:], 1.0), w=['tri_f'])
        S.pool(lambda e: e.affine_select(out=tri_f[:], in_=tri_f[:], pattern=[[1, 128]], compare_op=ALU.is_gt, fill=0.0, base=0, channel_multiplier=-1),
               r=['tri_f'], w=['tri_f'])
        S.dve(lambda e: e.tensor_copy(out=Lst[:], in_=tri_f[:]), r=['tri_f'], w=['Lst'])
        S.pool(lambda e: e.memset(gk[:], 0.0), w=['gk0'])
        S.pool(lambda e: e.memset(carry[:], 0.0), w=['carry'])
        S.pool(lambda e: e.iota(out=CEi[:], pattern=[[CAP, 32]], base=0, channel_multiplier=0), w=['CEi'])
        S.dve(lambda e: e.tensor_copy(out=CE[:], in_=CEi[:]), r=['CEi'], w=['CE'])
        S.pool(lambda e: e.memset(B0b[:, 8192:16384], 0.0), w=['zsrc'])
        for zz in range(NROWS // 1024):
            S.dma(lambda e, zz=zz: e.dma_start(out=xs[zz * 1024:(zz + 1) * 1024, :].rearrange("(n p) d -> p n d", p=128),
                                               in_=B0b[:, 8192:16384].rearrange("p (n d) -> p n d", n=8)), 'zf', r=['zsrc'])
        S.dma(lambda e: e.dma_start(out=g_oa[:], in_=g_oa_d), 'c1', w=['g_oa'])

        def rms_tile(src_ap, slot_tag, ss_ap, ss_tag, xb_ap, xb_tag, ncols=1024, inv_n=1.0 / 1024):
            S.act(lambda e: e.activation(out=junk[:, 0:ncols], in_=src_ap, func=AF.Square, accum_out=ss_ap),
                  r=[slot_tag], w=['junk', ss_tag])
            S.act(lambda e: e.activation(out=ss_ap, in_=ss_ap, func=AF.Ln, bias=epsc[:, 0:1], scale=inv_n),
                  r=[ss_tag, 'epsc'], w=[ss_tag])
            S.act(lambda e: e.activation(out=ss_ap, in_=ss_ap, func=AF.Exp, scale=-0.5), r=[ss_tag], w=[ss_tag])
            if xb_ap is not None:
                S.dve(lambda e: e.tensor_scalar(out=xb_ap, in0=src_ap, scalar1=ss_ap, scalar2=None, op0=ALU.mult),
                      r=[slot_tag, ss_tag], w=[xb_tag])

        def rope_tables(st, pos_src_ap, n, posi, posf, ang, kf, sn, cs, tagp, otag=None):
            otag = otag or tagp
            sl = slice(64, 96)
            S.dma(lambda e: e.dma_start(out=posi[sl, 0:n], in_=pos_src_ap.partition_broadcast(32)), 'pos', w=[tagp + 'posi'])
            S.dve(lambda e: e.tensor_copy(out=posf[sl, 0:n], in_=posi[sl, 0:n]), r=[tagp + 'posi'], w=[tagp + 'posf'])
            S.dve(lambda e: e.tensor_scalar(out=ang[sl, 0:n], in0=posf[sl, 0:n], scalar1=invf[sl, 0:1], scalar2=None, op0=ALU.mult),
                  r=[tagp + 'posf', 'invf'], w=[tagp + 'ang'])
            S.dve(lambda e: e.tensor_scalar(out=posi[sl, 0:n], in0=ang[sl, 0:n], scalar1=1.0 / TWO_PI, scalar2=None, op0=ALU.mult),
                  r=[tagp + 'ang', tagp + 'posf'], w=[tagp + 'posi'])
            S.dve(lambda e: e.tensor_copy(out=kf[sl, 0:n], in_=posi[sl, 0:n]), r=[tagp + 'posi'], w=[tagp + 'kf'])
            C1 = 6.28125
            C2 = float(TWO_PI - C1)
            S.dve(lambda e: e.scalar_tensor_tensor(out=ang[sl, 0:n], in0=kf[sl, 0:n], scalar=-C1, in1=ang[sl, 0:n], op0=ALU.mult, op1=ALU.add),
                  r=[tagp + 'kf', tagp + 'ang'], w=[tagp + 'ang'])
            S.dve(lambda e: e.scalar_tensor_tensor(out=ang[sl, 0:n], in0=kf[sl, 0:n], scalar=-C2, in1=ang[sl, 0:n], op0=ALU.mult, op1=ALU.add),
                  r=[tagp + 'kf', tagp + 'ang'], w=[tagp + 'ang'])
            S.dve(lambda e: e.tensor_scalar(out=kf[sl, 0:n], in0=ang[sl, 0:n], scalar1=PI, scalar2=-TWO_PI, op0=ALU.is_gt, op1=ALU.mult),
                  r=[tagp + 'ang'], w=[tagp + 'kf'])
            S.dve(lambda e: e.tensor_tensor(out=ang[sl, 0:n], in0=ang[sl, 0:n], in1=kf[sl, 0:n], op=ALU.add),
                  r=[tagp + 'ang', tagp + 'kf'], w=[tagp + 'ang'])
            S.dve(lambda e: e.tensor_scalar(out=kf[sl, 0:n], in0=ang[sl, 0:n], scalar1=-PI, scalar2=TWO_PI, op0=ALU.is_lt, op1=ALU.mult),
                  r=[tagp + 'ang'], w=[tagp + 'kf'])
            S.dve(lambda e: e.tensor_tensor(out=ang[sl, 0:n], in0=ang[sl, 0:n], in1=kf[sl, 0:n], op=ALU.add),
                  r=[tagp + 'ang', tagp + 'kf'], w=[tagp + 'ang'])
            S.act(lambda e: e.activation(out=sn[sl, 0:n], in_=ang[sl, 0:n], func=AF.Sin), r=[tagp + 'ang'], w=[otag + 'sn'])
            S.dve(lambda e: e.tensor_scalar(out=kf[sl, 0:n], in0=ang[sl, 0:n], scalar1=PI / 2, scalar2=-TWO_PI, op0=ALU.is_gt, op1=ALU.mult),
                  r=[tagp + 'ang'], w=[tagp + 'kf'])
            S.dve(lambda e: e.scalar_tensor_tensor(out=ang[sl, 0:n], in0=ang[sl, 0:n], scalar=PI / 2, in1=kf[sl, 0:n], op0=ALU.add, op1=ALU.add),
                  r=[tagp + 'ang', tagp + 'kf'], w=[tagp + 'ang'])
            S.act(lambda e: e.activation(out=cs[sl, 0:n], in_=ang[sl, 0:n], func=AF.Sin), r=[tagp + 'ang'], w=[otag + 'cs'])

        with ExitStack() as p2:
            Wkvlat = T(p2, "Wkvlat", [128, 8, 128], BF16)
            Wkr = T(p2, "Wkr", [128, 8, 96], BF16)
            Wkrot = T(p2, "Wkrot", [128, 8, 96], BF16)
            Wq = T(p2, "Wq", [128, 2, 768], BF16)
            Wqrot = T(p2, "Wqrot", [128, 2, 768], BF16)
            Wkv = T(p2, "Wkv", [128, 1024], BF16)
            wpool = T(p2, "wpool", [128, 512], BF16)
            qnT = T(p2, "qnT", [128, 2, 2048], BF16)
            g_attn = T(p2, "g_attn_t", [128, 8], F32)
            ng_attn = T(p2, "ng_attn_t", [128, 8], F32)
            g_q = T(p2, "g_q_t", [128, 2], F32)
            ng_q = T(p2, "ng_q_t", [128, 2], F32)
            g_kv = T(p2, "g_kv_t", [128, 1], F32)
            g_op = T(p2, "g_op_t", [128, 4], F32)
            bpool = T(p2, "bpool_t", [128, 4], F32)
            pscale = T(p2, "pscale_t", [128, 4], F32)
            bsc = T(p2, "bsc", [128, 4], F32)
            scg = T(p2, "scg", [128, 4], F32)
            bsg = T(p2, "bsg", [128, 4], F32)
            invcnt = T(p2, "invcnt_t", [128, 64], F32)

            Win_uq = B2[:, 8192:8192 + 6144].rearrange("p (c n) -> p c n", c=8)
            for i, (dst, src) in enumerate([(g_attn, g_attn_d), (g_q, g_q_d), (g_kv, g_kv_d), (g_op, g_op_d),
                                            (bpool, b_pool_d), (pscale, pool_scale_d), (invcnt, invcnt_d)]):
                S.dma(lambda e, dst=dst, src=src: e.dma_start(out=dst[:], in_=src), 'c%d' % (2 + i), w=[('cst', i)])
            S.dve(lambda e: e.tensor_scalar(out=ng_attn[:], in0=g_attn[:], scalar1=-1.0, scalar2=None, op0=ALU.mult), r=[('cst', 0)], w=['ng_attn'])
            S.dve(lambda e: e.tensor_scalar(out=ng_q[:], in0=g_q[:], scalar1=-1.0, scalar2=None, op0=ALU.mult), r=[('cst', 1)], w=['ng_q'])
            S.dve(lambda e: e.tensor_tensor(out=bsc[:], in0=bpool[:], in1=pscale[:], op=ALU.mult), r=[('cst', 4), ('cst', 5)], w=['bsc'])
            S.dve(lambda e: e.tensor_tensor(out=scg[:], in0=pscale[:], in1=g_op[:], op=ALU.mult), r=[('cst', 3), ('cst', 5)], w=['scg'])
            S.dve(lambda e: e.tensor_tensor(out=bsg[:], in0=bsc[:], in1=g_op[:], op=ALU.mult), r=[('cst', 3), 'bsc'], w=['bsg'])
            S.pool(lambda e: e.memset(Wkr[:], 0.0), w=['Wkr'])
            S.pool(lambda e: e.memset(Wkrot[:], 0.0), w=['Wkrot'])
            S.pool(lambda e: e.memset(Wqrot[:], 0.0), w=['Wqrot'])
            S.dma(lambda e: e.dma_start(out=wpool[:], in_=w_pool_d), 'wpool', w=['wpool'], q='pool')
            stg = B0[:, 0:4096]
            for half in range(2):
                stv = stg[:, 0:3712].rearrange("p (c n) -> p c n", c=4)
                S.dma(lambda e, half=half, stv=stv: e.dma_start(
                    out=stv, in_=w_in[half * 512:(half + 1) * 512, :].rearrange("(c p) n -> p c n", p=128)), 'stg', w=['stg'])
                for c in range(4):
                    cc = half * 4 + c
                    gs = g_attn[:, cc:cc + 1]
                    ngs = ng_attn[:, cc:cc + 1]
                    S.dve(lambda e, c=c, cc=cc, gs=gs, stv=stv: e.tensor_scalar(out=Win_uq[:, cc, :], in0=stv[:, c, 0:768], scalar1=gs, scalar2=None, op0=ALU.mult),
                          r=['stg', ('cst', 0)], w=['Win_uq'])
                    S.dve(lambda e, c=c, cc=cc, gs=gs, stv=stv: e.tensor_scalar(out=Wkvlat[:, cc, :], in0=stv[:, c, 768:896], scalar1=gs, scalar2=None, op0=ALU.mult),
                          r=['stg', ('cst', 0)], w=['Wkvlat'])
                    S.dve(lambda e, c=c, cc=cc, gs=gs, stv=stv: e.tensor_scalar(out=Wkr[:, cc, 64:96], in0=stv[:, c, 896:928], scalar1=gs, scalar2=None, op0=ALU.mult),
                          r=['stg', ('cst', 0), 'Wkr'], w=['Wkr'])
                    S.dve(lambda e, c=c, cc=cc, ngs=ngs, stv=stv: e.tensor_scalar(out=Wkrot[:, cc, 64:80], in0=stv[:, c, 912:928], scalar1=ngs, scalar2=None, op0=ALU.mult),
                          r=['stg', 'ng_attn', 'Wkrot'], w=['Wkrot'])
                    S.dve(lambda e, c=c, cc=cc, gs=gs, stv=stv: e.tensor_scalar(out=Wkrot[:, cc, 80:96], in0=stv[:, c, 896:912], scalar1=gs, scalar2=None, op0=ALU.mult),
                          r=['stg', ('cst', 0), 'Wkrot'], w=['Wkrot'])
            stq = stg[:, 0:1536].rearrange("p (c n) -> p c n", c=2)
            S.dma(lambda e: e.dma_start(out=stq, in_=w_q_b.rearrange("(c p) n -> p c n", p=128)), 'stg', w=['stg'])
            for c in range(2):
                S.dve(lambda e, c=c: e.tensor_scalar(out=Wq[:, c, :], in0=stq[:, c, :], scalar1=g_q[:, c:c + 1], scalar2=None, op0=ALU.mult),
                      r=['stg', ('cst', 1)], w=['Wq'])
                sq4 = stq[:, c, :].rearrange("p (h d) -> p h d", h=8)
                wr4 = Wqrot[:, c, :].rearrange("p (h d) -> p h d", h=8)
                S.dve(lambda e, c=c, sq4=sq4, wr4=wr4: e.tensor_scalar(out=wr4[:, :, 64:80], in0=sq4[:, :, 80:96], scalar1=ng_q[:, c:c + 1], scalar2=None, op0=ALU.mult),
                      r=['stg', 'ng_q', 'Wqrot'], w=['Wqrot'])
                S.dve(lambda e, c=c, sq4=sq4, wr4=wr4: e.tensor_scalar(out=wr4[:, :, 80:96], in0=sq4[:, :, 64:80], scalar1=g_q[:, c:c + 1], scalar2=None, op0=ALU.mult),
                      r=['stg', ('cst', 1), 'Wqrot'], w=['Wqrot'])
            S.dma(lambda e: e.dma_start(out=stg[:, 0:1024], in_=w_kv_b), 'stg', w=['stg'])
            S.dve(lambda e: e.tensor_scalar(out=Wkv[:], in0=stg[:, 0:1024], scalar1=g_kv[:, 0:1], scalar2=None, op0=ALU.mult),
                  r=['stg', ('cst', 2)], w=['Wkv'])
            S.flush()
            if upto == 'W':
                S.enabled = False

            with ExitStack() as pb:
                uT = B0[:, 0:9216].rearrange("p (g t) -> p g t", g=4)
                S1 = B0[:, 9216:11520]
                S2 = B0[:, 11520:13824]
                xa = [B0[:, 13824:14848], B0[:, 14848:15872]]
                diffT = B1[:, 0:9216].rearrange("p (g t) -> p g t", g=4)
                hT = [B1[:, 9216:12288].rearrange("p (c t) -> p c t", c=8), B1[:, 12288:15360].rearrange("p (c t) -> p c t", c=8)]
                xb = B1[:, 15360:16384]
                qn_ext = B2[:, 14336:14336 + 4608].rearrange("p (c t) -> p c t", c=2)
                ss_b = T(pb, "ss_b", [128, 18], F32)
                sqq = T(pb, "sqq", [128, 2, 384], BF16)
                rq = T(pb, "rq", [128, 384], F32)
                ysq = T(pb, "ysq", [128, 4, 512], BF16)
                tmp16 = T(pb, "tmp16", [128, 16], F32)
                pT = [PS(pb, "pT0", [128, 8, 128], BF16), PS(pb, "pT1", [128, 8, 128], BF16)]
                pU = [PS(pb, "pU0", [128, 512], F32), PS(pb, "pU1", [128, 512], F32)]
                pQ = [PS(pb, "pQ0", [128, 512], F32), PS(pb, "pQ1", [128, 512], F32)]
                pSS = PS(pb, "pSS", [128, 512], F32)
                xbs = [xb, T(pb, "xb2", [128, 1024], BF16)[:]]

                xa = xa + [T(pb, "xa2", [128, 1024], F32)[:], T(pb, "xa3", [128, 1024], F32)[:]]

                def stats_b(tl):
                    s4 = tl % 4
                    S.dma(lambda e, tl=tl, s4=s4: e.dma_start(out=xa[s4], in_=x_own[tl * 128:(tl + 1) * 128, :]), 'xa%d' % s4, w=[('xa', s4)])
                    rms_tile(xa[s4], ('xa', s4), ss_b[:, tl:tl + 1], ('ssb', tl), None, None)

                def norm_T_b(tl):
                    sl_ = tl % 2
                    s4 = tl % 4
                    xbc = xbs[sl_]
                    S.dve(lambda e, tl=tl, s4=s4, xbc=xbc: e.tensor_scalar(out=xbc, in0=xa[s4], scalar1=ss_b[:, tl:tl + 1], scalar2=None, op0=ALU.mult),
                          r=[('xa', s4), ('ssb', tl)], w=[('xb', sl_)])
                    for c in range(8):
                        S.pe(lambda e, c=c, sl_=sl_, xbc=xbc: e.transpose(out=pT[sl_][:, c, :], in_=xbc[:, c * 128:(c + 1) * 128], identity=ident_b[:]),
                             r=[('xb', sl_), 'ident_b'], w=[('pT', sl_)])

                def super_b(st):
                    hr = [('hT', st % 2, k) for k in range(3)]
                    for g in range(4):
                        for c in range(8):
                            S.pe(lambda e, g=g, c=c, st=st: e.matmul(pU[g % 2][:, 0:384], lhsT=Win_uq[:, c, g * 128:(g + 1) * 128], rhs=hT[st % 2][:, c, :],
                                                                      start=(c == 0), stop=(c == 7)),
                                 r=hr + ['Win_uq'], w=[('pU', g % 2)])
                        if g % 2 == 0:
                            S.act(lambda e, g=g, st=st: e.copy(out=uT[:, g, st * 384:(st + 1) * 384], in_=pU[g % 2][:, 0:384]), r=[('pU', g % 2)], w=[('uT', g)])
                        else:
                            S.dve(lambda e, g=g, st=st: e.tensor_copy(out=uT[:, g, st * 384:(st + 1) * 384], in_=pU[g % 2][:, 0:384]), r=[('pU', g % 2)], w=[('uT', g)])
                    for q in range(2):
                        for c in range(8):
                            S.pe(lambda e, q=q, c=c, st=st: e.matmul(pQ[q][:, 0:384], lhsT=Win_uq[:, c, 512 + q * 128:512 + (q + 1) * 128], rhs=hT[st % 2][:, c, :],
                                                                      start=(c == 0), stop=(c == 7)),
                                 r=hr + ['Win_uq'], w=[('pQ', q)])
                        S.act(lambda e, q=q: e.activation(out=sqq[:, q, :], in_=pQ[q][:, 0:384], func=AF.Square), r=[('pQ', q)], w=[('sqq', q)])
                    for q in range(2):
                        S.pe(lambda e, q=q: e.matmul(pSS[:, 0:384], lhsT=ones_b[:], rhs=sqq[:, q, :], start=(q == 0), stop=(q == 1)),
                             r=[('sqq', q), 'ones_b'], w=['pSS'])
                    S.act(lambda e: e.activation(out=rq[:], in_=pSS[:, 0:384], func=AF.Ln, bias=epsc[:, 0:1], scale=1.0 / 256),
                          r=['pSS', 'epsc'], w=['rq'])
                    S.act(lambda e: e.activation(out=rq[:], in_=rq[:], func=AF.Exp, scale=-0.5), r=['rq'], w=['rq'])
                    for q in range(2):
                        S.dve(lambda e, q=q, st=st: e.tensor_tensor(out=qn_ext[:, q, st * 384:(st + 1) * 384], in0=pQ[q][:, 0:384], in1=rq[:], op=ALU.mult),
                              r=[('pQ', q), 'rq'], w=['qn_ext'])

                def copy_b(tl):
                    sl_ = tl % 2
                    st = tl // 3
                    k3 = tl % 3
                    S.dve(lambda e: e.tensor_copy(out=hT[st % 2][:, :, k3 * 128:(k3 + 1) * 128], in_=pT[sl_][:]),
                          r=[('pT', sl_)], w=[('hT', st % 2, k3)])
                    if k3 == 2:
                        super_b(st)

                stats_b(0)
                stats_b(1)
                for tl in range(18):
                    if tl + 2 < 18:
                        stats_b(tl + 2)
                    norm_T_b(tl)
                    if tl >= 1:
                        copy_b(tl - 1)
                copy_b(17)

                for q in range(2):
                    S.dve(lambda e, q=q: e.tensor_copy(out=qnT[:, q, :].rearrange("p (i s) -> p i s", i=16),
                                                       in_=qn_ext[:, q, :].rearrange("p (i s) -> p i s", i=16)[:, :, 16:144]),
                          r=['qn_ext'], w=['qnT'])
                N = 2304
                S.pool(lambda e: e.memset(S1, 0.0), w=['S1'])
                S.pool(lambda e: e.memset(S2, 0.0), w=['S2'])

                def shift_add(dst, src, sh, rtag, wtag):
                    S.dve(lambda e: e.tensor_tensor(out=dst[:, sh:N], in0=src[:, sh:N], in1=src[:, 0:N - sh], op=ALU.add), r=[rtag], w=[wtag])

                for g, wdw in enumerate((2, 4, 8, 16)):
                    ug = uT[:, g, :]
                    shift_add(S1, ug, 1, ('uT', g), 'S1')
                    fin, ftag = S1, 'S1'
                    if wdw >= 4:
                        shift_add(S2, S1, 2, 'S1', 'S2')
                        fin, ftag = S2, 'S2'
                    if wdw >= 8:
                        shift_add(S1, S2, 4, 'S2', 'S1')
                        fin, ftag = S1, 'S1'
                    if wdw >= 16:
                        shift_add(S2, S1, 8, 'S1', 'S2')
                        fin, ftag = S2, 'S2'
                    S.dve(lambda e, g=g, fin=fin, wdw=wdw, ug=ug: e.scalar_tensor_tensor(out=diffT[:, g, 16:N], in0=fin[:, 16:N], scalar=1.0 / wdw, in1=ug[:, 16:N],
                                                                                     op0=ALU.mult, op1=ALU.subtract),
                          r=[ftag, ('uT', g)], w=[('diffT', g)])
                    S.dve(lambda e, g=g, fin=fin: e.tensor_tensor(out=tmp16[:], in0=fin[:, 16:32], in1=invcnt[:, g * 16:(g + 1) * 16], op=ALU.mult),
                          r=[ftag, ('cst', 6)], w=['tmp16'])
                    S.dve(lambda e, g=g, ug=ug: e.tensor_tensor(out=diffT[:, g, 16:32], in0=tmp16[:], in1=ug[:, 16:32], op=ALU.subtract),
                          r=['tmp16', ('uT', g), ('diffT', g)], w=[('diffT', g)])
                wpv = wpool[:].rearrange("p (g d) -> p g d", g=4)
                for qg in range(4):
                    for g in range(4):
                        rhs = diffT[:, g, :].rearrange("p (i s) -> p i s", i=16)[:, 4 * qg:4 * qg + 4, 16:144]
                        S.pe(lambda e, g=g, rhs=rhs: e.matmul(pU[g % 2][:, 0:512].rearrange("p (a b) -> p a b", a=4), lhsT=wpv[:, g, :], rhs=rhs, start=True, stop=True),
                             r=[('diffT', g), 'wpool'], w=[('pU', g % 2)])
                        S.act(lambda e, g=g, qg=qg: e.activation(out=ypoolT[:, g, qg * 512:(qg + 1) * 512], in_=pU[g % 2][:, 0:512], func=AF.Identity,
                                                                bias=bsg[:, g:g + 1], scale=scg[:, g:g + 1]),
                              r=[('pU', g % 2), 'bsg', 'scg'], w=[('ypoolT', qg)])
                        S.act(lambda e, g=g: e.activation(out=ysq[:, g, :], in_=pU[g % 2][:, 0:512], func=AF.Square,
                                                          bias=bsc[:, g:g + 1], scale=pscale[:, g:g + 1]),
                              r=[('pU', g % 2), 'bsc', ('cst', 5)], w=[('ysq', g)])
                    for t4 in range(4):
                        i = qg * 4 + t4
                        for g in range(4):
                            S.pe(lambda e, g=g, t4=t4, i=i: e.matmul(pSS[:, i:i + 1], lhsT=ysq[:, g, t4 * 128:(t4 + 1) * 128], rhs=ones_b[:, 0:1],
                                                                     start=(g == 0), stop=(g == 3)),
                                 r=[('ysq', g), 'ones_b'], w=['pSS'])
                S.act(lambda e: e.activation(out=rstdp[:], in_=pSS[:, 0:16], func=AF.Ln, bias=epsc[:, 0:1], scale=1.0 / 512),
                      r=['pSS', 'epsc'], w=['rstdp'])
                S.act(lambda e: e.activation(out=rstdp[:], in_=rstdp[:], func=AF.Exp, scale=-0.5), r=['rstdp'], w=['rstdp'])
                if debug:
                    S.dma(lambda e: e.dma_start(out=dbg_yp, in_=B2[:, 0:8192]), 'dbg0', r=[('ypoolT', q) for q in range(4)])
                    final_keys.append('dbg0')
                S.flush()
                if upto == 'B':
                    S.enabled = False

            cnT = B1[:, 0:8192]
            KT = B1[:, 8192:16384]
            with ExitStack() as pa:
                xa = [B0[:, 0:1024], B0[:, 1024:2048]]
                posf, ang, kf, sn, cs, t1 = [B0[:, 2048 + k * 512:2048 + (k + 1) * 512] for k in range(6)]
                posi = B0i[:, 5120:5632]
                rkv = B0[:, 5632:6144]
                t2 = B0[:, 6144:6656]
                hT = [B0b[:, 14336:18432].rearrange("p (c t) -> p c t", c=8), B0b[:, 18432:22528].rearrange("p (c t) -> p c t", c=8)]
                xb = B0b[:, 22528:23552]
                sq = B0b[:, 23552:24064]
                ss_a = T(pa, "ss_a", [128, 64], F32)
                pT = [PS(pa, "pT0", [128, 8, 128], BF16), PS(pa, "pT1", [128, 8, 128], BF16)]
                pKV = PS(pa, "pKV", [128, 512], F32)
                pKR = PS(pa, "pKR", [128, 512], F32)
                pKO = PS(pa, "pKO", [128, 512], F32)
                pSS = PS(pa, "pSS", [128, 512], F32)
                sl = slice(64, 96)
                xbs = [xb, B0b[:, 24064:25088]]

                xa = xa + [B0[:, 12544:13568], B0[:, 13568:14592]]

                def stats_a(tl):
                    s4 = tl % 4
                    S.dma(lambda e, tl=tl, s4=s4: e.dma_start(out=xa[s4], in_=x_all[tl * 128:(tl + 1) * 128, :]), 'xa%d' % s4, w=[('xa', s4)])
                    rms_tile(xa[s4], ('xa', s4), ss_a[:, tl:tl + 1], ('ssa_', tl), None, None)

                def norm_T_a(tl):
                    sl_ = tl % 2
                    s4 = tl % 4
                    xbc = xbs[sl_]
                    S.dve(lambda e, tl=tl, s4=s4, xbc=xbc: e.tensor_scalar(out=xbc, in0=xa[s4], scalar1=ss_a[:, tl:tl + 1], scalar2=None, op0=ALU.mult),
                          r=[('xa', s4), ('ssa_', tl)], w=[('xb', sl_)])
                    for c in range(8):
                        S.pe(lambda e, c=c, sl_=sl_, xbc=xbc: e.transpose(out=pT[sl_][:, c, :], in_=xbc[:, c * 128:(c + 1) * 128], identity=ident_b[:]),
                             r=[('xb', sl_), 'ident_b'], w=[('pT', sl_)])

                def super_a(st):
                    hr = [('hT', st % 2, k) for k in range(4)]
                    cols = slice(st * 512, (st + 1) * 512)
                    for c in range(8):
                        S.pe(lambda e, c=c, st=st: e.matmul(pKV[:], lhsT=Wkvlat[:, c, :], rhs=hT[st % 2][:, c, :], start=(c == 0), stop=(c == 7)),
                             r=hr + ['Wkvlat'], w=['pKV'])
                    for c in range(8):
                        S.pe(lambda e, c=c, st=st: e.matmul(pKR[0:96, :], lhsT=Wkr[:, c, :], rhs=hT[st % 2][:, c, :], start=(c == 0), stop=(c == 7)),
                             r=hr + ['Wkr'], w=['pKR'])
                    for c in range(8):
                        S.pe(lambda e, c=c, st=st: e.matmul(pKO[0:96, :], lhsT=Wkrot[:, c, :], rhs=hT[st % 2][:, c, :], start=(c == 0), stop=(c == 7)),
                             r=hr + ['Wkrot'], w=['pKO'])
                    S.act(lambda e: e.activation(out=sq, in_=pKV[:], func=AF.Square), r=['pKV'], w=['sq'])
                    S.pe(lambda e: e.matmul(pSS[:], lhsT=ones_b[:], rhs=sq, start=True, stop=True), r=['sq', 'ones_b'], w=['pSS'])
                    S.act(lambda e: e.activation(out=rkv, in_=pSS[:], func=AF.Ln, bias=epsc[:, 0:1], scale=1.0 / 128),
                          r=['pSS', 'epsc'], w=['rkv'])
                    S.act(lambda e: e.activation(out=rkv, in_=rkv, func=AF.Exp, scale=-0.5), r=['rkv'], w=['rkv'])
                    S.dve(lambda e, cols=cols: e.tensor_tensor(out=cnT[:, cols], in0=pKV[:], in1=rkv, op=ALU.mult), r=['pKV', 'rkv'], w=[('cnT', st)])
                    rope_tables(st, pos_all[0:1, cols], 512, posi, posf, ang, kf, sn, cs, 'A')
                    S.dve(lambda e: e.tensor_tensor(out=t1[sl, :], in0=pKR[sl, :], in1=cs[sl, :], op=ALU.mult), r=['pKR', 'Acs'], w=['t1'])
                    S.dve(lambda e: e.tensor_tensor(out=t2[sl, :], in0=pKO[sl, :], in1=sn[sl, :], op=ALU.mult), r=['pKO', 'Asn'], w=['t2'])
                    S.dve(lambda e, cols=cols: e.tensor_tensor(out=KT[sl, cols], in0=t1[sl, :], in1=t2[sl, :], op=ALU.add), r=['t1', 't2'], w=[('KTr', st)])

                def copy_a(tl):
                    sl_ = tl % 2
                    st = tl // 4
                    k4 = tl % 4
                    S.dve(lambda e: e.tensor_copy(out=hT[st % 2][:, :, k4 * 128:(k4 + 1) * 128], in_=pT[sl_][:]),
                          r=[('pT', sl_)], w=[('hT', st % 2, k4)])
                    if k4 == 3:
                        super_a(st)

                stats_a(0)
                stats_a(1)
                for tl in range(64):
                    if tl + 2 < 64:
                        stats_a(tl + 2)
                    norm_T_a(tl)
                    if tl >= 1:
                        copy_a(tl - 1)
                copy_a(63)

                if debug:
                    S.dma(lambda e: e.dma_start(out=dbg_cn, in_=cnT), 'dbg1', r=[('cnT', s) for s in range(16)])
                    final_keys.append('dbg1')
                S.flush()
                if upto == 'A':
                    S.enabled = False

            with ExitStack() as pc:
                masks = B0b[:, 0:8192].rearrange("p (m q) -> p m q", m=16)
                V = B0b[:, 8192:8192 + 4160].rearrange("p (n d) -> p n d", n=64)
                QT = B0b[:, 12352:14400]
                PT = [B0b[:, 14400 + k * 512:14400 + (k + 1) * 512] for k in range(4)]
                cosq = B0[:, 8224:10272]
                sinq = B0[:, 10272:12320]
                Osb = B0[:, 12320:12832]
                rec = B0[:, 12832:13344]
                posf, ang, kf = [B0[:, 13344 + k * 512:13344 + (k + 1) * 512] for k in range(3)]
                posi = B0i[:, 14880:15392]
                ytmp = B0[:, 15392:15904]
                ysqa = T(pc, "ysqa", [64, 512], BF16)
                PT = PT + [T(pc, "PT4", [128, 512], BF16)[:], T(pc, "PT5", [128, 512], BF16)[:]]
                dh = T(pc, "dh", [128, 512], BF16)
                dl = T(pc, "dl", [128, 512], BF16)
                ps = [PS(pc, "ps%d" % k, [128, 512], F32) for k in range(8)]
                S.dma(lambda e: e.dma_start(out=B0b[:, 0:8192], in_=masks_d), 'masks', w=['masks'])
                S.pool(lambda e: e.memset(V[:, :, 64:65], 1.0), w=['Vones'])
                for ch in range(4):
                    cc = slice(ch * 512, (ch + 1) * 512)
                    rope_tables(ch, pos_own[0:1, cc], 512, posi, posf, ang, kf, sinq[:, cc], cosq[:, cc], 'C', 'C%d' % ch)
                tabr = ['C%dsn' % ch for ch in range(4)] + ['C%dcs' % ch for ch in range(4)]
                scale = float(96 ** -0.5)
                sl = slice(64, 96)
                t1, t2 = posf, ang
                cnt = 0
                for h in range(8):
                    for st in range(16):
                        cols = slice(st * 512, (st + 1) * 512)
                        S.pe(lambda e, h=h, st=st, cols=cols: e.matmul(ps[st % 2][0:64, :], lhsT=Wkv[:, h * 128:h * 128 + 64], rhs=cnT[:, cols], start=True, stop=True),
                             r=['Wkv', ('cnT', st)], w=[('ps', st % 2)])
                        if st % 2 == 0:
                            S.act(lambda e, st=st, cols=cols: e.copy(out=KT[0:64, cols], in_=ps[st % 2][0:64, :]), r=[('ps', st % 2)], w=[('KTn', st)])
                        else:
                            S.dve(lambda e, st=st, cols=cols: e.tensor_copy(out=KT[0:64, cols], in_=ps[st % 2][0:64, :]), r=[('ps', st % 2)], w=[('KTn', st)])
                    for n8 in range(8):
                        for k in range(8):
                            n = n8 * 8 + k
                            S.pe(lambda e, h=h, n=n, k=k, n8=n8: e.matmul(ps[n8 % 2][:, k * 64:(k + 1) * 64], lhsT=cnT[:, n * 128:(n + 1) * 128],
                                                                           rhs=Wkv[:, h * 128 + 64:h * 128 + 128], start=True, stop=True),
                                 r=['Wkv', ('cnT', n // 4)], w=[('ps', n8 % 2)])
                        if n8 % 2 == 0:
                            S.dve(lambda e, n8=n8: e.tensor_copy(out=V[:, n8 * 8:(n8 + 1) * 8, 0:64], in_=ps[n8 % 2][:].rearrange("p (a b) -> p a b", a=8)),
                                  r=[('ps', n8 % 2)], w=[('V', n8)])
                        else:
                            S.act(lambda e, n8=n8: e.copy(out=V[:, n8 * 8:(n8 + 1) * 8, 0:64], in_=ps[n8 % 2][:].rearrange("p (a b) -> p a b", a=8)),
                                  r=[('ps', n8 % 2)], w=[('V', n8)])
                    for qg in range(4):
                        cols = slice(qg * 512, (qg + 1) * 512)
                        for c in range(2):
                            S.pe(lambda e, h=h, c=c, cols=cols: e.matmul(ps[0][0:96, :], lhsT=Wq[:, c, h * 96:(h + 1) * 96], rhs=qnT[:, c, cols], start=(c == 0), stop=(c == 1)),
                                 r=['Wq', 'qnT'], w=[('ps', 0)])
                        for c in range(2):
                            S.pe(lambda e, h=h, c=c, cols=cols: e.matmul(ps[1][0:96, :], lhsT=Wqrot[:, c, h * 96:(h + 1) * 96], rhs=qnT[:, c, cols], start=(c == 0), stop=(c == 1)),
                                 r=['Wqrot', 'qnT'], w=[('ps', 1)])
                        S.act(lambda e, cols=cols: e.copy(out=QT[0:64, cols], in_=ps[0][0:64, :]), r=[('ps', 0)], w=[('QT', qg)])
                        S.dve(lambda e, cols=cols: e.tensor_tensor(out=t1[sl, :], in0=ps[0][sl, :], in1=cosq[sl, cols], op=ALU.mult), r=[('ps', 0)] + tabr, w=['t1'])
                        S.dve(lambda e, cols=cols: e.tensor_tensor(out=t2[sl, :], in0=ps[1][sl, :], in1=sinq[sl, cols], op=ALU.mult), r=[('ps', 1)] + tabr, w=['t2'])
                        S.dve(lambda e, cols=cols: e.tensor_tensor(out=QT[sl, cols], in0=t1[sl, :], in1=t2[sl, :], op=ALU.add), r=['t1', 't2', ('QT', qg)], w=[('QT', qg)])
                    for qg in range(4):
                        qc0 = qg * 512
                        qcols = slice(qg * 512, (qg + 1) * 512)
                        nkb = 16 * qg + 16
                        base = cnt
                        order = [16 * qg] + [16 * qg + m for m in range(4, 16)] + list(range(16 * qg)) + [16 * qg + 1, 16 * qg + 2, 16 * qg + 3]
                        LA = 4

                        def c0_of(kb, qg=qg):
                            m = kb - 16 * qg
                            return 0 if m < 0 else (m // 4) * 128

                        def qk(ui, base=base, qc0=qc0, qg=qg, order=order):
                            kb = order[ui]
                            c0 = c0_of(kb)
                            b_ = (base + ui) % 5
                            S.pe(lambda e, kb=kb, b_=b_, c0=c0: e.matmul(ps[b_][:, c0:512], lhsT=KT[0:96, kb * 128:(kb + 1) * 128], rhs=QT[0:96, qc0 + c0:qc0 + 512],
                                                                          start=True, stop=True),
                                 r=[('KTn', kb // 4), ('KTr', kb // 4), ('QT', qg)], w=[('ps', b_)])

                        def pv(ui, base=base, qg=qg, nkb=nkb, order=order):
                            kb = order[ui]
                            c0 = c0_of(kb)
                            b_ = (base + ui) % 5
                            pt = (base + ui) % 6
                            S.act(lambda e, b_=b_, pt=pt, c0=c0: e.activation(out=PT[pt][:, c0:512], in_=ps[b_][:, c0:512], func=AF.Exp, scale=scale), r=[('ps', b_)], w=[('PT', pt)])
                            if kb >= 16 * qg:
                                m = kb - 16 * qg
                                S.dve(lambda e, pt=pt, m=m, c0=c0: e.tensor_tensor(out=PT[pt][:, c0:512], in0=PT[pt][:, c0:512], in1=masks[:, m, c0:512], op=ALU.mult),
                                      r=[('PT', pt), 'masks'], w=[('PT', pt)])
                            S.pe(lambda e, kb=kb, pt=pt, c0=c0, ui=ui: e.matmul(ps[5][0:65, c0:512], lhsT=V[:, kb, 0:65], rhs=PT[pt][:, c0:512], start=(ui == 0), stop=(ui == nkb - 1)),
                                 r=[('V', kb // 8), 'Vones', ('PT', pt)], w=[('ps', 5)])

                        for ui in range(min(LA, nkb)):
                            qk(ui)
                        for ui in range(nkb):
                            pv(ui)
                            if ui + LA < nkb:
                                qk(ui + LA)
                        cnt += nkb
                        S.act(lambda e: e.copy(out=Osb[0:65, :], in_=ps[5][0:65, :]), r=[('ps', 5)], w=['Osb'])
                        S.dve(lambda e: e.tensor_copy(out=dh[64:65, :], in_=Osb[64:65, :]), r=['Osb'], w=['dh'])
                        S.dve(lambda e: e.tensor_tensor(out=dl[64:65, :], in0=Osb[64:65, :], in1=dh[64:65, :], op=ALU.subtract), r=['Osb', 'dh'], w=['dl'])
                        S.pe(lambda e: e.matmul(ps[6][0:64, :], lhsT=ones_b[64:65, 0:64], rhs=dh[64:65, :], start=True, stop=False), r=['dh', 'ones_b'], w=[('ps', 6)])
                        S.pe(lambda e: e.matmul(ps[6][0:64, :], lhsT=ones_b[64:65, 0:64], rhs=dl[64:65, :], start=False, stop=True), r=['dl', 'ones_b'], w=[('ps', 6)])
                        S.dve(lambda e: e.reciprocal(out=rec[0:64, :], in_=ps[6][0:64, :]), r=[('ps', 6)], w=['rec'])
                        S.dve(lambda e: e.tensor_tensor(out=ytmp[0:64, :], in0=Osb[0:64, :], in1=rec[0:64, :], op=ALU.mult), r=['Osb', 'rec'], w=['ytmp'])
                        S.dve(lambda e, h=h, qcols=qcols: e.tensor_scalar(out=yattnT[0:64, h, qcols], in0=ytmp[0:64, :], scalar1=g_oa[:, h:h + 1], scalar2=None, op0=ALU.mult),
                              r=['ytmp', 'g_oa'], w=[('yattnT', h, qg)])
                        S.act(lambda e: e.activation(out=ysqa[:], in_=ytmp[0:64, :], func=AF.Square), r=['ytmp'], w=['ysqa'])
                        for t4 in range(4):
                            S.pe(lambda e, t4=t4: e.matmul(ps[7][:, t4:t4 + 1], lhsT=ysqa[:, t4 * 128:(t4 + 1) * 128], rhs=ones_b[0:64, 0:1], start=True, stop=True),
                                 r=['ysqa', 'ones_b'], w=[('ps', 7)])
                        if h == 0:
                            S.dve(lambda e, qg=qg: e.tensor_copy(out=ssa[:, qg * 4:(qg + 1) * 4], in_=ps[7][:, 0:4]), r=[('ps', 7)], w=[('ssa', qg)])
                        else:
                            S.dve(lambda e, qg=qg: e.tensor_tensor(out=ssa[:, qg * 4:(qg + 1) * 4], in0=ssa[:, qg * 4:(qg + 1) * 4], in1=ps[7][:, 0:4], op=ALU.add),
                                  r=[('ps', 7), ('ssa', qg)], w=[('ssa', qg)])
                S.act(lambda e: e.activation(out=rstda[:], in_=ssa[:], func=AF.Ln, bias=epsc[:, 0:1], scale=1.0 / 512),
                      r=[('ssa', q) for q in range(4)] + ['epsc'], w=['rstda'])
                S.act(lambda e: e.activation(out=rstda[:], in_=rstda[:], func=AF.Exp, scale=-0.5), r=['rstda'], w=['rstda'])
                if debug:
                    S.dma(lambda e: e.dma_start(out=dbg_ya, in_=B2[0:64, 8192:24576]), 'dbg2', r=[('yattnT', h, q) for h in range(8) for q in range(4)])
                    S.dma(lambda e: e.dma_start(out=dbg_kt, in_=KT[0:96, :]), 'dbg3', r=[('KTn', s) for s in range(16)] + [('KTr', s) for s in range(16)])
                    S.dma(lambda e: e.dma_start(out=dbg_rs[:, 0:16], in_=rstdp[:]), 'dbg4', r=['rstdp'])
                    S.dma(lambda e: e.dma_start(out=dbg_rs[:, 16:32], in_=rstda[:]), 'dbg4', r=['rstda'])
                    final_keys.extend(['dbg2', 'dbg3', 'dbg4'])
                S.flush()
                if upto == 'C':
                    S.enabled = False

        xn = B0.rearrange("p (i d) -> p i d", i=16)
        h2T = B1.rearrange("p (c t) -> p c t", c=8)

        with ExitStack() as pd:
            Wop = T(pd, "Wop", [128, 4, 1024], BF16)
            Woa = T(pd, "Woa", [64, 8, 1024], BF16)
            xo = [T(pd, "xo0", [128, 1024], F32), T(pd, "xo1", [128, 1024], F32)]
            pP = [PS(pd, "pP0", [128, 512], F32), PS(pd, "pP1", [128, 512], F32)]
            pA = [PS(pd, "pA0", [128, 512], F32), PS(pd, "pA1", [128, 512], F32)]
            S.dma(lambda e: e.dma_start(out=Wop[:], in_=w_out[0:512, :].rearrange("(g p) n -> p g n", p=128)), 'wop', w=['Wop'], q='pool')
            S.dma(lambda e: e.dma_start(out=Woa[:], in_=w_out[512:1024, :].rearrange("(h p) n -> p h n", p=64)), 'woa', w=['Woa'], q='pool')
            for i in range(16):
                tcols = slice(i * 128, (i + 1) * 128)
                S.dma(lambda e, i=i: e.dma_start(out=xo[i % 2][:], in_=x_own[144 * i + 16:144 * i + 144, :]), 'xo%d' % (i % 2), w=[('xo', i % 2)])
                for half in range(2):
                    hc = slice(half * 512, (half + 1) * 512)
                    for g in range(4):
                        S.pe(lambda e, g=g, half=half, tcols=tcols, hc=hc: e.matmul(pP[half][:], lhsT=ypoolT[:, g, tcols], rhs=Wop[:, g, hc], start=(g == 0), stop=(g == 3)),
                             r=['Wop'], w=[('pP', half)])
                    for h in range(8):
                        S.pe(lambda e, h=h, half=half, tcols=tcols, hc=hc: e.matmul(pA[half][:], lhsT=yattnT[0:64, h, tcols], rhs=Woa[:, h, hc], start=(h == 0), stop=(h == 7)),
                             r=['Woa'], w=[('pA', half)])
                    S.dve(lambda e, i=i, half=half, hc=hc: e.scalar_tensor_tensor(out=xn[:, i, hc], in0=pP[half][:], scalar=rstdp[:, i:i + 1], in1=xo[i % 2][:, hc],
                                                                                 op0=ALU.mult, op1=ALU.add),
                          r=[('pP', half), ('xo', i % 2)], w=[('xn', i, half)])
                    S.dve(lambda e, i=i, half=half, hc=hc: e.scalar_tensor_tensor(out=xn[:, i, hc], in0=pA[half][:], scalar=rstda[:, i:i + 1], in1=xn[:, i, hc],
                                                                                 op0=ALU.mult, op1=ALU.add),
                          r=[('pA', half), ('xn', i, half)], w=[('xn', i, half)])
                if debug:
                    S.dma(lambda e, i=i: e.dma_start(out=dbg_x1[i * 128:(i + 1) * 128, :], in_=xn[:, i, :]), 'dbg5', r=[('xn', i, 0), ('xn', i, 1)])
            if debug:
                final_keys.append('dbg5')
            S.flush()
            if upto == 'D':
                S.enabled = False

        Wgu = [B1.rearrange("p (c n) -> p c n", c=8), B2[:, 0:16384].rearrange("p (c n) -> p c n", c=8)]
        Wd = B2[:, 16384:24576].rearrange("p (c n) -> p c n", c=8)

        def load_gu(e_):
            S.dma(lambda e, e_=e_: e.dma_start(out=Wgu[e_ % 2], in_=w_gu_p[e_ // 8][e_ % 8].rearrange("(c p) n -> p c n", p=128)),
                  'wgu%d' % (e_ % 2), w=[('Wgu', e_ % 2)], q='pool')

        def load_d(e_):
            S.dma(lambda e, e_=e_: e.dma_start(out=Wd, in_=w_d_p[e_ // 8][e_ % 8].rearrange("(c p) n -> p c n", p=128)), 'wd', w=['Wd'], q='pool')

        S.no_barrier_keys.update(['wgu0', 'wgu1', 'wd'])

        with ExitStack() as pe_:
            gffn = T(pe_, "gffn", [128, 1024], F32)
            wrf = T(pe_, "wrf", [128, 8, 32], F32)
            brb = T(pe_, "brb", [128, 32], F32)
            h2tok = T(pe_, "h2tok", [128, 1024], F32)
            hib = T(pe_, "hib", [128, 1024], BF16)
            lob = T(pe_, "lob", [128, 1024], BF16)
            loT = T(pe_, "loT", [128, 8, 128], BF16)
            whi = T(pe_, "whi", [128, 8, 32], BF16)
            wlo = T(pe_, "wlo", [128, 8, 32], BF16)
            ss2 = T(pe_, "ss2", [128, 16], F32)
            lg = T(pe_, "lg", [128, 32], F32)
            top8 = T(pe_, "top8", [128, 8], F32)
            msk = T(pe_, "msk", [128, 32], F32)
            ex = T(pe_, "ex", [128, 32], F32)
            nm = T(pe_, "nm", [128, 1], F32)
            den = T(pe_, "den", [128, 1], F32)
            ptr = [PS(pe_, "ptr0", [128, 8, 128], BF16), PS(pe_, "ptr1", [128, 8, 128], BF16)]
            plg = PS(pe_, "plg", [128, 512], F32)
            plb = [PS(pe_, "plb0", [128, 512], F32), PS(pe_, "plb1", [128, 512], F32)]
            bdb = T(pe_, "bdb", [32, 1024], BF16)
            gb = T(pe_, "gb", [128, 32], BF16)
            gT = T(pe_, "gT", [32, 128], BF16)
            hiT = T(pe_, "hiT", [128, 8, 128], BF16)
            hibs = [hib, T(pe_, "hib1", [128, 1024], BF16)]
            mb = T(pe_, "mb", [128, 32], BF16)
            dtab = T(pe_, "dtab", [128, 32], F32)
            ov = T(pe_, "ov", [128, 32], F32)
            oh = T(pe_, "oh", [128, 32], F32)
            tmp32 = T(pe_, "tmp32", [128, 32], F32)
            destf = T(pe_, "destf", [128, 4], F32)
            okf = T(pe_, "okf", [128, 4], F32)
            ppx = PS(pe_, "ppx", [128, 512], F32)
            S.dma(lambda e: e.dma_start(out=bdb[:], in_=b_d_d), 'e3', w=['bdb'], q='pool')
            if stage >= 2:
                load_gu(0)
                load_gu(1)
                load_d(0)
            S.dma(lambda e: e.dma_start(out=gffn[:], in_=g_ffn_d.partition_broadcast(128)), 'e0', w=['gffn'])
            S.dma(lambda e: e.dma_start(out=wrf[:], in_=w_r.rearrange("(c p) n -> p c n", p=128)), 'e1', w=['wrf'])
            S.dma(lambda e: e.dma_start(out=brb[:], in_=b_r.partition_broadcast(128)), 'e2', w=['brb'])
            S.dve(lambda e: e.tensor_copy(out=whi[:], in_=wrf[:]), r=['wrf'], w=['whi'])
            S.dve(lambda e: e.tensor_tensor(out=wlo[:], in0=wrf[:], in1=whi[:], op=ALU.subtract), r=['wrf', 'whi'], w=['wlo'])
            for i in range(16):
                S.act(lambda e, i=i: e.activation(out=junk[:], in_=xn[:, i, :], func=AF.Square, accum_out=ss2[:, i:i + 1]),
                      r=[('xn', i, 0), ('xn', i, 1)], w=['junk', ('ss2q', i)])
            S.act(lambda e: e.activation(out=ss2[:], in_=ss2[:], func=AF.Ln, bias=epsc[:, 0:1], scale=1.0 / 1024),
                  r=[('ss2q', i) for i in range(16)] + ['epsc'], w=['ss2all'])
            S.act(lambda e: e.activation(out=ss2[:], in_=ss2[:], func=AF.Exp, scale=-0.5), r=['ss2all'], w=['ss2all'])
            for i in range(16):
                xt = [('xn', i, 0), ('xn', i, 1)]
                tc_ = slice(i * 128, (i + 1) * 128)
                S.dve(lambda e, i=i: e.scalar_tensor_tensor(out=h2tok[:], in0=xn[:, i, :], scalar=ss2[:, i:i + 1], in1=gffn[:], op0=ALU.mult, op1=ALU.mult),
                      r=xt + ['ss2all', 'gffn'], w=['h2tok'])
                hib = hibs[i % 2]
                hbt = ('hib', i % 2)
                S.dve(lambda e, hib=hib: e.tensor_copy(out=hib[:], in_=h2tok[:]), r=['h2tok'], w=[hbt])
                S.dve(lambda e, hib=hib: e.tensor_tensor(out=lob[:], in0=h2tok[:], in1=hib[:], op=ALU.subtract), r=['h2tok', hbt], w=['lob'])
                for c in range(8):
                    S.pe(lambda e, c=c, hib=hib: e.transpose(out=ptr[0][:, c, :], in_=hib[:, c * 128:(c + 1) * 128], identity=ident_b[:]), r=[hbt, 'ident_b'], w=[('ptr', 0)])
                for c in range(8):
                    S.pe(lambda e, c=c: e.transpose(out=ptr[1][:, c, :], in_=lob[:, c * 128:(c + 1) * 128], identity=ident_b[:]), r=['lob', 'ident_b'], w=[('ptr', 1)])
                S.act(lambda e: e.copy(out=hiT[:], in_=ptr[0][:]), r=[('ptr', 0)], w=['hiT'])
                S.dve(lambda e: e.tensor_copy(out=loT[:], in_=ptr[1][:]), r=[('ptr', 1)], w=['loT'])
                k = 0
                for c in range(8):
                    for (lt, ltag, rt, rtag) in ((hiT[:, c, :], 'hiT', whi, 'whi'), (hiT[:, c, :], 'hiT', wlo, 'wlo'), (loT[:, c, :], 'loT', whi, 'whi')):
                        S.pe(lambda e, lt=lt, rt=rt, c=c, k=k: e.matmul(plg[:, 0:32], lhsT=lt, rhs=rt[:, c, :], start=(k == 0), stop=(k == 23)),
                             r=[ltag, rtag], w=['plg'])
                        k += 1
                S.dve(lambda e: e.tensor_tensor(out=lg[:], in0=plg[:, 0:32], in1=brb[:], op=ALU.add), r=['plg', 'brb'], w=['lg'])
                S.dve(lambda e: e.max(out=top8[:], in_=lg[:]), r=['lg'], w=['top8'])
                S.dve(lambda e: e.tensor_scalar(out=msk[:], in0=lg[:], scalar1=top8[:, 3:4], scalar2=None, op0=ALU.is_ge), r=['lg', 'top8'], w=['msk'])
                S.dve(lambda e: e.tensor_scalar(out=nm[:], in0=top8[:, 0:1], scalar1=-1.0, scalar2=None, op0=ALU.mult), r=['top8'], w=['nm'])
                S.act(lambda e: e.activation(out=ex[:], in_=lg[:], func=AF.Exp, bias=nm[:, 0:1], scale=1.0), r=['lg', 'nm'], w=['ex'])
                S.dve(lambda e: e.tensor_tensor(out=ex[:], in0=ex[:], in1=msk[:], op=ALU.mult), r=['ex', 'msk'], w=['ex'])
                S.dve(lambda e: e.reduce_sum(out=den[:], in_=ex[:], axis=mybir.AxisListType.X), r=['ex'], w=['den'])
                S.dve(lambda e: e.reciprocal(out=den[:], in_=den[:]), r=['den'], w=['den'])
                S.dve(lambda e, i=i: e.tensor_scalar(out=gates[:, i * 32:(i + 1) * 32], in0=ex[:], scalar1=den[:, 0:1], scalar2=None, op0=ALU.mult),
                      r=['ex', 'den'], w=[('gates', i)])
                if stage >= 2:
                    S.dve(lambda e: e.tensor_copy(out=mb[:], in_=msk[:]), r=['msk'], w=['mb'])
                    S.pe(lambda e: e.matmul(ppx[:, 0:32], lhsT=Lst[:], rhs=mb[:], start=True, stop=True), r=['Lst', 'mb'], w=['ppx'])
                    S.pe(lambda e: e.matmul(ppx[:, 32:64], lhsT=ones_b[:], rhs=mb[:], start=True, stop=True), r=['ones_b', 'mb'], w=['ppx'])
                    S.dve(lambda e: e.tensor_tensor(out=dtab[:], in0=ppx[:, 0:32], in1=carry[:], op=ALU.add), r=['ppx', 'carry'], w=['dtab'])
                    S.dve(lambda e: e.tensor_tensor(out=carry[:], in0=carry[:], in1=ppx[:, 32:64], op=ALU.add), r=['ppx', 'carry'], w=['carry'])
                    S.dve(lambda e: e.tensor_scalar(out=ov[:], in0=dtab[:], scalar1=float(CAP), scalar2=BIGV, op0=ALU.is_ge, op1=ALU.mult), r=['dtab'], w=['ov'])
                    S.dve(lambda e: e.tensor_tensor(out=dtab[:], in0=dtab[:], in1=CE[:], op=ALU.add), r=['dtab', 'CE'], w=['dtab'])
                    S.dve(lambda e: e.tensor_tensor(out=dtab[:], in0=dtab[:], in1=ov[:], op=ALU.add), r=['dtab', 'ov'], w=['dtab'])
                    for k in range(4):
                        S.dve(lambda e, k=k: e.tensor_scalar(out=oh[:], in0=lg[:], scalar1=top8[:, k:k + 1], scalar2=None, op0=ALU.is_equal), r=['lg', 'top8'], w=['oh'])
                        S.dve(lambda e, k=k: e.scalar_tensor_tensor(out=tmp32[:], in0=oh[:], scalar=1.0, in1=dtab[:], op0=ALU.mult, op1=ALU.mult, accum_out=destf[:, k:k + 1]),
                              r=['oh', 'dtab'], w=['tmp32', 'destf'])
                        S.dve(lambda e, k=k, i=i: e.scalar_tensor_tensor(out=tmp32[:], in0=oh[:], scalar=1.0, in1=gates[:, i * 32:(i + 1) * 32], op0=ALU.mult, op1=ALU.mult,
                                                                       accum_out=gk[:, 4 * i + k:4 * i + k + 1]),
                              r=['oh', ('gates', i), 'gk0'], w=['tmp32', ('gk', i)])
                    S.dve(lambda e, i=i: e.tensor_copy(out=desti[:, 4 * i:4 * i + 4], in_=destf[:]), r=['destf'], w=[('desti', i)])
                    S.dve(lambda e: e.tensor_scalar(out=okf[:], in0=destf[:], scalar1=float(NROWS), scalar2=None, op0=ALU.is_lt), r=['destf'], w=['okf'])
                    S.dve(lambda e, i=i: e.tensor_tensor(out=gk[:, 4 * i:4 * i + 4], in0=gk[:, 4 * i:4 * i + 4], in1=okf[:], op=ALU.mult), r=['okf', ('gk', i)], w=[('gk', i)])
                    for k in range(4):
                        S.dma(lambda e, k=k, i=i, hib=hib: e.indirect_dma_start(
                            out=xs[:, :], out_offset=bass.IndirectOffsetOnAxis(ap=desti[:, 4 * i + k:4 * i + k + 1], axis=0),
                            in_=hib[:, :], in_offset=None, bounds_check=S.reg(e, NROWS - 1), oob_is_err=False),
                            'sc%d' % (i % 2), r=[hbt, ('desti', i)], q='pool')
                S.dve(lambda e, i=i: e.tensor_copy(out=gb[:], in_=gates[:, i * 32:(i + 1) * 32]), r=[('gates', i)], w=['gb'])
                S.pe(lambda e: e.transpose(out=ptr[1][0:32, 0, :], in_=gb[:], identity=ident_b[:]), r=['gb', 'ident_b'], w=[('ptr', 1)])
                S.act(lambda e: e.copy(out=gT[:], in_=ptr[1][0:32, 0, :]), r=[('ptr', 1)], w=['gT'])
                for half in range(2):
                    hc = slice(half * 512, (half + 1) * 512)
                    S.pe(lambda e, half=half, hc=hc: e.matmul(plb[half][:], lhsT=gT[:], rhs=bdb[:, hc], start=True, stop=True), r=['gT', 'bdb'], w=[('plb', half)])
                    S.dve(lambda e, i=i, half=half, hc=hc: e.tensor_tensor(out=xn[:, i, hc], in0=xn[:, i, hc], in1=plb[half][:], op=ALU.add),
                          r=[('plb', half), ('xn', i, half)], w=[('xn', i, half)])
            if debug:
                S.dma(lambda e: e.dma_start(out=dbg_gates, in_=gates[:]), 'dbg6', r=[('gates', i) for i in range(16)])
                final_keys.append('dbg6')
            S.flush()
            if upto == 'E':
                S.enabled = False

        if stage >= 2:
            with ExitStack() as pf:
                KC = CAP // 128
                bgu = T(pf, "bgu", [128, 512], F32)
                xg = [T(pf, "xg0", [128, KC, 1024], BF16), T(pf, "xg1", [128, KC, 1024], BF16)]
                selT = [T(pf, "selT0", [128, 8, CAP], BF16), T(pf, "selT1", [128, 8, CAP], BF16)]
                actT = T(pf, "actT", [128, 8, CAP], BF16)
                yo = T(pf, "yo", [128, KC, 1024], F32)
                g1 = T(pf, "g1", [128, CAP], F32)
                sg = T(pf, "sg", [128, CAP], F32)
                ur = T(pf, "ur", [128, CAP], F32)
                tt = T(pf, "tt", [128, CAP], F32)
                pTf = [PS(pf, "pTf0", [128, 512], F32), PS(pf, "pTf1", [128, 512], F32)]
                pG = [PS(pf, "pG0", [128, 512], F32), PS(pf, "pG1", [128, 512], F32)]
                pUp = [PS(pf, "pUp0", [128, 512], F32), PS(pf, "pUp1", [128, 512], F32)]
                pY = [PS(pf, "pY0", [128, 512], F32), PS(pf, "pY1", [128, 512], F32)]
                S.dma(lambda e: e.dma_start(out=bgu[:], in_=b_gu_d), 'f0', w=['bgu'])

                def load_x(e_):
                    S.dma(lambda e, e_=e_: e.dma_start(out=xg[e_ % 2][:], in_=xs[e_ * CAP:(e_ + 1) * CAP, :].rearrange("(k p) d -> p k d", p=128)),
                          'xg%d' % (e_ % 2), w=[('xg', e_ % 2)])

                load_x(0)
                blk = 0
                for ex_ in range(32):
                    sl_ = ex_ % 2
                    if ex_ + 1 < 32:
                        load_x(ex_ + 1)
                    for k in range(KC):
                        for hh in range(2):
                            pb2 = (2 * k + hh) % 2
                            for c4 in range(4):
                                c = hh * 4 + c4
                                S.pe(lambda e, k=k, c=c, c4=c4, sl_=sl_, pb2=pb2: e.matmul(pTf[pb2][:, c4 * 128:(c4 + 1) * 128], lhsT=xg[sl_][:, k, c * 128:(c + 1) * 128],
                                                                                     rhs=ident_b[:], start=True, stop=True),
                                     r=[('xg', sl_), 'ident_b'], w=[('pTf', pb2)])
                            if hh == 0:
                                S.act(lambda e, k=k, hh=hh, sl_=sl_, pb2=pb2: e.copy(out=selT[sl_][:, hh * 4:(hh + 1) * 4, k * 128:(k + 1) * 128],
                                                                                  in_=pTf[pb2][:].rearrange("p (a b) -> p a b", a=4)),
                                      r=[('pTf', pb2)], w=[('selT', sl_, k, hh)])
                            else:
                                S.dve(lambda e, k=k, hh=hh, sl_=sl_, pb2=pb2: e.tensor_copy(out=selT[sl_][:, hh * 4:(hh + 1) * 4, k * 128:(k + 1) * 128],
                                                                                         in_=pTf[pb2][:].rearrange("p (a b) -> p a b", a=4)),
                                      r=[('pTf', pb2)], w=[('selT', sl_, k, hh)])
                    sr = [('selT', sl_, k, hh) for k in range(KC) for hh in range(2)]
                    W = Wgu[sl_]
                    for fc in range(8):
                        pb_ = blk % 2
                        blk += 1
                        for c in range(8):
                            S.pe(lambda e, fc=fc, c=c, pb_=pb_, W=W, sl_=sl_: e.matmul(pG[pb_][:, 0:CAP], lhsT=W[:, c, fc * 128:(fc + 1) * 128], rhs=selT[sl_][:, c, :],
                                                                                 start=(c == 0), stop=(c == 7)),
                                 r=sr + [('Wgu', sl_)], w=[('pG', pb_)])
                        for c in range(8):
                            S.pe(lambda e, fc=fc, c=c, pb_=pb_, W=W, sl_=sl_: e.matmul(pUp[pb_][:, 0:CAP], lhsT=W[:, c, 1024 + fc * 128:1024 + (fc + 1) * 128], rhs=selT[sl_][:, c, :],
                                                                                 start=(c == 0), stop=(c == 7)),
                                 r=sr + [('Wgu', sl_)], w=[('pUp', pb_)])
                        bg = bgu[:, ex_ * 16 + fc:ex_ * 16 + fc + 1]
                        bu = bgu[:, ex_ * 16 + 8 + fc:ex_ * 16 + 8 + fc + 1]
                        S.dve(lambda e, pb_=pb_, bg=bg: e.tensor_scalar(out=g1[:], in0=pG[pb_][:, 0:CAP], scalar1=bg, scalar2=7.0, op0=ALU.add, op1=ALU.min),
                              r=[('pG', pb_), 'bgu'], w=['g1'])
                        S.act(lambda e: e.activation(out=sg[:], in_=g1[:], func=AF.Sigmoid, scale=1.702), r=['g1'], w=['sg'])
                        S.act(lambda e, pb_=pb_, bu=bu: e.activation(out=ur[:], in_=pUp[pb_][:, 0:CAP], func=AF.Identity, bias=bu, scale=1.0),
                              r=[('pUp', pb_), 'bgu'], w=['ur'])
                        S.dve(lambda e: e.tensor_scalar(out=ur[:], in0=ur[:], scalar1=7.0, scalar2=-7.0, op0=ALU.min, op1=ALU.max), r=['ur'], w=['ur'])
                        S.dve(lambda e: e.tensor_tensor(out=tt[:], in0=g1[:], in1=sg[:], op=ALU.mult), r=['g1', 'sg'], w=['tt'])
                        S.dve(lambda e, fc=fc: e.scalar_tensor_tensor(out=actT[:, fc, :], in0=ur[:], scalar=1.0, in1=tt[:], op0=ALU.add, op1=ALU.mult),
                              r=['ur', 'tt'], w=[('actT', fc)])
                    if ex_ + 2 < 32:
                        load_gu(ex_ + 2)
                    for k in range(KC):
                        for half in range(2):
                            hc = slice(half * 512, (half + 1) * 512)
                            for fc in range(8):
                                S.pe(lambda e, fc=fc, k=k, half=half, hc=hc: e.matmul(pY[half][:], lhsT=actT[:, fc, k * 128:(k + 1) * 128], rhs=Wd[:, fc, hc],
                                                                                 start=(fc == 0), stop=(fc == 7)),
                                     r=[('actT', fc), 'Wd'], w=[('pY', half)])
                            if half == 0:
                                S.act(lambda e, k=k, half=half, hc=hc: e.copy(out=yo[:, k, hc], in_=pY[half][:]), r=[('pY', half)], w=[('yo', k, half)])
                            else:
                                S.dve(lambda e, k=k, half=half, hc=hc: e.tensor_copy(out=yo[:, k, hc], in_=pY[half][:]), r=[('pY', half)], w=[('yo', k, half)])
                    if ex_ + 1 < 32:
                        load_d(ex_ + 1)
                    S.dma(lambda e, ex_=ex_: e.dma_start(out=ys[ex_ * CAP:(ex_ + 1) * CAP, :].rearrange("(k p) d -> p k d", p=128), in_=yo[:]),
                          'ys', r=[('yo', k, h_) for k in range(KC) for h_ in range(2)])
                S.flush()
                if upto == 'F':
                    S.enabled = False
            with ExitStack() as pf2:
                NG = 8
                gbf = BIG2[:].bitcast(F32)
                gbuf = [gbf[:, k * 1024:(k + 1) * 1024] for k in range(NG)]
                S.pool(lambda e: e.memset(gbf[:, 0:NG * 1024], 0.0), w=[('gbuf', k) for k in range(NG)])
                for i in range(16):
                    for k in range(4):
                        j = 4 * i + k
                        gs = j % NG
                        S.dma(lambda e, j=j, gs=gs: e.indirect_dma_start(
                            out=gbuf[gs], out_offset=None, in_=ys[:, :],
                            in_offset=bass.IndirectOffsetOnAxis(ap=desti[:, j:j + 1], axis=0), bounds_check=S.reg(e, NROWS - 1), oob_is_err=False),
                            'ga%d' % gs, w=[('gbuf', gs)], q='pool')
                        S.dve(lambda e, i=i, j=j, gs=gs: e.scalar_tensor_tensor(out=xn[:, i, :], in0=gbuf[gs], scalar=gk[:, j:j + 1], in1=xn[:, i, :], op0=ALU.mult, op1=ALU.add),
                              r=[('gbuf', gs), ('xn', i, 0), ('xn', i, 1)], w=[('xn', i, 0), ('xn', i, 1)])
                S.flush()

        S.enabled = True
        with ExitStack() as pg:
            gfin = T(pg, "gfin", [128, 1024], F32)
            ot = [T(pg, "ot0", [128, 1024], F32), T(pg, "ot1", [128, 1024], F32)]
            ss3 = T(pg, "ss3", [128, 16], F32)
            S.dma(lambda e: e.dma_start(out=gfin[:], in_=g_fin_d.partition_broadcast(128)), 'g0', w=['gfin'])
            for i in range(16):
                S.act(lambda e, i=i: e.activation(out=junk[:], in_=xn[:, i, :], func=AF.Square, accum_out=ss3[:, i:i + 1]),
                      r=[('xn', i, 0), ('xn', i, 1)], w=['junk', ('ss3q', i)])
            S.act(lambda e: e.activation(out=ss3[:], in_=ss3[:], func=AF.Ln, bias=epsc[:, 0:1], scale=1.0 / 1024),
                  r=[('ss3q', i) for i in range(16)] + ['epsc'], w=['ss3all'])
            S.act(lambda e: e.activation(out=ss3[:], in_=ss3[:], func=AF.Exp, scale=-0.5), r=['ss3all'], w=['ss3all'])
            for i in range(16):
                xt = [('xn', i, 0), ('xn', i, 1)]
                S.dve(lambda e, i=i: e.scalar_tensor_tensor(out=ot[i % 2][:], in0=xn[:, i, :], scalar=ss3[:, i:i + 1], in1=gfin[:], op0=ALU.mult, op1=ALU.mult),
                      r=xt + ['ss3all', 'gfin'], w=[('ot', i % 2)])
                S.dma(lambda e, i=i: e.dma_start(out=y[i * 128:(i + 1) * 128, :], in_=ot[i % 2][:]), 'yo%d' % (i % 2), r=[('ot', i % 2)])
            final_keys.extend(['yo0', 'yo1'])
            S.flush(final_dma_keys=final_keys)
    return nc


def make_in_maps(stage, x, positions, g_attn_norm, w_in, w_pool, b_pool, pool_scale, g_q_a, w_q_b,
                 g_kv_a, w_kv_b, g_out_pool, g_out_attn, w_out, g_ffn_norm, w_router, b_router,
                 w_gate_up, b_gate_up, w_down, b_down, g_final):
    f32 = lambda a: np.ascontiguousarray(np.asarray(a, dtype=np.float32))
    x = f32(x)
    positions = np.asarray(positions, dtype=np.int32)
    inv = (10000.0 ** (-np.arange(16, dtype=np.float32) / 16)).astype(np.float32)
    invf = np.zeros((128, 1), np.float32)
    invf[64:80, 0] = inv
    invf[80:96, 0] = inv
    shared = dict(
        invf=invf,
        g_attn=f32(np.asarray(g_attn_norm)[0].reshape(8, 128).T),
        w_in=f32(np.asarray(w_in)[0]),
        w_pool=f32(np.asarray(w_pool)[0].transpose(1, 0, 2).reshape(128, 512)),
        b_pool=f32(np.asarray(b_pool)[0].T),
        pool_scale=f32(np.asarray(pool_scale)[0].reshape(4, 128).T),
        g_q=f32(np.asarray(g_q_a)[0].reshape(2, 128).T),
        w_q_b=f32(np.asarray(w_q_b)[0]),
        g_kv=f32(np.asarray(g_kv_a)[0].reshape(128, 1)),
        w_kv_b=f32(np.asarray(w_kv_b)[0]),
        g_op=f32(np.asarray(g_out_pool)[0].reshape(4, 128).T),
        g_oa=f32(np.asarray(g_out_attn)[0].reshape(8, 64).T),
        w_out=f32(np.asarray(w_out)[0]),
        g_ffn=f32(np.asarray(g_ffn_norm)[0].reshape(1, 1024)),
        g_fin=f32(np.asarray(g_final).reshape(1, 1024)),
        w_r=f32(np.asarray(w_router)[0]),
        b_r=f32(np.asarray(b_router)[0].reshape(1, 32)),
        b_gu=f32(np.asarray(b_gate_up)[0].reshape(32, 16, 128).transpose(2, 0, 1).reshape(128, 512)),
        b_d=f32(np.asarray(b_down)[0].reshape(32, 1024)),
    )
    wgu = f32(np.asarray(w_gate_up)[0])
    wdn = f32(np.asarray(w_down)[0])
    for k in range(4 if stage >= 2 else 0):
        shared["w_gu%d" % k] = wgu[8 * k:8 * k + 8]
        shared["w_d%d" % k] = wdn[8 * k:8 * k + 8]
    in_maps = []
    kk = np.arange(128)[:, None]
    qq = np.arange(128)[None, :]
    tri = (kk <= qq)
    for c in range(8):
        b, j = c // 4, c % 4
        xb = x[b]
        xo = np.zeros((16, 144, 1024), np.float32)
        po = np.zeros((16, 128), np.int32)
        for i in range(16):
            n = 4 * i + j
            lo = 128 * n - 16
            if lo < 0:
                xo[i, 16:] = xb[0:128]
            else:
                xo[i] = xb[lo:lo + 144]
            po[i] = positions[b, 128 * n:128 * n + 128]
        mk = np.zeros((128, 16, 4, 128), np.float32)
        for m in range(16):
            for r in range(4):
                if m < 4 * r + j:
                    mk[:, m, r, :] = 1.0
                elif m == 4 * r + j:
                    mk[:, m, r, :] = tri
        ic = np.zeros((128, 4, 16), np.float32)
        for gi, wdw in enumerate((2, 4, 8, 16)):
            if j == 0:
                ic[:, gi, :] = 1.0 / np.minimum(np.arange(16) + 1, wdw).astype(np.float32)
            else:
                ic[:, gi, :] = 1.0 / wdw
        m = dict(shared)
        m.update(
            x_all=np.ascontiguousarray(xb),
            x_own=xo.reshape(2304, 1024),
            pos_all=np.ascontiguousarray(positions[b].reshape(1, 8192)),
            pos_own=po.reshape(1, 2048),
            masks=mk.reshape(128, 8192).astype(ml_dtypes.bfloat16),
            invcnt=ic.reshape(128, 64),
        )
        in_maps.append(m)
    return in_maps


def assemble(results, key="y"):
    out = np.zeros((2, 8192, 1024), np.float32)
    for c in range(8):
        b, j = c // 4, c % 4
        yc = np.asarray(results[c][key]).reshape(16, 128, 1024)
        for i in range(16):
            n = 4 * i + j
            out[b, 128 * n:128 * n + 128] = yc[i]
    return out


def kernel(**inputs):
    nc = build()
    in_maps = make_in_maps(99, **inputs)
    res = run_bass_kernel_spmd(nc, in_maps, core_ids=list(range(8)))
    return assemble(res.results)
```

```python
import numpy as np
from contextlib import ExitStack
import ml_dtypes
import concourse.bass as bass
import concourse.mybir as mybir
from concourse.bass_utils import run_bass_kernel_spmd

F32 = mybir.dt.float32
BF16 = mybir.dt.bfloat16
I32 = mybir.dt.int32
AF = mybir.ActivationFunctionType
ALU = mybir.AluOpType

ENGS = ('pe', 'act', 'dve', 'pool', 'sp')
EPS = 1e-6
PI = float(np.pi)
TWO_PI = float(2 * np.pi)


class Op:
    __slots__ = ('eng', 'fn', 'waits', 'flag', 'count', 'dkey', 'dcount', 'done')

    def __init__(self, eng, fn):
        self.eng = eng
        self.fn = fn
        self.waits = []
        self.flag = False
        self.count = None
        self.dkey = None
        self.dcount = None
        self.done = False


class Sched:
    def __init__(self, nc, stack):
        self.nc = nc
        self.stack = stack
        self.sem = {e: stack.enter_context(nc.semaphore('s_' + e)) for e in ENGS}
        self.dsem = {}
        self.dcnt = {}
        self.ops = {e: [] for e in ENGS}
        self.base = {e: 0 for e in ENGS}
        self.lastw = {}
        self.readers = {}
        self.waited = {e: {} for e in ENGS}
        self.no_barrier_keys = set()
        self.flush_id = 0
        self._regs = {}
        self.enabled = True

    def _dsem(self, key):
        if key not in self.dsem:
            self.dsem[key] = self.stack.enter_context(self.nc.semaphore('d_' + str(key)))
            self.dcnt[key] = 0
        return self.dsem[key]

    def op(self, eng, fn, r=(), w=(), dma=None):
        if not self.enabled:
            return None
        o = Op(eng, fn)
        deps = []
        for t in r:
            d = self.lastw.get(t)
            if d is not None:
                deps.append(d)
        for t in w:
            d = self.lastw.get(t)
            if d is not None:
                deps.append(d)
            rd = self.readers.get(t)
            if rd:
                deps.extend(rd.values())
        seen = set()
        for d in deps:
            if d is o or id(d) in seen or d.done:
                continue
            seen.add(id(d))
            if d.dkey is None and d.eng == 'pe' and eng == 'pe':
                continue
            if d.dkey is None:
                d.flag = True
                o.waits.append(d)
            else:
                o.waits.append(('d', d.dkey, self.dcnt[d.dkey]))
        if dma is not None:
            self._dsem(dma)
            self.dcnt[dma] += 16
            o.dkey = dma
            o.dcount = self.dcnt[dma]
        for t in w:
            self.lastw[t] = o
            self.readers[t] = {}
        for t in r:
            rk = ('dma', o.dkey) if o.dkey is not None else o.eng
            self.readers.setdefault(t, {})[rk] = o
        self.ops[eng].append(o)
        return o

    def reg(self, eng, val):
        key = (self.flush_id, val)
        if key not in self._regs:
            self._regs[key] = eng.to_reg(val)
        return self._regs[key]

    def pe(self, fn, r=(), w=()):
        return self.op('pe', fn, r, w)

    def act(self, fn, r=(), w=()):
        return self.op('act', fn, r, w)

    def dve(self, fn, r=(), w=()):
        return self.op('dve', fn, r, w)

    def pool(self, fn, r=(), w=()):
        return self.op('pool', fn, r, w)

    def dma(self, fn, key, r=(), w=(), q='sp'):
        return self.op(q, fn, r, w, dma=key)

    def flush(self, final_dma_keys=()):
        nc = self.nc
        if not any(self.ops[e] for e in ENGS):
            return
        lasts = []
        for e in ENGS:
            for o in reversed(self.ops[e]):
                if o.dkey is None and o.fn is not None:
                    o.flag = True
                    lasts.append(o)
                    break
        for e in ENGS:
            b = Op(e, None)
            b.waits = [o for o in lasts if o.eng != e]
            for k in self.dcnt:
                if k not in self.no_barrier_keys:
                    b.waits.append(('d', k, self.dcnt[k]))
            self.ops[e].append(b)
        for e in ENGS:
            c = self.base[e]
            for o in self.ops[e]:
                if o.dkey is None and o.flag:
                    c += 1
                    o.count = c
            self.base[e] = c
        sem, dsem, waited, ops = self.sem, self.dsem, self.waited, self.ops

        def replay(ename):
            def body(eng):
                wd = waited[ename]
                for o in ops[ename]:
                    for d in o.waits:
                        if isinstance(d, tuple):
                            k, s, v = ('d', d[1]), dsem[d[1]], d[2]
                        else:
                            k, s, v = d.eng, sem[d.eng], d.count
                        if wd.get(k, 0) < v:
                            eng.wait_ge(s, v)
                            wd[k] = v
                    if o.fn is None:
                        continue
                    inst = o.fn(eng)
                    if o.dkey is not None:
                        inst.then_inc(dsem[o.dkey], 16)
                    elif o.flag:
                        inst.then_inc(sem[ename], 1)
            return body

        self.flush_id += 1
        with nc.Block() as blk:
            blk.tensor(replay('pe'))
            blk.scalar(replay('act'))
            blk.vector(replay('dve'))
            blk.gpsimd(replay('pool'))
            blk.sync(replay('sp'))
        for e in ENGS:
            for o in self.ops[e]:
                if o.dkey is None:
                    o.done = True
            self.ops[e] = []
        for t in list(self.lastw.keys()):
            if self.lastw[t].done:
                del self.lastw[t]
        for t in list(self.readers.keys()):
            rd = self.readers[t]
            for k in [k for k, o in rd.items() if o.done]:
                del rd[k]
            if not rd:
                del self.readers[t]


def build(stage=99, debug=False, upto='Z'):
    nc = bass.Bass("TRN2", target_bir_lowering=False)
    D = lambda name, shape, dt, kind="ExternalInput": nc.dram_tensor(name, shape, dt, kind=kind).ap()
    x_all = D("x_all", [8192, 1024], F32)
    x_own = D("x_own", [2304, 1024], F32)
    pos_all = D("pos_all", [1, 8192], I32)
    pos_own = D("pos_own", [1, 2048], I32)
    masks_d = D("masks", [128, 8192], BF16)
    invcnt_d = D("invcnt", [128, 64], F32)
    invf_d = D("invf", [128, 1], F32)
    g_attn_d = D("g_attn", [128, 8], F32)
    w_in = D("w_in", [1024, 928], F32)
    w_pool_d = D("w_pool", [128, 512], F32)
    b_pool_d = D("b_pool", [128, 4], F32)
    pool_scale_d = D("pool_scale", [128, 4], F32)
    g_q_d = D("g_q", [128, 2], F32)
    w_q_b = D("w_q_b", [256, 768], F32)
    g_kv_d = D("g_kv", [128, 1], F32)
    w_kv_b = D("w_kv_b", [128, 1024], F32)
    g_op_d = D("g_op", [128, 4], F32)
    g_oa_d = D("g_oa", [64, 8], F32)
    w_out = D("w_out", [1024, 1024], F32)
    g_ffn_d = D("g_ffn", [1, 1024], F32)
    g_fin_d = D("g_fin", [1, 1024], F32)
    w_r = D("w_r", [1024, 32], F32)
    b_r = D("b_r", [1, 32], F32)
    w_gu_p = [D("w_gu%d" % k, [8, 1024, 2048], F32) for k in range(4)] if stage >= 2 else None
    b_gu_d = D("b_gu", [128, 512], F32)
    w_d_p = [D("w_d%d" % k, [8, 1024, 1024], F32) for k in range(4)] if stage >= 2 else None
    b_d_d = D("b_d", [32, 1024], F32)
    y = D("y", [2048, 1024], F32, kind="ExternalOutput")
    if debug:
        dbg_x1 = D("dbg_x1", [2048, 1024], F32, kind="ExternalOutput")
        dbg_yp = D("dbg_yp", [128, 8192], BF16, kind="ExternalOutput")
        dbg_ya = D("dbg_ya", [64, 16384], BF16, kind="ExternalOutput")
        dbg_cn = D("dbg_cn", [128, 8192], BF16, kind="ExternalOutput")
        dbg_kt = D("dbg_kt", [96, 8192], BF16, kind="ExternalOutput")
        dbg_gates = D("dbg_gates", [128, 512], F32, kind="ExternalOutput")
        dbg_rs = D("dbg_rs", [128, 32], F32, kind="ExternalOutput")
    final_keys = []
    CAP = 384
    NROWS = 32 * CAP
    BIGV = 1.0e6
    xs = nc.dram_tensor("xs_scratch", [NROWS, 1024], BF16, kind="Internal").ap()
    ys = nc.dram_tensor("ys_scratch", [NROWS, 1024], F32, kind="Internal").ap()

    with ExitStack() as top:
        S = Sched(nc, top)
        uid = [0]

        def T(st, name, shape, dt):
            uid[0] += 1
            return st.enter_context(nc.sbuf_tensor("%s_%d" % (name, uid[0]), shape, dt))

        def PS(st, name, shape, dt):
            uid[0] += 1
            return st.enter_context(nc.psum_tensor("%s_%d" % (name, uid[0]), shape, dt))
        BIG0 = T(top, "BIG0", [128, 16384], F32)
        BIG1 = T(top, "BIG1", [128, 16384], BF16)
        BIG2 = T(top, "BIG2", [128, 24576], BF16)
        B0 = BIG0[:]
        B0b = BIG0[:].bitcast(BF16)
        B0i = BIG0[:].bitcast(I32)
        B1 = BIG1[:]
        B2 = BIG2[:]
        ident_f = T(top, "ident_f", [128, 128], F32)
        ident_b = T(top, "ident_b", [128, 128], BF16)
        ones_b = T(top, "ones_b", [128, 128], BF16)
        sel_f = T(top, "sel_f", [128, 64], F32)
        epsc = T(top, "epsc", [128, 1], F32)
        invf = T(top, "invf_t", [128, 1], F32)
        rstdp = T(top, "rstdp", [128, 16], F32)
        rstda = T(top, "rstda", [128, 16], F32)
        ssa = T(top, "ssa", [128, 16], F32)
        gates = T(top, "gates", [128, 512], F32)
        g_oa = T(top, "g_oa_t", [64, 8], F32)
        junk = T(top, "junk", [128, 1024], BF16)
        Lst = T(top, "Lst", [128, 128], BF16)
        tri_f = T(top, "tri_f", [128, 128], F32)
        CE = T(top, "CE", [128, 32], F32)
        CEi = T(top, "CEi", [128, 32], I32)
        carry = T(top, "carry", [128, 32], F32)
        desti = T(top, "desti", [128, 64], I32)
        gk = T(top, "gk", [128, 64], F32)

        ypoolT = B2[:, 0:8192].rearrange("p (g t) -> p g t", g=4)
        yattnT = B2[:, 8192:24576].rearrange("p (h t) -> p h t", h=8)

        S.pool(lambda e: e.memset(ident_f[:], 0.0), w=['ident_f'])
        S.pool(lambda e: e.affine_select(out=ident_f[:], in_=ident_f[:], pattern=[[-1, 128]],
                                         compare_op=ALU.not_equal, fill=1.0, base=0, channel_multiplier=1),
               r=['ident_f'], w=['ident_f'])
        S.dve(lambda e: e.tensor_copy(out=ident_b[:], in_=ident_f[:]), r=['ident_f'], w=['ident_b'])
        S.pool(lambda e: e.memset(ones_b[:], 1.0), w=['ones_b'])
        S.pool(lambda e: e.memset(epsc[:], EPS), w=['epsc'])
        S.pool(lambda e: e.memset(sel_f[:], 0.0), w=['sel_f'])
        S.pool(lambda e: e.memset(sel_f[64:65, :], 1.0), r=['sel_f'], w=['sel_f'])
        S.dma(lambda e: e.dma_start(out=invf[:], in_=invf_d), 'c0', w=['invf'])
        S.pool(lambda e: e.memset(tri_f[:], 1.0), w=['tri_f'])
        S.pool(lambda e: e.affine_select(out=tri_f[:], in_=tri_f[:], pattern=[[1, 128]], compare_op=ALU.is_gt, fill=0.0, base=0, channel_multiplier=-1),
               r=['tri_f'], w=['tri_f'])
        S.dve(lambda e: e.tensor_copy(out=Lst[:], in_=tri_f[:]), r=['tri_f'], w=['Lst'])
        S.pool(lambda e: e.memset(gk[:], 0.0), w=['gk0'])
        S.pool(lambda e: e.memset(carry[:], 0.0), w=['carry'])
        S.pool(lambda e: e.iota(out=CEi[:], pattern=[[CAP, 32]], base=0, channel_multiplier=0), w=['CEi'])
        S.dve(lambda e: e.tensor_copy(out=CE[:], in_=CEi[:]), r=['CEi'], w=['CE'])
        S.pool(lambda e: e.memset(B0b[:, 8192:16384], 0.0), w=['zsrc'])
        for zz in range(NROWS // 1024):
            S.dma(lambda e, zz=zz: e.dma_start(out=xs[zz * 1024:(zz + 1) * 1024, :].rearrange("(n p) d -> p n d", p=128),
                                               in_=B0b[:, 8192:16384].rearrange("p (n d) -> p n d", n=8)), 'zf', r=['zsrc'])
        S.dma(lambda e: e.dma_start(out=g_oa[:], in_=g_oa_d), 'c1', w=['g_oa'])

        def rms_tile(src_ap, slot_tag, ss_ap, ss_tag, xb_ap, xb_tag, ncols=1024, inv_n=1.0 / 1024):
            S.act(lambda e: e.activation(out=junk[:, 0:ncols], in_=src_ap, func=AF.Square, accum_out=ss_ap),
                  r=[slot_tag], w=['junk', ss_tag])
            S.act(lambda e: e.activation(out=ss_ap, in_=ss_ap, func=AF.Ln, bias=epsc[:, 0:1], scale=inv_n),
                  r=[ss_tag, 'epsc'], w=[ss_tag])
            S.act(lambda e: e.activation(out=ss_ap, in_=ss_ap, func=AF.Exp, scale=-0.5), r=[ss_tag], w=[ss_tag])
            if xb_ap is not None:
                S.dve(lambda e: e.tensor_scalar(out=xb_ap, in0=src_ap, scalar1=ss_ap, scalar2=None, op0=ALU.mult),
                      r=[slot_tag, ss_tag], w=[xb_tag])

        def rope_tables(st, pos_src_ap, n, posi, posf, ang, kf, sn, cs, tagp, otag=None):
            otag = otag or tagp
            sl = slice(64, 96)
            S.dma(lambda e: e.dma_start(out=posi[sl, 0:n], in_=pos_src_ap.partition_broadcast(32)), 'pos', w=[tagp + 'posi'])
            S.dve(lambda e: e.tensor_copy(out=posf[sl, 0:n], in_=posi[sl, 0:n]), r=[tagp + 'posi'], w=[tagp + 'posf'])
            S.dve(lambda e: e.tensor_scalar(out=ang[sl, 0:n], in0=posf[sl, 0:n], scalar1=invf[sl, 0:1], scalar2=None, op0=ALU.mult),
                  r=[tagp + 'posf', 'invf'], w=[tagp + 'ang'])
            S.dve(lambda e: e.tensor_scalar(out=posi[sl, 0:n], in0=ang[sl, 0:n], scalar1=1.0 / TWO_PI, scalar2=None, op0=ALU.mult),
                  r=[tagp + 'ang', tagp + 'posf'], w=[tagp + 'posi'])
            S.dve(lambda e: e.tensor_copy(out=kf[sl, 0:n], in_=posi[sl, 0:n]), r=[tagp + 'posi'], w=[tagp + 'kf'])
            C1 = 6.28125
            C2 = float(TWO_PI - C1)
            S.dve(lambda e: e.scalar_tensor_tensor(out=ang[sl, 0:n], in0=kf[sl, 0:n], scalar=-C1, in1=ang[sl, 0:n], op0=ALU.mult, op1=ALU.add),
                  r=[tagp + 'kf', tagp + 'ang'], w=[tagp + 'ang'])
            S.dve(lambda e: e.scalar_tensor_tensor(out=ang[sl, 0:n], in0=kf[sl, 0:n], scalar=-C2, in1=ang[sl, 0:n], op0=ALU.mult, op1=ALU.add),
                  r=[tagp + 'kf', tagp + 'ang'], w=[tagp + 'ang'])
            S.dve(lambda e: e.tensor_scalar(out=kf[sl, 0:n], in0=ang[sl, 0:n], scalar1=PI, scalar2=-TWO_PI, op0=ALU.is_gt, op1=ALU.mult),
                  r=[tagp + 'ang'], w=[tagp + 'kf'])
            S.dve(lambda e: e.tensor_tensor(out=ang[sl, 0:n], in0=ang[sl, 0:n], in1=kf[sl, 0:n], op=ALU.add),
                  r=[tagp + 'ang', tagp + 'kf'], w=[tagp + 'ang'])
            S.dve(lambda e: e.tensor_scalar(out=kf[sl, 0:n], in0=ang[sl, 0:n], scalar1=-PI, scalar2=TWO_PI, op0=ALU.is_lt, op1=ALU.mult),
                  r=[tagp + 'ang'], w=[tagp + 'kf'])
            S.dve(lambda e: e.tensor_tensor(out=ang[sl, 0:n], in0=ang[sl, 0:n], in1=kf[sl, 0:n], op=ALU.add),
                  r=[tagp + 'ang', tagp + 'kf'], w=[tagp + 'ang'])
            S.act(lambda e: e.activation(out=sn[sl, 0:n], in_=ang[sl, 0:n], func=AF.Sin), r=[tagp + 'ang'], w=[otag + 'sn'])
            S.dve(lambda e: e.tensor_scalar(out=kf[sl, 0:n], in0=ang[sl, 0:n], scalar1=PI / 2, scalar2=-TWO_PI, op0=ALU.is_gt, op1=ALU.mult),
                  r=[tagp + 'ang'], w=[tagp + 'kf'])
            S.dve(lambda e: e.scalar_tensor_tensor(out=ang[sl, 0:n], in0=ang[sl, 0:n], scalar=PI / 2, in1=kf[sl, 0:n], op0=ALU.add, op1=ALU.add),
                  r=[tagp + 'ang', tagp + 'kf'], w=[tagp + 'ang'])
            S.act(lambda e: e.activation(out=cs[sl, 0:n], in_=ang[sl, 0:n], func=AF.Sin), r=[tagp + 'ang'], w=[otag + 'cs'])

        with ExitStack() as p2:
            Wkvlat = T(p2, "Wkvlat", [128, 8, 128], BF16)
            Wkr = T(p2, "Wkr", [128, 8, 96], BF16)
            Wkrot = T(p2, "Wkrot", [128, 8, 96], BF16)
            Wq = T(p2, "Wq", [128, 2, 768], BF16)
            Wqrot = T(p2, "Wqrot", [128, 2, 768], BF16)
            Wkv = T(p2, "Wkv", [128, 1024], BF16)
            wpool = T(p2, "wpool", [128, 512], BF16)
            qnT = T(p2, "qnT", [128, 2, 2048], BF16)
            g_attn = T(p2, "g_attn_t", [128, 8], F32)
            ng_attn = T(p2, "ng_attn_t", [128, 8], F32)
            g_q = T(p2, "g_q_t", [128, 2], F32)
            ng_q = T(p2, "ng_q_t", [128, 2], F32)
            g_kv = T(p2, "g_kv_t", [128, 1], F32)
            g_op = T(p2, "g_op_t", [128, 4], F32)
            bpool = T(p2, "bpool_t", [128, 4], F32)
            pscale = T(p2, "pscale_t", [128, 4], F32)
            bsc = T(p2, "bsc", [128, 4], F32)
            scg = T(p2, "scg", [128, 4], F32)
            bsg = T(p2, "bsg", [128, 4], F32)
            invcnt = T(p2, "invcnt_t", [128, 64], F32)

            Win_uq = B2[:, 8192:8192 + 6144].rearrange("p (c n) -> p c n", c=8)
            for i, (dst, src) in enumerate([(g_attn, g_attn_d), (g_q, g_q_d), (g_kv, g_kv_d), (g_op, g_op_d),
                                            (bpool, b_pool_d), (pscale, pool_scale_d), (invcnt, invcnt_d)]):
                S.dma(lambda e, dst=dst, src=src: e.dma_start(out=dst[:], in_=src), 'c%d' % (2 + i), w=[('cst', i)])
            S.dve(lambda e: e.tensor_scalar(out=ng_attn[:], in0=g_attn[:], scalar1=-1.0, scalar2=None, op0=ALU.mult), r=[('cst', 0)], w=['ng_attn'])
            S.dve(lambda e: e.tensor_scalar(out=ng_q[:], in0=g_q[:], scalar1=-1.0, scalar2=None, op0=ALU.mult), r=[('cst', 1)], w=['ng_q'])
            S.dve(lambda e: e.tensor_tensor(out=bsc[:], in0=bpool[:], in1=pscale[:], op=ALU.mult), r=[('cst', 4), ('cst', 5)], w=['bsc'])
            S.dve(lambda e: e.tensor_tensor(out=scg[:], in0=pscale[:], in1=g_op[:], op=ALU.mult), r=[('cst', 3), ('cst', 5)], w=['scg'])
            S.dve(lambda e: e.tensor_tensor(out=bsg[:], in0=bsc[:], in1=g_op[:], op=ALU.mult), r=[('cst', 3), 'bsc'], w=['bsg'])
            S.pool(lambda e: e.memset(Wkr[:], 0.0), w=['Wkr'])
            S.pool(lambda e: e.memset(Wkrot[:], 0.0), w=['Wkrot'])
            S.pool(lambda e: e.memset(Wqrot[:], 0.0), w=['Wqrot'])
            S.dma(lambda e: e.dma_start(out=wpool[:], in_=w_pool_d), 'wpool', w=['wpool'], q='pool')
            stg = B0[:, 0:4096]
            for half in range(2):
                stv = stg[:, 0:3712].rearrange("p (c n) -> p c n", c=4)
                S.dma(lambda e, half=half, stv=stv: e.dma_start(
                    out=stv, in_=w_in[half * 512:(half + 1) * 512, :].rearrange("(c p) n -> p c n", p=128)), 'stg', w=['stg'])
                for c in range(4):
                    cc = half * 4 + c
                    gs = g_attn[:, cc:cc + 1]
                    ngs = ng_attn[:, cc:cc + 1]
                    S.dve(lambda e, c=c, cc=cc, gs=gs, stv=stv: e.tensor_scalar(out=Win_uq[:, cc, :], in0=stv[:, c, 0:768], scalar1=gs, scalar2=None, op0=ALU.mult),
                          r=['stg', ('cst', 0)], w=['Win_uq'])
                    S.dve(lambda e, c=c, cc=cc, gs=gs, stv=stv: e.tensor_scalar(out=Wkvlat[:, cc, :], in0=stv[:, c, 768:896], scalar1=gs, scalar2=None, op0=ALU.mult),
                          r=['stg', ('cst', 0)], w=['Wkvlat'])
                    S.dve(lambda e, c=c, cc=cc, gs=gs, stv=stv: e.tensor_scalar(out=Wkr[:, cc, 64:96], in0=stv[:, c, 896:928], scalar1=gs, scalar2=None, op0=ALU.mult),
                          r=['stg', ('cst', 0), 'Wkr'], w=['Wkr'])
                    S.dve(lambda e, c=c, cc=cc, ngs=ngs, stv=stv: e.tensor_scalar(out=Wkrot[:, cc, 64:80], in0=stv[:, c, 912:928], scalar1=ngs, scalar2=None, op0=ALU.mult),
                          r=['stg', 'ng_attn', 'Wkrot'], w=['Wkrot'])
                    S.dve(lambda e, c=c, cc=cc, gs=gs, stv=stv: e.tensor_scalar(out=Wkrot[:, cc, 80:96], in0=stv[:, c, 896:912], scalar1=gs, scalar2=None, op0=ALU.mult),
                          r=['stg', ('cst', 0), 'Wkrot'], w=['Wkrot'])
            stq = stg[:, 0:1536].rearrange("p (c n) -> p c n", c=2)
            S.dma(lambda e: e.dma_start(out=stq, in_=w_q_b.rearrange("(c p) n -> p c n", p=128)), 'stg', w=['stg'])
            for c in range(2):
                S.dve(lambda e, c=c: e.tensor_scalar(out=Wq[:, c, :], in0=stq[:, c, :], scalar1=g_q[:, c:c + 1], scalar2=None, op0=ALU.mult),
                      r=['stg', ('cst', 1)], w=['Wq'])
                sq4 = stq[:, c, :].rearrange("p (h d) -> p h d", h=8)
                wr4 = Wqrot[:, c, :].rearrange("p (h d) -> p h d", h=8)
                S.dve(lambda e, c=c, sq4=sq4, wr4=wr4: e.tensor_scalar(out=wr4[:, :, 64:80], in0=sq4[:, :, 80:96], scalar1=ng_q[:, c:c + 1], scalar2=None, op0=ALU.mult),
                      r=['stg', 'ng_q', 'Wqrot'], w=['Wqrot'])
                S.dve(lambda e, c=c, sq4=sq4, wr4=wr4: e.tensor_scalar(out=wr4[:, :, 80:96], in0=sq4[:, :, 64:80], scalar1=g_q[:, c:c + 1], scalar2=None, op0=ALU.mult),
                      r=['stg', ('cst', 1), 'Wqrot'], w=['Wqrot'])
            S.dma(lambda e: e.dma_start(out=stg[:, 0:1024], in_=w_kv_b), 'stg', w=['stg'])
            S.dve(lambda e: e.tensor_scalar(out=Wkv[:], in0=stg[:, 0:1024], scalar1=g_kv[:, 0:1], scalar2=None, op0=ALU.mult),
                  r=['stg', ('cst', 2)], w=['Wkv'])
            S.flush()
            if upto == 'W':
                S.enabled = False

            with ExitStack() as pb:
                uT = B0[:, 0:9216].rearrange("p (g t) -> p g t", g=4)
                S1 = B0[:, 9216:11520]
                S2 = B0[:, 11520:13824]
                xa = [B0[:, 13824:14848], B0[:, 14848:15872]]
                diffT = B1[:, 0:9216].rearrange("p (g t) -> p g t", g=4)
                hT = [B1[:, 9216:12288].rearrange("p (c t) -> p c t", c=8), B1[:, 12288:15360].rearrange("p (c t) -> p c t", c=8)]
                xb = B1[:, 15360:16384]
                qn_ext = B2[:, 14336:14336 + 4608].rearrange("p (c t) -> p c t", c=2)
                ss_b = T(pb, "ss_b", [128, 18], F32)
                sqq = T(pb, "sqq", [128, 2, 384], BF16)
                rq = T(pb, "rq", [128, 384], F32)
                ysq = T(pb, "ysq", [128, 4, 512], BF16)
                tmp16 = T(pb, "tmp16", [128, 16], F32)
                pT = [PS(pb, "pT0", [128, 8, 128], BF16), PS(pb, "pT1", [128, 8, 128], BF16)]
                pU = [PS(pb, "pU0", [128, 512], F32), PS(pb, "pU1", [128, 512], F32)]
                pQ = [PS(pb, "pQ0", [128, 512], F32), PS(pb, "pQ1", [128, 512], F32)]
                pSS = PS(pb, "pSS", [128, 512], F32)
                xbs = [xb, T(pb, "xb2", [128, 1024], BF16)[:]]

                xa = xa + [T(pb, "xa2", [128, 1024], F32)[:], T(pb, "xa3", [128, 1024], F32)[:]]

                def stats_b(tl):
                    s4 = tl % 4
                    S.dma(lambda e, tl=tl, s4=s4: e.dma_start(out=xa[s4], in_=x_own[tl * 128:(tl + 1) * 128, :]), 'xa%d' % s4, w=[('xa', s4)])
                    rms_tile(xa[s4], ('xa', s4), ss_b[:, tl:tl + 1], ('ssb', tl), None, None)

                def norm_T_b(tl):
                    sl_ = tl % 2
                    s4 = tl % 4
                    xbc = xbs[sl_]
                    S.dve(lambda e, tl=tl, s4=s4, xbc=xbc: e.tensor_scalar(out=xbc, in0=xa[s4], scalar1=ss_b[:, tl:tl + 1], scalar2=None, op0=ALU.mult),
                          r=[('xa', s4), ('ssb', tl)], w=[('xb', sl_)])
                    for c in range(8):
                        S.pe(lambda e, c=c, sl_=sl_, xbc=xbc: e.transpose(out=pT[sl_][:, c, :], in_=xbc[:, c * 128:(c + 1) * 128], identity=ident_b[:]),
                             r=[('xb', sl_), 'ident_b'], w=[('pT', sl_)])

                def super_b(st):
                    hr = [('hT', st % 2, k) for k in range(3)]
                    for g in range(4):
                        for c in range(8):
                            S.pe(lambda e, g=g, c=c, st=st: e.matmul(pU[g % 2][:, 0:384], lhsT=Win_uq[:, c, g * 128:(g + 1) * 128], rhs=hT[st % 2][:, c, :],
                                                                      start=(c == 0), stop=(c == 7)),
                                 r=hr + ['Win_uq'], w=[('pU', g % 2)])
                        if g % 2 == 0:
                            S.act(lambda e, g=g, st=st: e.copy(out=uT[:, g, st * 384:(st + 1) * 384], in_=pU[g % 2][:, 0:384]), r=[('pU', g % 2)], w=[('uT', g)])
                        else:
                            S.dve(lambda e, g=g, st=st: e.tensor_copy(out=uT[:, g, st * 384:(st + 1) * 384], in_=pU[g % 2][:, 0:384]), r=[('pU', g % 2)], w=[('uT', g)])
                    for q in range(2):
                        for c in range(8):
                            S.pe(lambda e, q=q, c=c, st=st: e.matmul(pQ[q][:, 0:384], lhsT=Win_uq[:, c, 512 + q * 128:512 + (q + 1) * 128], rhs=hT[st % 2][:, c, :],
                                                                      start=(c == 0), stop=(c == 7)),
                                 r=hr + ['Win_uq'], w=[('pQ', q)])
                        S.act(lambda e, q=q: e.activation(out=sqq[:, q, :], in_=pQ[q][:, 0:384], func=AF.Square), r=[('pQ', q)], w=[('sqq', q)])
                    for q in range(2):
                        S.pe(lambda e, q=q: e.matmul(pSS[:, 0:384], lhsT=ones_b[:], rhs=sqq[:, q, :], start=(q == 0), stop=(q == 1)),
                             r=[('sqq', q), 'ones_b'], w=['pSS'])
                    S.act(lambda e: e.activation(out=rq[:], in_=pSS[:, 0:384], func=AF.Ln, bias=epsc[:, 0:1], scale=1.0 / 256),
                          r=['pSS', 'epsc'], w=['rq'])
                    S.act(lambda e: e.activation(out=rq[:], in_=rq[:], func=AF.Exp, scale=-0.5), r=['rq'], w=['rq'])
                    for q in range(2):
                        S.dve(lambda e, q=q, st=st: e.tensor_tensor(out=qn_ext[:, q, st * 384:(st + 1) * 384], in0=pQ[q][:, 0:384], in1=rq[:], op=ALU.mult),
                              r=[('pQ', q), 'rq'], w=['qn_ext'])

                def copy_b(tl):
                    sl_ = tl % 2
                    st = tl // 3
                    k3 = tl % 3
                    S.dve(lambda e: e.tensor_copy(out=hT[st % 2][:, :, k3 * 128:(k3 + 1) * 128], in_=pT[sl_][:]),
                          r=[('pT', sl_)], w=[('hT', st % 2, k3)])
                    if k3 == 2:
                        super_b(st)

                stats_b(0)
                stats_b(1)
                for tl in range(18):
                    if tl + 2 < 18:
                        stats_b(tl + 2)
                    norm_T_b(tl)
                    if tl >= 1:
                        copy_b(tl - 1)
                copy_b(17)

                for q in range(2):
                    S.dve(lambda e, q=q: e.tensor_copy(out=qnT[:, q, :].rearrange("p (i s) -> p i s", i=16),
                                                       in_=qn_ext[:, q, :].rearrange("p (i s) -> p i s", i=16)[:, :, 16:144]),
                          r=['qn_ext'], w=['qnT'])
                N = 2304
                S.pool(lambda e: e.memset(S1, 0.0), w=['S1'])
                S.pool(lambda e: e.memset(S2, 0.0), w=['S2'])

                def shift_add(dst, src, sh, rtag, wtag):
                    S.dve(lambda e: e.tensor_tensor(out=dst[:, sh:N], in0=src[:, sh:N], in1=src[:, 0:N - sh], op=ALU.add), r=[rtag], w=[wtag])

                for g, wdw in enumerate((2, 4, 8, 16)):
                    ug = uT[:, g, :]
                    shift_add(S1, ug, 1, ('uT', g), 'S1')
                    fin, ftag = S1, 'S1'
                    if wdw >= 4:
                        shift_add(S2, S1, 2, 'S1', 'S2')
                        fin, ftag = S2, 'S2'
                    if wdw >= 8:
                        shift_add(S1, S2, 4, 'S2', 'S1')
                        fin, ftag = S1, 'S1'
                    if wdw >= 16:
                        shift_add(S2, S1, 8, 'S1', 'S2')
                        fin, ftag = S2, 'S2'
                    S.dve(lambda e, g=g, fin=fin, wdw=wdw, ug=ug: e.scalar_tensor_tensor(out=diffT[:, g, 16:N], in0=fin[:, 16:N], scalar=1.0 / wdw, in1=ug[:, 16:N],
                                                                                     op0=ALU.mult, op1=ALU.subtract),
                          r=[ftag, ('uT', g)], w=[('diffT', g)])
                    S.dve(lambda e, g=g, fin=fin: e.tensor_tensor(out=tmp16[:], in0=fin[:, 16:32], in1=invcnt[:, g * 16:(g + 1) * 16], op=ALU.mult),
                          r=[ftag, ('cst', 6)], w=['tmp16'])
                    S.dve(lambda e, g=g, ug=ug: e.tensor_tensor(out=diffT[:, g, 16:32], in0=tmp16[:], in1=ug[:, 16:32], op=ALU.subtract),
                          r=['tmp16', ('uT', g), ('diffT', g)], w=[('diffT', g)])
                wpv = wpool[:].rearrange("p (g d) -> p g d", g=4)
                for qg in range(4):
                    for g in range(4):
                        rhs = diffT[:, g, :].rearrange("p (i s) -> p i s", i=16)[:, 4 * qg:4 * qg + 4, 16:144]
                        S.pe(lambda e, g=g, rhs=rhs: e.matmul(pU[g % 2][:, 0:512].rearrange("p (a b) -> p a b", a=4), lhsT=wpv[:, g, :], rhs=rhs, start=True, stop=True),
                             r=[('diffT', g), 'wpool'], w=[('pU', g % 2)])
                        S.act(lambda e, g=g, qg=qg: e.activation(out=ypoolT[:, g, qg * 512:(qg + 1) * 512], in_=pU[g % 2][:, 0:512], func=AF.Identity,
                                                                bias=bsg[:, g:g + 1], scale=scg[:, g:g + 1]),
                              r=[('pU', g % 2), 'bsg', 'scg'], w=[('ypoolT', qg)])
                        S.act(lambda e, g=g: e.activation(out=ysq[:, g, :], in_=pU[g % 2][:, 0:512], func=AF.Square,
                                                          bias=bsc[:, g:g + 1], scale=pscale[:, g:g + 1]),
                              r=[('pU', g % 2), 'bsc', ('cst', 5)], w=[('ysq', g)])
                    for t4 in range(4):
                        i = qg * 4 + t4
                        for g in range(4):
                            S.pe(lambda e, g=g, t4=t4, i=i: e.matmul(pSS[:, i:i + 1], lhsT=ysq[:, g, t4 * 128:(t4 + 1) * 128], rhs=ones_b[:, 0:1],
                                                                     start=(g == 0), stop=(g == 3)),
                                 r=[('ysq', g), 'ones_b'], w=['pSS'])
                S.act(lambda e: e.activation(out=rstdp[:], in_=pSS[:, 0:16], func=AF.Ln, bias=epsc[:, 0:1], scale=1.0 / 512),
                      r=['pSS', 'epsc'], w=['rstdp'])
                S.act(lambda e: e.activation(out=rstdp[:], in_=rstdp[:], func=AF.Exp, scale=-0.5), r=['rstdp'], w=['rstdp'])
                if debug:
                    S.dma(lambda e: e.dma_start(out=dbg_yp, in_=B2[:, 0:8192]), 'dbg0', r=[('ypoolT', q) for q in range(4)])
                    final_keys.append('dbg0')
                S.flush()
                if upto == 'B':
                    S.enabled = False

            cnT = B1[:, 0:8192]
            KT = B1[:, 8192:16384]
            with ExitStack() as pa:
                xa = [B0[:, 0:1024], B0[:, 1024:2048]]
                posf, ang, kf, sn, cs, t1 = [B0[:, 2048 + k * 512:2048 + (k + 1) * 512] for k in range(6)]
                posi = B0i[:, 5120:5632]
                rkv = B0[:, 5632:6144]
                t2 = B0[:, 6144:6656]
                hT = [B0b[:, 14336:18432].rearrange("p (c t) -> p c t", c=8), B0b[:, 18432:22528].rearrange("p (c t) -> p c t", c=8)]
                xb = B0b[:, 22528:23552]
                sq = B0b[:, 23552:24064]
                ss_a = T(pa, "ss_a", [128, 64], F32)
                pT = [PS(pa, "pT0", [128, 8, 128], BF16), PS(pa, "pT1", [128, 8, 128], BF16)]
                pKV = PS(pa, "pKV", [128, 512], F32)
                pKR = PS(pa, "pKR", [128, 512], F32)
                pKO = PS(pa, "pKO", [128, 512], F32)
                pSS = PS(pa, "pSS", [128, 512], F32)
                sl = slice(64, 96)
                xbs = [xb, B0b[:, 24064:25088]]

                xa = xa + [B0[:, 12544:13568], B0[:, 13568:14592]]

                def stats_a(tl):
                    s4 = tl % 4
                    S.dma(lambda e, tl=tl, s4=s4: e.dma_start(out=xa[s4], in_=x_all[tl * 128:(tl + 1) * 128, :]), 'xa%d' % s4, w=[('xa', s4)])
                    rms_tile(xa[s4], ('xa', s4), ss_a[:, tl:tl + 1], ('ssa_', tl), None, None)

                def norm_T_a(tl):
                    sl_ = tl % 2
                    s4 = tl % 4
                    xbc = xbs[sl_]
                    S.dve(lambda e, tl=tl, s4=s4, xbc=xbc: e.tensor_scalar(out=xbc, in0=xa[s4], scalar1=ss_a[:, tl:tl + 1], scalar2=None, op0=ALU.mult),
                          r=[('xa', s4), ('ssa_', tl)], w=[('xb', sl_)])
                    for c in range(8):
                        S.pe(lambda e, c=c, sl_=sl_, xbc=xbc: e.transpose(out=pT[sl_][:, c, :], in_=xbc[:, c * 128:(c + 1) * 128], identity=ident_b[:]),
                             r=[('xb', sl_), 'ident_b'], w=[('pT', sl_)])

                def super_a(st):
                    hr = [('hT', st % 2, k) for k in range(4)]
                    cols = slice(st * 512, (st + 1) * 512)
                    for c in range(8):
                        S.pe(lambda e, c=c, st=st: e.matmul(pKV[:], lhsT=Wkvlat[:, c, :], rhs=hT[st % 2][:, c, :], start=(c == 0), stop=(c == 7)),
                             r=hr + ['Wkvlat'], w=['pKV'])
                    for c in range(8):
                        S.pe(lambda e, c=c, st=st: e.matmul(pKR[0:96, :], lhsT=Wkr[:, c, :], rhs=hT[st % 2][:, c, :], start=(c == 0), stop=(c == 7)),
                             r=hr + ['Wkr'], w=['pKR'])
                    for c in range(8):
                        S.pe(lambda e, c=c, st=st: e.matmul(pKO[0:96, :], lhsT=Wkrot[:, c, :], rhs=hT[st % 2][:, c, :], start=(c == 0), stop=(c == 7)),
                             r=hr + ['Wkrot'], w=['pKO'])
                    S.act(lambda e: e.activation(out=sq, in_=pKV[:], func=AF.Square), r=['pKV'], w=['sq'])
                    S.pe(lambda e: e.matmul(pSS[:], lhsT=ones_b[:], rhs=sq, start=True, stop=True), r=['sq', 'ones_b'], w=['pSS'])
                    S.act(lambda e: e.activation(out=rkv, in_=pSS[:], func=AF.Ln, bias=epsc[:, 0:1], scale=1.0 / 128),
                          r=['pSS', 'epsc'], w=['rkv'])
                    S.act(lambda e: e.activation(out=rkv, in_=rkv, func=AF.Exp, scale=-0.5), r=['rkv'], w=['rkv'])
                    S.dve(lambda e, cols=cols: e.tensor_tensor(out=cnT[:, cols], in0=pKV[:], in1=rkv, op=ALU.mult), r=['pKV', 'rkv'], w=[('cnT', st)])
                    rope_tables(st, pos_all[0:1, cols], 512, posi, posf, ang, kf, sn, cs, 'A')
                    S.dve(lambda e: e.tensor_tensor(out=t1[sl, :], in0=pKR[sl, :], in1=cs[sl, :], op=ALU.mult), r=['pKR', 'Acs'], w=['t1'])
                    S.dve(lambda e: e.tensor_tensor(out=t2[sl, :], in0=pKO[sl, :], in1=sn[sl, :], op=ALU.mult), r=['pKO', 'Asn'], w=['t2'])
                    S.dve(lambda e, cols=cols: e.tensor_tensor(out=KT[sl, cols], in0=t1[sl, :], in1=t2[sl, :], op=ALU.add), r=['t1', 't2'], w=[('KTr', st)])

                def copy_a(tl):
                    sl_ = tl % 2
                    st = tl // 4
                    k4 = tl % 4
                    S.dve(lambda e: e.tensor_copy(out=hT[st % 2][:, :, k4 * 128:(k4 + 1) * 128], in_=pT[sl_][:]),
                          r=[('pT', sl_)], w=[('hT', st % 2, k4)])
                    if k4 == 3:
                        super_a(st)

                stats_a(0)
                stats_a(1)
                for tl in range(64):
                    if tl + 2 < 64:
                        stats_a(tl + 2)
                    norm_T_a(tl)
                    if tl >= 1:
                        copy_a(tl - 1)
                copy_a(63)

                if debug:
                    S.dma(lambda e: e.dma_start(out=dbg_cn, in_=cnT), 'dbg1', r=[('cnT', s) for s in range(16)])
                    final_keys.append('dbg1')
                S.flush()
                if upto == 'A':
                    S.enabled = False

            with ExitStack() as pc:
                masks = B0b[:, 0:8192].rearrange("p (m q) -> p m q", m=16)
                V = B0b[:, 8192:8192 + 4160].rearrange("p (n d) -> p n d", n=64)
                QT = B0b[:, 12352:14400]
                PT = [B0b[:, 14400 + k * 512:14400 + (k + 1) * 512] for k in range(4)]
                cosq = B0[:, 8224:10272]
                sinq = B0[:, 10272:12320]
                Osb = B0[:, 12320:12832]
                rec = B0[:, 12832:13344]
                posf, ang, kf = [B0[:, 13344 + k * 512:13344 + (k + 1) * 512] for k in range(3)]
                posi = B0i[:, 14880:15392]
                ytmp = B0[:, 15392:15904]
                ysqa = T(pc, "ysqa", [64, 512], BF16)
                PT = PT + [T(pc, "PT4", [128, 512], BF16)[:], T(pc, "PT5", [128, 512], BF16)[:]]
                dh = T(pc, "dh", [128, 512], BF16)
                dl = T(pc, "dl", [128, 512], BF16)
                ps = [PS(pc, "ps%d" % k, [128, 512], F32) for k in range(8)]
                S.dma(lambda e: e.dma_start(out=B0b[:, 0:8192], in_=masks_d), 'masks', w=['masks'])
                S.pool(lambda e: e.memset(V[:, :, 64:65], 1.0), w=['Vones'])
                for ch in range(4):
                    cc = slice(ch * 512, (ch + 1) * 512)
                    rope_tables(ch, pos_own[0:1, cc], 512, posi, posf, ang, kf, sinq[:, cc], cosq[:, cc], 'C', 'C%d' % ch)
                tabr = ['C%dsn' % ch for ch in range(4)] + ['C%dcs' % ch for ch in range(4)]
                scale = float(96 ** -0.5)
                sl = slice(64, 96)
                t1, t2 = posf, ang
                cnt = 0
                for h in range(8):
                    for st in range(16):
                        cols = slice(st * 512, (st + 1) * 512)
                        S.pe(lambda e, h=h, st=st, cols=cols: e.matmul(ps[st % 2][0:64, :], lhsT=Wkv[:, h * 128:h * 128 + 64], rhs=cnT[:, cols], start=True, stop=True),
                             r=['Wkv', ('cnT', st)], w=[('ps', st % 2)])
                        if st % 2 == 0:
                            S.act(lambda e, st=st, cols=cols: e.copy(out=KT[0:64, cols], in_=ps[st % 2][0:64, :]), r=[('ps', st % 2)], w=[('KTn', st)])
                        else:
                            S.dve(lambda e, st=st, cols=cols: e.tensor_copy(out=KT[0:64, cols], in_=ps[st % 2][0:64, :]), r=[('ps', st % 2)], w=[('KTn', st)])
                    for n8 in range(8):
                        for k in range(8):
                            n = n8 * 8 + k
                            S.pe(lambda e, h=h, n=n, k=k, n8=n8: e.matmul(ps[n8 % 2][:, k * 64:(k + 1) * 64], lhsT=cnT[:, n * 128:(n + 1) * 128],
                                                                           rhs=Wkv[:, h * 128 + 64:h * 128 + 128], start=True, stop=True),
                                 r=['Wkv', ('cnT', n // 4)], w=[('ps', n8 % 2)])
                        if n8 % 2 == 0:
                            S.dve(lambda e, n8=n8: e.tensor_copy(out=V[:, n8 * 8:(n8 + 1) * 8, 0:64], in_=ps[n8 % 2][:].rearrange("p (a b) -> p a b", a=8)),
                                  r=[('ps', n8 % 2)], w=[('V', n8)])
                        else:
                            S.act(lambda e, n8=n8: e.copy(out=V[:, n8 * 8:(n8 + 1) * 8, 0:64], in_=ps[n8 % 2][:].rearrange("p (a b) -> p a b", a=8)),
                                  r=[('ps', n8 % 2)], w=[('V', n8)])
                    for qg in range(4):
                        cols = slice(qg * 512, (qg + 1) * 512)
                        for c in range(2):
                            S.pe(lambda e, h=h, c=c, cols=cols: e.matmul(ps[0][0:96, :], lhsT=Wq[:, c, h * 96:(h + 1) * 96], rhs=qnT[:, c, cols], start=(c == 0), stop=(c == 1)),
                                 r=['Wq', 'qnT'], w=[('ps', 0)])
                        for c in range(2):
                            S.pe(lambda e, h=h, c=c, cols=cols: e.matmul(ps[1][0:96, :], lhsT=Wqrot[:, c, h * 96:(h + 1) * 96], rhs=qnT[:, c, cols], start=(c == 0), stop=(c == 1)),
                                 r=['Wqrot', 'qnT'], w=[('ps', 1)])
                        S.act(lambda e, cols=cols: e.copy(out=QT[0:64, cols], in_=ps[0][0:64, :]), r=[('ps', 0)], w=[('QT', qg)])
                        S.dve(lambda e, cols=cols: e.tensor_tensor(out=t1[sl, :], in0=ps[0][sl, :], in1=cosq[sl, cols], op=ALU.mult), r=[('ps', 0)] + tabr, w=['t1'])
                        S.dve(lambda e, cols=cols: e.tensor_tensor(out=t2[sl, :], in0=ps[1][sl, :], in1=sinq[sl, cols], op=ALU.mult), r=[('ps', 1)] + tabr, w=['t2'])
                        S.dve(lambda e, cols=cols: e.tensor_tensor(out=QT[sl, cols], in0=t1[sl, :], in1=t2[sl, :], op=ALU.add), r=['t1', 't2', ('QT', qg)], w=[('QT', qg)])
                    for qg in range(4):
                        qc0 = qg * 512
                        qcols = slice(qg * 512, (qg + 1) * 512)
                        nkb = 16 * qg + 16
                        base = cnt
                        order = [16 * qg] + [16 * qg + m for m in range(4, 16)] + list(range(16 * qg)) + [16 * qg + 1, 16 * qg + 2, 16 * qg + 3]
                        LA = 4

                        def c0_of(kb, qg=qg):
                            m = kb - 16 * qg
                            return 0 if m < 0 else (m // 4) * 128

                        def qk(ui, base=base, qc0=qc0, qg=qg, order=order):
                            kb = order[ui]
                            c0 = c0_of(kb)
                            b_ = (base + ui) % 5
                            S.pe(lambda e, kb=kb, b_=b_, c0=c0: e.matmul(ps[b_][:, c0:512], lhsT=KT[0:96, kb * 128:(kb + 1) * 128], rhs=QT[0:96, qc0 + c0:qc0 + 512],
                                                                          start=True, stop=True),
                                 r=[('KTn', kb // 4), ('KTr', kb // 4), ('QT', qg)], w=[('ps', b_)])

                        def pv(ui, base=base, qg=qg, nkb=nkb, order=order):
                            kb = order[ui]
                            c0 = c0_of(kb)
                            b_ = (base + ui) % 5
                            pt = (base + ui) % 6
                            S.act(lambda e, b_=b_, pt=pt, c0=c0: e.activation(out=PT[pt][:, c0:512], in_=ps[b_][:, c0:512], func=AF.Exp, scale=scale), r=[('ps', b_)], w=[('PT', pt)])
                            if kb >= 16 * qg:
                                m = kb - 16 * qg
                                S.dve(lambda e, pt=pt, m=m, c0=c0: e.tensor_tensor(out=PT[pt][:, c0:512], in0=PT[pt][:, c0:512], in1=masks[:, m, c0:512], op=ALU.mult),
                                      r=[('PT', pt), 'masks'], w=[('PT', pt)])
                            S.pe(lambda e, kb=kb, pt=pt, c0=c0, ui=ui: e.matmul(ps[5][0:65, c0:512], lhsT=V[:, kb, 0:65], rhs=PT[pt][:, c0:512], start=(ui == 0), stop=(ui == nkb - 1)),
                                 r=[('V', kb // 8), 'Vones', ('PT', pt)], w=[('ps', 5)])

                        for ui in range(min(LA, nkb)):
                            qk(ui)
                        for ui in range(nkb):
                            pv(ui)
                            if ui + LA < nkb:
                                qk(ui + LA)
                        cnt += nkb
                        S.act(lambda e: e.copy(out=Osb[0:65, :], in_=ps[5][0:65, :]), r=[('ps', 5)], w=['Osb'])
                        S.dve(lambda e: e.tensor_copy(out=dh[64:65, :], in_=Osb[64:65, :]), r=['Osb'], w=['dh'])
                        S.dve(lambda e: e.tensor_tensor(out=dl[64:65, :], in0=Osb[64:65, :], in1=dh[64:65, :], op=ALU.subtract), r=['Osb', 'dh'], w=['dl'])
                        S.pe(lambda e: e.matmul(ps[6][0:64, :], lhsT=ones_b[64:65, 0:64], rhs=dh[64:65, :], start=True, stop=False), r=['dh', 'ones_b'], w=[('ps', 6)])
                        S.pe(lambda e: e.matmul(ps[6][0:64, :], lhsT=ones_b[64:65, 0:64], rhs=dl[64:65, :], start=False, stop=True), r=['dl', 'ones_b'], w=[('ps', 6)])
                        S.dve(lambda e: e.reciprocal(out=rec[0:64, :], in_=ps[6][0:64, :]), r=[('ps', 6)], w=['rec'])
                        S.dve(lambda e: e.tensor_tensor(out=ytmp[0:64, :], in0=Osb[0:64, :], in1=rec[0:64, :], op=ALU.mult), r=['Osb', 'rec'], w=['ytmp'])
                        S.dve(lambda e, h=h, qcols=qcols: e.tensor_scalar(out=yattnT[0:64, h, qcols], in0=ytmp[0:64, :], scalar1=g_oa[:, h:h + 1], scalar2=None, op0=ALU.mult),
                              r=['ytmp', 'g_oa'], w=[('yattnT', h, qg)])
                        S.act(lambda e: e.activation(out=ysqa[:], in_=ytmp[0:64, :], func=AF.Square), r=['ytmp'], w=['ysqa'])
                        for t4 in range(4):
                            S.pe(lambda e, t4=t4: e.matmul(ps[7][:, t4:t4 + 1], lhsT=ysqa[:, t4 * 128:(t4 + 1) * 128], rhs=ones_b[0:64, 0:1], start=True, stop=True),
                                 r=['ysqa', 'ones_b'], w=[('ps', 7)])
                        if h == 0:
                            S.dve(lambda e, qg=qg: e.tensor_copy(out=ssa[:, qg * 4:(qg + 1) * 4], in_=ps[7][:, 0:4]), r=[('ps', 7)], w=[('ssa', qg)])
                        else:
                            S.dve(lambda e, qg=qg: e.tensor_tensor(out=ssa[:, qg * 4:(qg + 1) * 4], in0=ssa[:, qg * 4:(qg + 1) * 4], in1=ps[7][:, 0:4], op=ALU.add),
                                  r=[('ps', 7), ('ssa', qg)], w=[('ssa', qg)])
                S.act(lambda e: e.activation(out=rstda[:], in_=ssa[:], func=AF.Ln, bias=epsc[:, 0:1], scale=1.0 / 512),
                      r=[('ssa', q) for q in range(4)] + ['epsc'], w=['rstda'])
                S.act(lambda e: e.activation(out=rstda[:], in_=rstda[:], func=AF.Exp, scale=-0.5), r=['rstda'], w=['rstda'])
                if debug:
                    S.dma(lambda e: e.dma_start(out=dbg_ya, in_=B2[0:64, 8192:24576]), 'dbg2', r=[('yattnT', h, q) for h in range(8) for q in range(4)])
                    S.dma(lambda e: e.dma_start(out=dbg_kt, in_=KT[0:96, :]), 'dbg3', r=[('KTn', s) for s in range(16)] + [('KTr', s) for s in range(16)])
                    S.dma(lambda e: e.dma_start(out=dbg_rs[:, 0:16], in_=rstdp[:]), 'dbg4', r=['rstdp'])
                    S.dma(lambda e: e.dma_start(out=dbg_rs[:, 16:32], in_=rstda[:]), 'dbg4', r=['rstda'])
                    final_keys.extend(['dbg2', 'dbg3', 'dbg4'])
                S.flush()
                if upto == 'C':
                    S.enabled = False

        xn = B0.rearrange("p (i d) -> p i d", i=16)
        h2T = B1.rearrange("p (c t) -> p c t", c=8)

        with ExitStack() as pd:
            Wop = T(pd, "Wop", [128, 4, 1024], BF16)
            Woa = T(pd, "Woa", [64, 8, 1024], BF16)
            xo = [T(pd, "xo0", [128, 1024], F32), T(pd, "xo1", [128, 1024], F32)]
            pP = [PS(pd, "pP0", [128, 512], F32), PS(pd, "pP1", [128, 512], F32)]
            pA = [PS(pd, "pA0", [128, 512], F32), PS(pd, "pA1", [128, 512], F32)]
            S.dma(lambda e: e.dma_start(out=Wop[:], in_=w_out[0:512, :].rearrange("(g p) n -> p g n", p=128)), 'wop', w=['Wop'], q='pool')
            S.dma(lambda e: e.dma_start(out=Woa[:], in_=w_out[512:1024, :].rearrange("(h p) n -> p h n", p=64)), 'woa', w=['Woa'], q='pool')
            for i in range(16):
                tcols = slice(i * 128, (i + 1) * 128)
                S.dma(lambda e, i=i: e.dma_start(out=xo[i % 2][:], in_=x_own[144 * i + 16:144 * i + 144, :]), 'xo%d' % (i % 2), w=[('xo', i % 2)])
                for half in range(2):
                    hc = slice(half * 512, (half + 1) * 512)
                    for g in range(4):
                        S.pe(lambda e, g=g, half=half, tcols=tcols, hc=hc: e.matmul(pP[half][:], lhsT=ypoolT[:, g, tcols], rhs=Wop[:, g, hc], start=(g == 0), stop=(g == 3)),
                             r=['Wop'], w=[('pP', half)])
                    for h in range(8):
                        S.pe(lambda e, h=h, half=half, tcols=tcols, hc=hc: e.matmul(pA[half][:], lhsT=yattnT[0:64, h, tcols], rhs=Woa[:, h, hc], start=(h == 0), stop=(h == 7)),
                             r=['Woa'], w=[('pA', half)])
                    S.dve(lambda e, i=i, half=half, hc=hc: e.scalar_tensor_tensor(out=xn[:, i, hc], in0=pP[half][:], scalar=rstdp[:, i:i + 1], in1=xo[i % 2][:, hc],
                                                                                 op0=ALU.mult, op1=ALU.add),
                          r=[('pP', half), ('xo', i % 2)], w=[('xn', i, half)])
                    S.dve(lambda e, i=i, half=half, hc=hc: e.scalar_tensor_tensor(out=xn[:, i, hc], in0=pA[half][:], scalar=rstda[:, i:i + 1], in1=xn[:, i, hc],
                                                                                 op0=ALU.mult, op1=ALU.add),
                          r=[('pA', half), ('xn', i, half)], w=[('xn', i, half)])
                if debug:
                    S.dma(lambda e, i=i: e.dma_start(out=dbg_x1[i * 128:(i + 1) * 128, :], in_=xn[:, i, :]), 'dbg5', r=[('xn', i, 0), ('xn', i, 1)])
            if debug:
                final_keys.append('dbg5')
            S.flush()
            if upto == 'D':
                S.enabled = False

        Wgu = [B1.rearrange("p (c n) -> p c n", c=8), B2[:, 0:16384].rearrange("p (c n) -> p c n", c=8)]
        Wd = B2[:, 16384:24576].rearrange("p (c n) -> p c n", c=8)

        def load_gu(e_):
            S.dma(lambda e, e_=e_: e.dma_start(out=Wgu[e_ % 2], in_=w_gu_p[e_ // 8][e_ % 8].rearrange("(c p) n -> p c n", p=128)),
                  'wgu%d' % (e_ % 2), w=[('Wgu', e_ % 2)], q='pool')

        def load_d(e_):
            S.dma(lambda e, e_=e_: e.dma_start(out=Wd, in_=w_d_p[e_ // 8][e_ % 8].rearrange("(c p) n -> p c n", p=128)), 'wd', w=['Wd'], q='pool')

        S.no_barrier_keys.update(['wgu0', 'wgu1', 'wd'])

        with ExitStack() as pe_:
            gffn = T(pe_, "gffn", [128, 1024], F32)
            wrf = T(pe_, "wrf", [128, 8, 32], F32)
            brb = T(pe_, "brb", [128, 32], F32)
            h2tok = T(pe_, "h2tok", [128, 1024], F32)
            hib = T(pe_, "hib", [128, 1024], BF16)
            lob = T(pe_, "lob", [128, 1024], BF16)
            loT = T(pe_, "loT", [128, 8, 128], BF16)
            whi = T(pe_, "whi", [128, 8, 32], BF16)
            wlo = T(pe_, "wlo", [128, 8, 32], BF16)
            ss2 = T(pe_, "ss2", [128, 16], F32)
            lg = T(pe_, "lg", [128, 32], F32)
            top8 = T(pe_, "top8", [128, 8], F32)
            msk = T(pe_, "msk", [128, 32], F32)
            ex = T(pe_, "ex", [128, 32], F32)
            nm = T(pe_, "nm", [128, 1], F32)
            den = T(pe_, "den", [128, 1], F32)
            ptr = [PS(pe_, "ptr0", [128, 8, 128], BF16), PS(pe_, "ptr1", [128, 8, 128], BF16)]
            plg = PS(pe_, "plg", [128, 512], F32)
            plb = [PS(pe_, "plb0", [128, 512], F32), PS(pe_, "plb1", [128, 512], F32)]
            bdb = T(pe_, "bdb", [32, 1024], BF16)
            gb = T(pe_, "gb", [128, 32], BF16)
            gT = T(pe_, "gT", [32, 128], BF16)
            hiT = T(pe_, "hiT", [128, 8, 128], BF16)
            hibs = [hib, T(pe_, "hib1", [128, 1024], BF16)]
            mb = T(pe_, "mb", [128, 32], BF16)
            dtab = T(pe_, "dtab", [128, 32], F32)
            ov = T(pe_, "ov", [128, 32], F32)
            oh = T(pe_, "oh", [128, 32], F32)
            tmp32 = T(pe_, "tmp32", [128, 32], F32)
            destf = T(pe_, "destf", [128, 4], F32)
            okf = T(pe_, "okf", [128, 4], F32)
            ppx = PS(pe_, "ppx", [128, 512], F32)
            S.dma(lambda e: e.dma_start(out=bdb[:], in_=b_d_d), 'e3', w=['bdb'], q='pool')
            if stage >= 2:
                load_gu(0)
                load_gu(1)
                load_d(0)
            S.dma(lambda e: e.dma_start(out=gffn[:], in_=g_ffn_d.partition_broadcast(128)), 'e0', w=['gffn'])
            S.dma(lambda e: e.dma_start(out=wrf[:], in_=w_r.rearrange("(c p) n -> p c n", p=128)), 'e1', w=['wrf'])
            S.dma(lambda e: e.dma_start(out=brb[:], in_=b_r.partition_broadcast(128)), 'e2', w=['brb'])
            S.dve(lambda e: e.tensor_copy(out=whi[:], in_=wrf[:]), r=['wrf'], w=['whi'])
            S.dve(lambda e: e.tensor_tensor(out=wlo[:], in0=wrf[:], in1=whi[:], op=ALU.subtract), r=['wrf', 'whi'], w=['wlo'])
            for i in range(16):
                S.act(lambda e, i=i: e.activation(out=junk[:], in_=xn[:, i, :], func=AF.Square, accum_out=ss2[:, i:i + 1]),
                      r=[('xn', i, 0), ('xn', i, 1)], w=['junk', ('ss2q', i)])
            S.act(lambda e: e.activation(out=ss2[:], in_=ss2[:], func=AF.Ln, bias=epsc[:, 0:1], scale=1.0 / 1024),
                  r=[('ss2q', i) for i in range(16)] + ['epsc'], w=['ss2all'])
            S.act(lambda e: e.activation(out=ss2[:], in_=ss2[:], func=AF.Exp, scale=-0.5), r=['ss2all'], w=['ss2all'])
            for i in range(16):
                xt = [('xn', i, 0), ('xn', i, 1)]
                tc_ = slice(i * 128, (i + 1) * 128)
                S.dve(lambda e, i=i: e.scalar_tensor_tensor(out=h2tok[:], in0=xn[:, i, :], scalar=ss2[:, i:i + 1], in1=gffn[:], op0=ALU.mult, op1=ALU.mult),
                      r=xt + ['ss2all', 'gffn'], w=['h2tok'])
                hib = hibs[i % 2]
                hbt = ('hib', i % 2)
                S.dve(lambda e, hib=hib: e.tensor_copy(out=hib[:], in_=h2tok[:]), r=['h2tok'], w=[hbt])
                S.dve(lambda e, hib=hib: e.tensor_tensor(out=lob[:], in0=h2tok[:], in1=hib[:], op=ALU.subtract), r=['h2tok', hbt], w=['lob'])
                for c in range(8):
                    S.pe(lambda e, c=c, hib=hib: e.transpose(out=ptr[0][:, c, :], in_=hib[:, c * 128:(c + 1) * 128], identity=ident_b[:]), r=[hbt, 'ident_b'], w=[('ptr', 0)])
                for c in range(8):
                    S.pe(lambda e, c=c: e.transpose(out=ptr[1][:, c, :], in_=lob[:, c * 128:(c + 1) * 128], identity=ident_b[:]), r=['lob', 'ident_b'], w=[('ptr', 1)])
                S.act(lambda e: e.copy(out=hiT[:], in_=ptr[0][:]), r=[('ptr', 0)], w=['hiT'])
                S.dve(lambda e: e.tensor_copy(out=loT[:], in_=ptr[1][:]), r=[('ptr', 1)], w=['loT'])
                k = 0
                for c in range(8):
                    for (lt, ltag, rt, rtag) in ((hiT[:, c, :], 'hiT', whi, 'whi'), (hiT[:, c, :], 'hiT', wlo, 'wlo'), (loT[:, c, :], 'loT', whi, 'whi')):
                        S.pe(lambda e, lt=lt, rt=rt, c=c, k=k: e.matmul(plg[:, 0:32], lhsT=lt, rhs=rt[:, c, :], start=(k == 0), stop=(k == 23)),
                             r=[ltag, rtag], w=['plg'])
                        k += 1
                S.dve(lambda e: e.tensor_tensor(out=lg[:], in0=plg[:, 0:32], in1=brb[:], op=ALU.add), r=['plg', 'brb'], w=['lg'])
                S.dve(lambda e: e.max(out=top8[:], in_=lg[:]), r=['lg'], w=['top8'])
                S.dve(lambda e: e.tensor_scalar(out=msk[:], in0=lg[:], scalar1=top8[:, 3:4], scalar2=None, op0=ALU.is_ge), r=['lg', 'top8'], w=['msk'])
                S.dve(lambda e: e.tensor_scalar(out=nm[:], in0=top8[:, 0:1], scalar1=-1.0, scalar2=None, op0=ALU.mult), r=['top8'], w=['nm'])
                S.act(lambda e: e.activation(out=ex[:], in_=lg[:], func=AF.Exp, bias=nm[:, 0:1], scale=1.0), r=['lg', 'nm'], w=['ex'])
                S.dve(lambda e: e.tensor_tensor(out=ex[:], in0=ex[:], in1=msk[:], op=ALU.mult), r=['ex', 'msk'], w=['ex'])
                S.dve(lambda e: e.reduce_sum(out=den[:], in_=ex[:], axis=mybir.AxisListType.X), r=['ex'], w=['den'])
                S.dve(lambda e: e.reciprocal(out=den[:], in_=den[:]), r=['den'], w=['den'])
                S.dve(lambda e, i=i: e.tensor_scalar(out=gates[:, i * 32:(i + 1) * 32], in0=ex[:], scalar1=den[:, 0:1], scalar2=None, op0=ALU.mult),
                      r=['ex', 'den'], w=[('gates', i)])
                if stage >= 2:
                    S.dve(lambda e: e.tensor_copy(out=mb[:], in_=msk[:]), r=['msk'], w=['mb'])
                    S.pe(lambda e: e.matmul(ppx[:, 0:32], lhsT=Lst[:], rhs=mb[:], start=True, stop=True), r=['Lst', 'mb'], w=['ppx'])
                    S.pe(lambda e: e.matmul(ppx[:, 32:64], lhsT=ones_b[:], rhs=mb[:], start=True, stop=True), r=['ones_b', 'mb'], w=['ppx'])
                    S.dve(lambda e: e.tensor_tensor(out=dtab[:], in0=ppx[:, 0:32], in1=carry[:], op=ALU.add), r=['ppx', 'carry'], w=['dtab'])
                    S.dve(lambda e: e.tensor_tensor(out=carry[:], in0=carry[:], in1=ppx[:, 32:64], op=ALU.add), r=['ppx', 'carry'], w=['carry'])
                    S.dve(lambda e: e.tensor_scalar(out=ov[:], in0=dtab[:], scalar1=float(CAP), scalar2=BIGV, op0=ALU.is_ge, op1=ALU.mult), r=['dtab'], w=['ov'])
                    S.dve(lambda e: e.tensor_tensor(out=dtab[:], in0=dtab[:], in1=CE[:], op=ALU.add), r=['dtab', 'CE'], w=['dtab'])
                    S.dve(lambda e: e.tensor_tensor(out=dtab[:], in0=dtab[:], in1=ov[:], op=ALU.add), r=['dtab', 'ov'], w=['dtab'])
                    for k in range(4):
                        S.dve(lambda e, k=k: e.tensor_scalar(out=oh[:], in0=lg[:], scalar1=top8[:, k:k + 1], scalar2=None, op0=ALU.is_equal), r=['lg', 'top8'], w=['oh'])
                        S.dve(lambda e, k=k: e.scalar_tensor_tensor(out=tmp32[:], in0=oh[:], scalar=1.0, in1=dtab[:], op0=ALU.mult, op1=ALU.mult, accum_out=destf[:, k:k + 1]),
                              r=['oh', 'dtab'], w=['tmp32', 'destf'])
                        S.dve(lambda e, k=k, i=i: e.scalar_tensor_tensor(out=tmp32[:], in0=oh[:], scalar=1.0, in1=gates[:, i * 32:(i + 1) * 32], op0=ALU.mult, op1=ALU.mult,
                                                                       accum_out=gk[:, 4 * i + k:4 * i + k + 1]),
                              r=['oh', ('gates', i), 'gk0'], w=['tmp32', ('gk', i)])
                    S.dve(lambda e, i=i: e.tensor_copy(out=desti[:, 4 * i:4 * i + 4], in_=destf[:]), r=['destf'], w=[('desti', i)])
                    S.dve(lambda e: e.tensor_scalar(out=okf[:], in0=destf[:], scalar1=float(NROWS), scalar2=None, op0=ALU.is_lt), r=['destf'], w=['okf'])
                    S.dve(lambda e, i=i: e.tensor_tensor(out=gk[:, 4 * i:4 * i + 4], in0=gk[:, 4 * i:4 * i + 4], in1=okf[:], op=ALU.mult), r=['okf', ('gk', i)], w=[('gk', i)])
                    for k in range(4):
                        S.dma(lambda e, k=k, i=i, hib=hib: e.indirect_dma_start(
                            out=xs[:, :], out_offset=bass.IndirectOffsetOnAxis(ap=desti[:, 4 * i + k:4 * i + k + 1], axis=0),
                            in_=hib[:, :], in_offset=None, bounds_check=S.reg(e, NROWS - 1), oob_is_err=False),
                            'sc%d' % (i % 2), r=[hbt, ('desti', i)], q='pool')
                S.dve(lambda e, i=i: e.tensor_copy(out=gb[:], in_=gates[:, i * 32:(i + 1) * 32]), r=[('gates', i)], w=['gb'])
                S.pe(lambda e: e.transpose(out=ptr[1][0:32, 0, :], in_=gb[:], identity=ident_b[:]), r=['gb', 'ident_b'], w=[('ptr', 1)])
                S.act(lambda e: e.copy(out=gT[:], in_=ptr[1][0:32, 0, :]), r=[('ptr', 1)], w=['gT'])
                for half in range(2):
                    hc = slice(half * 512, (half + 1) * 512)
                    S.pe(lambda e, half=half, hc=hc: e.matmul(plb[half][:], lhsT=gT[:], rhs=bdb[:, hc], start=True, stop=True), r=['gT', 'bdb'], w=[('plb', half)])
                    S.dve(lambda e, i=i, half=half, hc=hc: e.tensor_tensor(out=xn[:, i, hc], in0=xn[:, i, hc], in1=plb[half][:], op=ALU.add),
                          r=[('plb', half), ('xn', i, half)], w=[('xn', i, half)])
            if debug:
                S.dma(lambda e: e.dma_start(out=dbg_gates, in_=gates[:]), 'dbg6', r=[('gates', i) for i in range(16)])
                final_keys.append('dbg6')
            S.flush()
            if upto == 'E':
                S.enabled = False

        if stage >= 2:
            with ExitStack() as pf:
                KC = CAP // 128
                bgu = T(pf, "bgu", [128, 512], F32)
                xg = [T(pf, "xg0", [128, KC, 1024], BF16), T(pf, "xg1", [128, KC, 1024], BF16)]
                selT = [T(pf, "selT0", [128, 8, CAP], BF16), T(pf, "selT1", [128, 8, CAP], BF16)]
                actT = T(pf, "actT", [128, 8, CAP], BF16)
                yo = T(pf, "yo", [128, KC, 1024], F32)
                g1 = T(pf, "g1", [128, CAP], F32)
                sg = T(pf, "sg", [128, CAP], F32)
                ur = T(pf, "ur", [128, CAP], F32)
                tt = T(pf, "tt", [128, CAP], F32)
                pTx = [PS(pf, "pTx0", [128, 8, 128], BF16), PS(pf, "pTx1", [128, 8, 128], BF16)]
                pG = [PS(pf, "pG0", [128, 512], F32), PS(pf, "pG1", [128, 512], F32)]
                pUp = [PS(pf, "pUp0", [128, 512], F32), PS(pf, "pUp1", [128, 512], F32)]
                pY = [PS(pf, "pY0", [128, 512], F32), PS(pf, "pY1", [128, 512], F32)]
                S.dma(lambda e: e.dma_start(out=bgu[:], in_=b_gu_d), 'f0', w=['bgu'])

                def load_x(e_):
                    S.dma(lambda e, e_=e_: e.dma_start(out=xg[e_ % 2][:], in_=xs[e_ * CAP:(e_ + 1) * CAP, :].rearrange("(k p) d -> p k d", p=128)),
                          'xg%d' % (e_ % 2), w=[('xg', e_ % 2)])

                load_x(0)
                blk = 0
                for ex_ in range(32):
                    sl_ = ex_ % 2
                    if ex_ + 1 < 32:
                        load_x(ex_ + 1)
                    for k in range(KC):
                        for c in range(8):
                            S.pe(lambda e, k=k, c=c, sl_=sl_: e.transpose(out=pTx[k % 2][:, c, :], in_=xg[sl_][:, k, c * 128:(c + 1) * 128], identity=ident_b[:]),
                                 r=[('xg', sl_), 'ident_b'], w=[('pTx', k % 2)])
                        if k % 2 == 0:
                            S.act(lambda e, k=k, sl_=sl_: e.copy(out=selT[sl_][:, :, k * 128:(k + 1) * 128], in_=pTx[k % 2][:]), r=[('pTx', k % 2)], w=[('selT', sl_, k)])
                        else:
                            S.dve(lambda e, k=k, sl_=sl_: e.tensor_copy(out=selT[sl_][:, :, k * 128:(k + 1) * 128], in_=pTx[k % 2][:]), r=[('pTx', k % 2)], w=[('selT', sl_, k)])
                    sr = [('selT', sl_, k) for k in range(KC)]
                    W = Wgu[sl_]
                    for fc in range(8):
                        pb_ = blk % 2
                        blk += 1
                        for c in range(8):
                            S.pe(lambda e, fc=fc, c=c, pb_=pb_, W=W, sl_=sl_: e.matmul(pG[pb_][:, 0:CAP], lhsT=W[:, c, fc * 128:(fc + 1) * 128], rhs=selT[sl_][:, c, :],
                                                                                 start=(c == 0), stop=(c == 7)),
                                 r=sr + [('Wgu', sl_)], w=[('pG', pb_)])
                        for c in range(8):
                            S.pe(lambda e, fc=fc, c=c, pb_=pb_, W=W, sl_=sl_: e.matmul(pUp[pb_][:, 0:CAP], lhsT=W[:, c, 1024 + fc * 128:1024 + (fc + 1) * 128], rhs=selT[sl_][:, c, :],
                                                                                 start=(c == 0), stop=(c == 7)),
                                 r=sr + [('Wgu', sl_)], w=[('pUp', pb_)])
                        bg = bgu[:, ex_ * 16 + fc:ex_ * 16 + fc + 1]
                        bu = bgu[:, ex_ * 16 + 8 + fc:ex_ * 16 + 8 + fc + 1]
                        S.dve(lambda e, pb_=pb_, bg=bg: e.tensor_scalar(out=g1[:], in0=pG[pb_][:, 0:CAP], scalar1=bg, scalar2=7.0, op0=ALU.add, op1=ALU.min),
                              r=[('pG', pb_), 'bgu'], w=['g1'])
                        S.act(lambda e: e.activation(out=sg[:], in_=g1[:], func=AF.Sigmoid, scale=1.702), r=['g1'], w=['sg'])
                        S.act(lambda e, pb_=pb_, bu=bu: e.activation(out=ur[:], in_=pUp[pb_][:, 0:CAP], func=AF.Identity, bias=bu, scale=1.0),
                              r=[('pUp', pb_), 'bgu'], w=['ur'])
                        S.dve(lambda e: e.tensor_scalar(out=ur[:], in0=ur[:], scalar1=7.0, scalar2=-7.0, op0=ALU.min, op1=ALU.max), r=['ur'], w=['ur'])
                        S.dve(lambda e: e.tensor_tensor(out=tt[:], in0=g1[:], in1=sg[:], op=ALU.mult), r=['g1', 'sg'], w=['tt'])
                        S.dve(lambda e, fc=fc: e.scalar_tensor_tensor(out=actT[:, fc, :], in0=ur[:], scalar=1.0, in1=tt[:], op0=ALU.add, op1=ALU.mult),
                              r=['ur', 'tt'], w=[('actT', fc)])
                    if ex_ + 2 < 32:
                        load_gu(ex_ + 2)
                    for k in range(KC):
                        for half in range(2):
                            hc = slice(half * 512, (half + 1) * 512)
                            for fc in range(8):
                                S.pe(lambda e, fc=fc, k=k, half=half, hc=hc: e.matmul(pY[half][:], lhsT=actT[:, fc, k * 128:(k + 1) * 128], rhs=Wd[:, fc, hc],
                                                                                 start=(fc == 0), stop=(fc == 7)),
                                     r=[('actT', fc), 'Wd'], w=[('pY', half)])
                            if half == 0:
                                S.act(lambda e, k=k, half=half, hc=hc: e.copy(out=yo[:, k, hc], in_=pY[half][:]), r=[('pY', half)], w=[('yo', k, half)])
                            else:
                                S.dve(lambda e, k=k, half=half, hc=hc: e.tensor_copy(out=yo[:, k, hc], in_=pY[half][:]), r=[('pY', half)], w=[('yo', k, half)])
                    if ex_ + 1 < 32:
                        load_d(ex_ + 1)
                    S.dma(lambda e, ex_=ex_: e.dma_start(out=ys[ex_ * CAP:(ex_ + 1) * CAP, :].rearrange("(k p) d -> p k d", p=128), in_=yo[:]),
                          'ys', r=[('yo', k, h_) for k in range(KC) for h_ in range(2)])
                S.flush()
                if upto == 'F':
                    S.enabled = False
            with ExitStack() as pf2:
                NG = 8
                gbf = BIG2[:].bitcast(F32)
                gbuf = [gbf[:, k * 1024:(k + 1) * 1024] for k in range(NG)]
                S.pool(lambda e: e.memset(gbf[:, 0:NG * 1024], 0.0), w=[('gbuf', k) for k in range(NG)])
                for i in range(16):
                    for k in range(4):
                        j = 4 * i + k
                        gs = j % NG
                        S.dma(lambda e, j=j, gs=gs: e.indirect_dma_start(
                            out=gbuf[gs], out_offset=None, in_=ys[:, :],
                            in_offset=bass.IndirectOffsetOnAxis(ap=desti[:, j:j + 1], axis=0), bounds_check=S.reg(e, NROWS - 1), oob_is_err=False),
                            'ga%d' % gs, w=[('gbuf', gs)], q='pool')
                        S.dve(lambda e, i=i, j=j, gs=gs: e.scalar_tensor_tensor(out=xn[:, i, :], in0=gbuf[gs], scalar=gk[:, j:j + 1], in1=xn[:, i, :], op0=ALU.mult, op1=ALU.add),
                              r=[('gbuf', gs), ('xn', i, 0), ('xn', i, 1)], w=[('xn', i, 0), ('xn', i, 1)])

        S.enabled = True
        with ExitStack() as pg:
            gfin = T(pg, "gfin", [128, 1024], F32)
            ot = [T(pg, "ot0", [128, 1024], F32), T(pg, "ot1", [128, 1024], F32)]
            ss3 = T(pg, "ss3", [128, 16], F32)
            S.dma(lambda e: e.dma_start(out=gfin[:], in_=g_fin_d.partition_broadcast(128)), 'g0', w=['gfin'])
            for i in range(16):
                S.act(lambda e, i=i: e.activation(out=junk[:], in_=xn[:, i, :], func=AF.Square, accum_out=ss3[:, i:i + 1]),
                      r=[('xn', i, 0), ('xn', i, 1)], w=['junk', ('ss3q', i)])
            S.act(lambda e: e.activation(out=ss3[:], in_=ss3[:], func=AF.Ln, bias=epsc[:, 0:1], scale=1.0 / 1024),
                  r=[('ss3q', i) for i in range(16)] + ['epsc'], w=['ss3all'])
            S.act(lambda e: e.activation(out=ss3[:], in_=ss3[:], func=AF.Exp, scale=-0.5), r=['ss3all'], w=['ss3all'])
            for i in range(16):
                xt = [('xn', i, 0), ('xn', i, 1)]
                S.dve(lambda e, i=i: e.scalar_tensor_tensor(out=ot[i % 2][:], in0=xn[:, i, :], scalar=ss3[:, i:i + 1], in1=gfin[:], op0=ALU.mult, op1=ALU.mult),
                      r=xt + ['ss3all', 'gfin'], w=[('ot', i % 2)])
                S.dma(lambda e, i=i: e.dma_start(out=y[i * 128:(i + 1) * 128, :], in_=ot[i % 2][:]), 'yo%d' % (i % 2), r=[('ot', i % 2)])
            final_keys.extend(['yo0', 'yo1'])
            S.flush(final_dma_keys=final_keys)
    return nc


def make_in_maps(stage, x, positions, g_attn_norm, w_in, w_pool, b_pool, pool_scale, g_q_a, w_q_b,
                 g_kv_a, w_kv_b, g_out_pool, g_out_attn, w_out, g_ffn_norm, w_router, b_router,
                 w_gate_up, b_gate_up, w_down, b_down, g_final):
    f32 = lambda a: np.ascontiguousarray(np.asarray(a, dtype=np.float32))
    x = f32(x)
    positions = np.asarray(positions, dtype=np.int32)
    inv = (10000.0 ** (-np.arange(16, dtype=np.float32) / 16)).astype(np.float32)
    invf = np.zeros((128, 1), np.float32)
    invf[64:80, 0] = inv
    invf[80:96, 0] = inv
    shared = dict(
        invf=invf,
        g_attn=f32(np.asarray(g_attn_norm)[0].reshape(8, 128).T),
        w_in=f32(np.asarray(w_in)[0]),
        w_pool=f32(np.asarray(w_pool)[0].transpose(1, 0, 2).reshape(128, 512)),
        b_pool=f32(np.asarray(b_pool)[0].T),
        pool_scale=f32(np.asarray(pool_scale)[0].reshape(4, 128).T),
        g_q=f32(np.asarray(g_q_a)[0].reshape(2, 128).T),
        w_q_b=f32(np.asarray(w_q_b)[0]),
        g_kv=f32(np.asarray(g_kv_a)[0].reshape(128, 1)),
        w_kv_b=f32(np.asarray(w_kv_b)[0]),
        g_op=f32(np.asarray(g_out_pool)[0].reshape(4, 128).T),
        g_oa=f32(np.asarray(g_out_attn)[0].reshape(8, 64).T),
        w_out=f32(np.asarray(w_out)[0]),
        g_ffn=f32(np.asarray(g_ffn_norm)[0].reshape(1, 1024)),
        g_fin=f32(np.asarray(g_final).reshape(1, 1024)),
        w_r=f32(np.asarray(w_router)[0]),
        b_r=f32(np.asarray(b_router)[0].reshape(1, 32)),
        b_gu=f32(np.asarray(b_gate_up)[0].reshape(32, 16, 128).transpose(2, 0, 1).reshape(128, 512)),
        b_d=f32(np.asarray(b_down)[0].reshape(32, 1024)),
    )
    wgu = f32(np.asarray(w_gate_up)[0])
    wdn = f32(np.asarray(w_down)[0])
    for k in range(4 if stage >= 2 else 0):
        shared["w_gu%d" % k] = wgu[8 * k:8 * k + 8]
        shared["w_d%d" % k] = wdn[8 * k:8 * k + 8]
    in_maps = []
    kk = np.arange(128)[:, None]
    qq = np.arange(128)[None, :]
    tri = (kk <= qq)
    for c in range(8):
        b, j = c // 4, c % 4
        xb = x[b]
        xo = np.zeros((16, 144, 1024), np.float32)
        po = np.zeros((16, 128), np.int32)
        for i in range(16):
            n = 4 * i + j
            lo = 128 * n - 16
            if lo < 0:
                xo[i, 16:] = xb[0:128]
            else:
                xo[i] = xb[lo:lo + 144]
            po[i] = positions[b, 128 * n:128 * n + 128]
        mk = np.zeros((128, 16, 4, 128), np.float32)
        for m in range(16):
            for r in range(4):
                if m < 4 * r + j:
                    mk[:, m, r, :] = 1.0
                elif m == 4 * r + j:
                    mk[:, m, r, :] = tri
        ic = np.zeros((128, 4, 16), np.float32)
        for gi, wdw in enumerate((2, 4, 8, 16)):
            if j == 0:
                ic[:, gi, :] = 1.0 / np.minimum(np.arange(16) + 1, wdw).astype(np.float32)
            else:
                ic[:, gi, :] = 1.0 / wdw
        m = dict(shared)
        m.update(
            x_all=np.ascontiguousarray(xb),
            x_own=xo.reshape(2304, 1024),
            pos_all=np.ascontiguousarray(positions[b].reshape(1, 8192)),
            pos_own=po.reshape(1, 2048),
            masks=mk.reshape(128, 8192).astype(ml_dtypes.bfloat16),
            invcnt=ic.reshape(128, 64),
        )
        in_maps.append(m)
    return in_maps


def assemble(results, key="y"):
    out = np.zeros((2, 8192, 1024), np.float32)
    for c in range(8):
        b, j = c // 4, c % 4
        yc = np.asarray(results[c][key]).reshape(16, 128, 1024)
        for i in range(16):
            n = 4 * i + j
            out[b, 128 * n:128 * n + 128] = yc[i]
    return out


def kernel(**inputs):
    nc = build()
    in_maps = make_in_maps(99, **inputs)
    res = run_bass_kernel_spmd(nc, in_maps, core_ids=list(range(8)))
    return assemble(res.results)
```
